# Optimizing a Trainium2 kernel written in Bass

```python
import jax, jax.numpy as jnp
from jax import lax
import numpy as np

D_MODEL = 2048
BATCH = 4
SEQ = 4096
DEPTH = 4
DEC_BATCH = 32
DEC_SEQ = 16
PAST_LEN = 2048

CHUNK = 64
GROUP_W = D_MODEL // 4
SC_WIDTH = 3
GLA_HEADS = 4
GLA_DV = GROUP_W // GLA_HEADS
GLA_DK = GLA_DV // 2
GLA_RANK = 16
GATE_TAU = 16.0
GLA_BLOCK = CHUNK
CC_WIDTH = 31
ATT_HD = 64
ATT_HEADS = GROUP_W // ATT_HD
BAND_PREV = 8
REL_CLIP = 128
D_FF = 256 * ((8 * D_MODEL // 3 + 255) // 256)
FFN_CONV_WIDTH = 3
NORM_EPS = 1e-6
MIX_W = 4 * GROUP_W
SPLIT_SIZES = (GROUP_W, GROUP_W, GROUP_W,
               GLA_HEADS * GLA_DK, GLA_HEADS * GLA_DK, GROUP_W, GROUP_W, GLA_RANK,
               GROUP_W, GROUP_W,
               GROUP_W, GROUP_W, GROUP_W)
IN_COLS = sum(SPLIT_SIZES)
SPLIT_POINTS = tuple(int(s) for s in np.cumsum(SPLIT_SIZES)[:-1])

kernel_name = "hybrid_stream_encoder_step"


def rmsnorm(x, g):
    xf = x.astype(jnp.float32)
    y = xf * lax.rsqrt(jnp.mean(xf * xf, axis=-1, keepdims=True) + NORM_EPS)
    return (y * g.astype(jnp.float32)).astype(x.dtype)


def layernorm(x, g, b):
    xf = x.astype(jnp.float32)
    mu = jnp.mean(xf, axis=-1, keepdims=True)
    var = jnp.mean(jnp.square(xf - mu), axis=-1, keepdims=True)
    y = (xf - mu) * lax.rsqrt(var + NORM_EPS) * g.astype(jnp.float32) + b.astype(jnp.float32)
    return y.astype(x.dtype)


def causal_dwconv(x, w, hist):
    K, C = w.shape
    xp = jnp.concatenate([hist.astype(x.dtype), x], axis=1)
    y = lax.conv_general_dilated(xp, w[:, None, :].astype(x.dtype), window_strides=(1,),
                                 padding='VALID', dimension_numbers=('NWC', 'WIO', 'NWC'),
                                 feature_group_count=C)
    return y, xp[:, xp.shape[1] - (K - 1):]


def gla_recurrence(q, k, v, loga, s0):
    B, T, H, DK = q.shape
    DV = v.shape[-1]
    L = T if T <= GLA_BLOCK else GLA_BLOCK
    n = T // L
    f32 = jnp.float32
    qc = q.astype(f32).reshape(B, n, L, H, DK)
    kc = k.astype(f32).reshape(B, n, L, H, DK)
    vc = v.astype(f32).reshape(B, n, L, H, DV)
    b = jnp.cumsum(loga.astype(f32).reshape(B, n, L, H, DK), axis=2)
    b_last = b[:, :, -1]
    ref = b[:, :, L // 2][:, :, None]
    a = jnp.einsum('bnihd,bnjhd->bnhij', qc * jnp.exp(b - ref), kc * jnp.exp(ref - b))
    a = jnp.where(jnp.tril(jnp.ones((L, L), dtype=bool)), a, 0.0)
    o_intra = jnp.einsum('bnhij,bnjhv->bnihv', a, vc)
    u = jnp.einsum('bnjhd,bnjhv->bnhdv', kc * jnp.exp(b_last[:, :, None] - b), vc)
    decay = jnp.exp(b_last)

    def step(s, inp):
        d, uu = inp
        return d[..., None] * s + uu, s

    s_final, s_in = lax.scan(step, s0.astype(f32), (jnp.swapaxes(decay, 0, 1), jnp.swapaxes(u, 0, 1)))
    s_in = jnp.swapaxes(s_in, 0, 1)
    o_inter = jnp.einsum('bnihd,bnhdv->bnihv', qc * jnp.exp(b), s_in)
    return (o_intra + o_inter).reshape(B, T, H, DV), s_final


def band_attention(q, k, v, rel_bias):
    B, T, H, D = q.shape
    nc = T // CHUNK
    band_len = (BAND_PREV + 1) * CHUNK
    qc = q.reshape(B, nc, CHUNK, H, D)
    pad = ((0, 0), (BAND_PREV * CHUNK, 0), (0, 0), (0, 0))
    kp = jnp.pad(k, pad).reshape(B, nc + BAND_PREV, CHUNK, H, D)
    vp = jnp.pad(v, pad).reshape(B, nc + BAND_PREV, CHUNK, H, D)
    idx = jnp.arange(nc)[:, None] + jnp.arange(BAND_PREV + 1)[None, :]
    kb = kp[:, idx].reshape(B, nc, band_len, H, D)
    vb = vp[:, idx].reshape(B, nc, band_len, H, D)
    s = jnp.einsum('bcqhd,bckhd->bchqk', qc, kb).astype(jnp.float32)
    i = jnp.arange(CHUNK)[:, None]
    j = jnp.arange(band_len)[None, :]
    dist = i + BAND_PREV * CHUNK - j
    bias = rel_bias[:, jnp.clip(dist, -REL_CLIP, REL_CLIP) + REL_CLIP].astype(jnp.float32)
    kpos = (jnp.arange(nc)[:, None] - BAND_PREV) * CHUNK + jnp.arange(band_len)[None, :]
    s = jnp.where((kpos >= 0)[None, :, None, None, :], s + bias[None, None], -1e30)
    p = jax.nn.softmax(s, axis=-1).astype(v.dtype)
    return jnp.einsum('bchqk,bckhd->bcqhd', p, vb).reshape(B, T, H, D)


def cached_attention(q, k, v, k_cache, v_cache, rel_bias):
    Lc = k_cache.shape[1]
    Tn = q.shape[1]
    kk = jnp.concatenate([k_cache.astype(k.dtype), k], axis=1)
    vv = jnp.concatenate([v_cache.astype(v.dtype), v], axis=1)
    s = jnp.einsum('bqhd,bkhd->bhqk', q, kk).astype(jnp.float32)
    dist = (Lc + jnp.arange(Tn))[:, None] - jnp.arange(Lc + Tn)[None, :]
    bias = rel_bias[:, jnp.clip(dist, -REL_CLIP, REL_CLIP) + REL_CLIP].astype(jnp.float32)
    p = jax.nn.softmax(s + bias[None], axis=-1).astype(v.dtype)
    return jnp.einsum('bhqk,bkhd->bqhd', p, vv)


def layer(x, lp, states):
    (g_mix, w_in, conv_a_w, gla_w_gate2, gla_b_gate, gla_g_norm, cconv_w, cconv_b,
     cln_g, cln_b, rel_bias, w_out, g_ffn, w_ffn_gate, w_ffn_up, ffn_conv_w, w_ffn_down) = lp
    B, T, _ = x.shape
    if states is None:
        hist_a = jnp.zeros((B, SC_WIDTH - 1, GROUP_W), x.dtype)
        s_gla = jnp.zeros((B, GLA_HEADS, GLA_DK, GLA_DV), jnp.float32)
        hist_c = jnp.zeros((B, CC_WIDTH - 1, GROUP_W), x.dtype)
        hist_f = jnp.zeros((B, FFN_CONV_WIDTH - 1, D_FF), x.dtype)
    else:
        hist_a, s_gla, hist_c, k_cache, v_cache, hist_f = states

    h = rmsnorm(x, g_mix)
    z = h @ w_in
    (a_x, a_b, a_c, g_q, g_k, g_v, g_g, g_r, c_v, c_g, d_q, d_k, d_v) = jnp.split(z, SPLIT_POINTS, axis=-1)

    conv_u, new_hist_a = causal_dwconv(a_c * a_x, conv_a_w, hist_a)
    y_a = a_b * conv_u

    q = g_q.reshape(B, T, GLA_HEADS, GLA_DK) * (GLA_DK ** -0.5)
    k = g_k.reshape(B, T, GLA_HEADS, GLA_DK)
    v = g_v.reshape(B, T, GLA_HEADS, GLA_DV)
    loga = jax.nn.log_sigmoid((g_r @ gla_w_gate2 + gla_b_gate).astype(jnp.float32)) / GATE_TAU
    o, new_s_gla = gla_recurrence(q, k, v, loga.reshape(B, T, GLA_HEADS, GLA_DK), s_gla)
    o = rmsnorm(o, gla_g_norm).astype(x.dtype)
    y_b = (o * jax.nn.silu(g_g.reshape(B, T, GLA_HEADS, GLA_DV))).reshape(B, T, GROUP_W)

    cc, new_hist_c = causal_dwconv(c_v * jax.nn.sigmoid(c_g), cconv_w, hist_c)
    y_c = jax.nn.silu(layernorm(cc + cconv_b.astype(x.dtype), cln_g, cln_b))

    aq = d_q.reshape(B, T, ATT_HEADS, ATT_HD) * (ATT_HD ** -0.5)
    ak = d_k.reshape(B, T, ATT_HEADS, ATT_HD)
    av = d_v.reshape(B, T, ATT_HEADS, ATT_HD)
    if states is None:
        od = band_attention(aq, ak, av, rel_bias)
        keep = min(BAND_PREV * CHUNK, T)
        k_rows, v_rows = ak[:, T - keep:], av[:, T - keep:]
    else:
        od = cached_attention(aq, ak, av, k_cache, v_cache, rel_bias)
        k_rows, v_rows = ak, av
    y_d = od.reshape(B, T, GROUP_W)

    x = x + jnp.concatenate([y_a, y_b, y_c, y_d], axis=-1) @ w_out

    h = rmsnorm(x, g_ffn)
    gt, new_hist_f = causal_dwconv(h @ w_ffn_gate, ffn_conv_w, hist_f)
    x = x + (jax.nn.silu(gt) * (h @ w_ffn_up)) @ w_ffn_down
    return x, (new_hist_a, new_s_gla, new_hist_c, k_rows, v_rows, new_hist_f)


def setup_inputs(seed: int = 0) -> dict:
    key = jax.random.key(seed)
    ks = iter(jax.random.split(key, 40))
    nrm = lambda shape, scale: jax.random.normal(next(ks), shape, jnp.float32) * scale
    kv_len = min(BAND_PREV * CHUNK, PAST_LEN)
    return {
        "x_prompt": nrm((BATCH, SEQ, D_MODEL), 1.0),
        "x_sample": nrm((DEC_BATCH, DEC_SEQ, D_MODEL), 1.0),
        "state_short_conv": nrm((DEPTH, DEC_BATCH, SC_WIDTH - 1, GROUP_W), 1.0),
        "state_gla": nrm((DEPTH, DEC_BATCH, GLA_HEADS, GLA_DK, GLA_DV), 0.5),
        "state_conformer_conv": nrm((DEPTH, DEC_BATCH, CC_WIDTH - 1, GROUP_W), 1.0),
        "cache_attn_k": nrm((DEPTH, DEC_BATCH, kv_len, ATT_HEADS, ATT_HD), 1.0),
        "cache_attn_v": nrm((DEPTH, DEC_BATCH, kv_len, ATT_HEADS, ATT_HD), 1.0),
        "state_ffn_conv": nrm((DEPTH, DEC_BATCH, FFN_CONV_WIDTH - 1, D_FF), 1.0),
        "g_mix": 1.0 + nrm((DEPTH, D_MODEL), 0.02),
        "w_in": nrm((DEPTH, D_MODEL, IN_COLS), D_MODEL ** -0.5),
        "conv_a_w": nrm((DEPTH, SC_WIDTH, GROUP_W), SC_WIDTH ** -0.5),
        "gla_w_gate2": nrm((DEPTH, GLA_RANK, GLA_HEADS * GLA_DK), GLA_RANK ** -0.5),
        "gla_b_gate": nrm((DEPTH, GLA_HEADS * GLA_DK), 0.1),
        "gla_g_norm": 1.0 + nrm((DEPTH, GLA_DV), 0.02),
        "cconv_w": nrm((DEPTH, CC_WIDTH, GROUP_W), CC_WIDTH ** -0.5),
        "cconv_b": nrm((DEPTH, GROUP_W), 0.02),
        "cln_g": 1.0 + nrm((DEPTH, GROUP_W), 0.02),
        "cln_b": nrm((DEPTH, GROUP_W), 0.02),
        "rel_bias": nrm((DEPTH, ATT_HEADS, 2 * REL_CLIP + 1), 0.5),
        "w_out": nrm((DEPTH, MIX_W, D_MODEL), MIX_W ** -0.5),
        "g_ffn": 1.0 + nrm((DEPTH, D_MODEL), 0.02),
        "w_ffn_gate": nrm((DEPTH, D_MODEL, D_FF), D_MODEL ** -0.5),
        "w_ffn_up": nrm((DEPTH, D_MODEL, D_FF), D_MODEL ** -0.5),
        "ffn_conv_w": nrm((DEPTH, FFN_CONV_WIDTH, D_FF), FFN_CONV_WIDTH ** -0.5),
        "w_ffn_down": nrm((DEPTH, D_FF, D_MODEL), D_FF ** -0.5),
        "g_final": 1.0 + nrm((D_MODEL,), 0.02),
    }


def reference(x_prompt, x_sample, state_short_conv, state_gla, state_conformer_conv,
              cache_attn_k, cache_attn_v, state_ffn_conv, g_mix, w_in, conv_a_w,
              gla_w_gate2, gla_b_gate, gla_g_norm, cconv_w, cconv_b, cln_g, cln_b,
              rel_bias, w_out, g_ffn, w_ffn_gate, w_ffn_up, ffn_conv_w, w_ffn_down, g_final):
    yp, ys = x_prompt, x_sample
    p_out = [[], [], [], [], [], []]
    s_out = [[], [], [], [], [], []]
    for l in range(DEPTH):
        lp = (g_mix[l], w_in[l], conv_a_w[l], gla_w_gate2[l], gla_b_gate[l], gla_g_norm[l],
              cconv_w[l], cconv_b[l], cln_g[l], cln_b[l], rel_bias[l], w_out[l], g_ffn[l],
              w_ffn_gate[l], w_ffn_up[l], ffn_conv_w[l], w_ffn_down[l])
        yp, pst = layer(yp, lp, None)
        ys, sst = layer(ys, lp, (state_short_conv[l], state_gla[l], state_conformer_conv[l],
                                 cache_attn_k[l], cache_attn_v[l], state_ffn_conv[l]))
        for lst, a in zip(p_out, pst):
            lst.append(a)
        for lst, a in zip(s_out, sst):
            lst.append(a)
    y_prompt = rmsnorm(yp, g_final)
    y_sample = rmsnorm(ys, g_final)
    p_short, p_gla, p_cconv, p_k, p_v, p_ffn = [jnp.stack(a) for a in p_out]
    s_short, s_gla, s_cconv, s_k, s_v, s_ffn = [jnp.stack(a) for a in s_out]
    return (y_prompt, y_sample, p_short, p_gla, p_cconv, p_k, p_v, p_ffn,
            s_short, s_gla, s_cconv, s_k, s_v, s_ffn)
```

```python
import math
from contextlib import ExitStack

import numpy as np
import concourse.bass as bass
import concourse.mybir as mybir
from concourse.bass_utils import run_bass_kernel_spmd

F32 = mybir.dt.float32
BF16 = mybir.dt.bfloat16
AF = mybir.ActivationFunctionType
ALU = mybir.AluOpType
AX = mybir.AxisListType

D = 2048
GW = 512
DFF = 5632
INC = 5648
EPS = 1e-6
DEPTH = 4
O_XA, O_BG, O_CG, O_Q, O_K, O_V, O_G, O_R, O_CV, O_CGT, O_DQ, O_DK, O_DV = (
    0, 512, 1024, 1536, 1792, 2048, 2560, 3072, 3088, 3600, 4112, 4624, 5136)

PV_GMIX, PV_GFFN, PV_CAW, PV_CCW, PV_CCB, PV_CLG, PV_CLB, PV_GLAB, PV_GLAG, PV_FCW, PV_GFIN = (
    0, 16, 32, 44, 168, 172, 176, 180, 182, 183, 315)
PV_GLAB4 = 331
NV = 335


class Sem:
    __slots__ = ("h", "n")

    def __init__(self, h):
        self.h = h
        self.n = 0


class Tile:
    __slots__ = ("ap", "w", "r", "war", "sem", "excl")

    def __init__(self, ap, sem=None, excl=False):
        self.ap = ap
        self.excl = excl
        self.w = []
        self.r = []
        self.war = []
        self.sem = sem

    def __getitem__(self, idx):
        return self.ap[idx]


def _merge(evs):
    d = {}
    for S, v in evs:
        if v > d.get(S, 0):
            d[S] = v
    return list(d.items())


class Eng:
    def __init__(self, eng, S, selfsync):
        self.eng = eng
        self.S = S
        self.selfsync = selfsync
        self.seen = {}

    def wait_for(self, evs):
        for S, v in _merge(evs):
            if S is self.S and not self.selfsync:
                continue
            if self.seen.get(S, 0) >= v:
                continue
            self.eng.wait_ge(S.h, v)
            self.seen[S] = v

    @staticmethod
    def _deps(r, w, wm):
        evs = []
        for t in r:
            evs += t.w
            if t.excl:
                evs += t.r
        for t in w:
            evs += t.w
            evs += t.r
            evs += t.war
        for t in wm:
            if t.r:
                t.war = _merge(t.war + t.r + t.w)
                t.w = []
                t.r = []
            evs += t.war
        return evs

    @staticmethod
    def _post(ev, r, w, wm):
        for t in r:
            t.r = _merge(t.r + [ev])
        for t in w:
            t.w = [ev]
            t.r = []
            t.war = []
        for t in wm:
            t.w = _merge(t.w + [ev])

    def op(self, fn, r=(), w=(), wm=()):
        self.wait_for(self._deps(r, w, wm))
        inst = fn(self.eng)
        self.S.n += 1
        inst.then_inc(self.S.h, 1)
        self._post((self.S, self.S.n), r, w, wm)

    def dma(self, out, in_, S, r=(), w=(), wm=(), **kw):
        evs = self._deps(r, w, wm)
        if S.n:
            evs.append((S, S.n))
        self.wait_for(evs)
        inst = self.eng.dma_start(out=out, in_=in_, **kw)
        S.n += 16
        inst.then_inc(S.h, 16)
        self._post((S, S.n), r, w, wm)


def split_even(n, maxw, mult=16):
    k = (n + maxw - 1) // maxw
    base = ((n + k - 1) // k + mult - 1) // mult * mult
    out = []
    s = 0
    while s < n:
        w = min(base, n - s)
        out.append((s, w))
        s += w
    return out


class K:
    def __init__(self, TP, NS, depth, debug=()):
        self.TP, self.NS, self.depth = TP, NS, depth
        self.TS = 16 * NS
        self.TT = TP + self.TS
        self.debug = set(debug)
        self.nc = nc = bass.Bass("TRN2", target_bir_lowering=False)
        self.es = ExitStack()
        self.sems_free = []
        self.all_sems = []
        S = lambda: self._new_sem()
        self.pe = Eng(nc.tensor, S(), False)
        self.act = Eng(nc.scalar, S(), True)
        self.dve = Eng(nc.vector, S(), True)
        self.pool = Eng(nc.gpsimd, S(), True)
        self.sp = Eng(nc.sync, S(), True)
        self.engs = [self.pe, self.act, self.dve, self.pool, self.sp]
        self.sw_sems = [S() for _ in range(2)]
        self.scr_sem = S()
        self.dma_sems = [S() for _ in range(21)]
        self._dma_rr = 0
        self._evac_rr = 0

    def _new_sem(self):
        h = self.es.enter_context(self.nc.semaphore(f"s{len(self.all_sems)}"))
        s = Sem(h)
        self.all_sems.append(s)
        return s

    def dsem(self):
        s = self.dma_sems[self._dma_rr % len(self.dma_sems)]
        self._dma_rr += 1
        return s

    def sb(self, es, name, shape, dtype, dma=False):
        self._uid = getattr(self, '_uid', 0) + 1
        t = es.enter_context(self.nc.sbuf_tensor(f"{name}_{self._uid}", list(shape), dtype))
        if dma == "sw0" or dma == "sw1":
            return Tile(t, self.sw_sems[int(dma[2])])
        return Tile(t, self.dsem() if dma else None)

    def dram_in(self, name, shape, dtype=F32):
        return self.nc.dram_tensor(name, list(shape), dtype, kind="ExternalInput").ap()

    def dram_out(self, name, shape, dtype=F32):
        return self.nc.dram_tensor(name, list(shape), dtype, kind="ExternalOutput").ap()

    def scratch(self, name, shape, dtype):
        kind = "ExternalOutput" if name in self.debug else "Internal"
        ap = self.nc.dram_tensor(name, list(shape), dtype, kind=kind).ap()
        return Tile(ap)

    def barrier(self):
        evs = [(s, s.n) for s in self.all_sems if s.n]
        for e in self.engs:
            e.wait_for(evs)

    def evac_eng(self):
        self._evac_rr += 1
        return self.act if self._evac_rr % 2 else self.dve

    def copy(self, eng, out, in_, **kw):
        if eng is self.act:
            eng.op(lambda e: e.activation(out=out, in_=in_, func=AF.Copy), **kw)
        else:
            eng.op(lambda e: e.tensor_copy(out=out, in_=in_), **kw)

    def setup(self):
        nc, es = self.nc, self.es
        TT, NS, dp = self.TT, self.NS, self.depth
        self.ps = [Tile(es.enter_context(nc.psum_tensor(f"ps{i}", [128, 512], F32)), excl=True) for i in range(8)]
        self._ps_rr = 0
        self.x_all = self.dram_in("x_all", [TT, D])
        self.pvec = self.dram_in("pvec", [dp, 128, NV])
        self.w_in = self.dram_in("w_in", [dp, D, INC])
        self.w_out = self.dram_in("w_out", [dp, D, D])
        self.w_gate = self.dram_in("w_gate", [dp, D, DFF])
        self.w_up = self.dram_in("w_up", [dp, D, DFF])
        self.w_down = self.dram_in("w_down", [dp, DFF, D])
        self.gla_w2 = self.dram_in("gla_w2", [dp, 16, 256])
        self.biasT = self.dram_in("biasT", [dp, 128, 8 * 5 * 64])
        self.rb_rep = self.dram_in("rb_rep", [dp, 128, 8 * 257])
        self.st_short = self.dram_in("st_short", [dp, NS, 2, GW])
        self.st_gla = self.dram_in("st_gla", [dp, NS, 256, 128])
        self.st_cconv = self.dram_in("st_cconv", [dp, NS, 30, GW])
        self.st_k = self.dram_in("st_k", [dp, NS, 512, GW])
        self.st_v = self.dram_in("st_v", [dp, NS, 512, GW])
        self.st_ffn = self.dram_in("st_ffn", [dp, NS, 2, DFF])
        self.y_all = self.dram_out("y_all", [TT, D])
        self.o_p = {
            "short": self.dram_out("p_short", [dp, 2, GW]),
            "gla": self.dram_out("p_gla", [dp, 256, 128]),
            "cconv": self.dram_out("p_cconv", [dp, 30, GW]),
            "k": self.dram_out("p_k", [dp, 512, GW]),
            "v": self.dram_out("p_v", [dp, 512, GW]),
            "ffn": self.dram_out("p_ffn", [dp, 2, DFF]),
        }
        self.o_s = {
            "short": self.dram_out("s_short", [dp, NS, 2, GW]),
            "gla": self.dram_out("s_gla", [dp, NS, 256, 128]),
            "cconv": self.dram_out("s_cconv", [dp, NS, 30, GW]),
            "k": self.dram_out("s_k", [dp, NS, 16, GW]),
            "v": self.dram_out("s_v", [dp, NS, 16, GW]),
            "ffn": self.dram_out("s_ffn", [dp, NS, 2, DFF]),
        }
        self.xT = self.scratch("xT", [D, TT], F32)
        self.xTt = [Tile(self.xT.ap[k * 128:(k + 1) * 128, :]) for k in range(16)]
        self.hT = self.scratch("hT", [D, TT], BF16)
        self.zT = self.scratch("zT", [INC, TT], F32)
        self.vtok_a = self.scratch("vtok_a", [TT, GW], F32)
        self.vtok_g = self.scratch("vtok_g", [TT, GW], F32)
        self.ktok = self.scratch("ktok", [TT, GW], F32)
        self.yT = self.scratch("yT", [D, TT], BF16)
        self.aT = self.scratch("aT", [DFF, TT], BF16)
        self.ident = self.sb(es, "ident", [128, 128], F32)
        self.ones32 = self.sb(es, "ones32", [128, 128], F32)
        self.identb = self.sb(es, "identb", [128, 128], BF16)
        self.onesb = self.sb(es, "onesb", [128, 128], BF16)
        self.selA = self.sb(es, "selA", [128, 128], BF16)
        self.selB = self.sb(es, "selB", [128, 128], BF16)
        self.trimask = self.sb(es, "trimask", [64, 64], F32)
        self.pv = self.sb(es, "pv", [128, NV], F32, dma=True)
        P = self.pool
        self.epsc = self.sb(es, 'epsc', [128, 1], F32)
        P.op(lambda e: e.memset(self.epsc.ap[:, :], EPS), w=[self.epsc])
        self.onec = self.sb(es, 'onec', [128, 1], F32)
        P.op(lambda e: e.memset(self.onec.ap[:, :], 1.0), w=[self.onec])
        P.op(lambda e: e.memset(self.ident.ap[:, :], 0.0), w=[self.ident])
        P.op(lambda e: e.affine_select(out=self.ident.ap[:, :], in_=self.ident.ap[:, :], compare_op=ALU.not_equal,
                                       fill=1.0, base=0, pattern=[[-1, 128]], channel_multiplier=1),
             w=[self.ident])
        P.op(lambda e: e.memset(self.ones32.ap[:, :], 1.0), w=[self.ones32])
        P.op(lambda e: e.memset(self.onesb.ap[:, :], 1.0), w=[self.onesb])
        P.op(lambda e: e.tensor_copy(out=self.identb.ap[:, :], in_=self.ident.ap[:, :]), r=[self.ident], w=[self.identb])
        P.op(lambda e: e.memset(self.selA.ap[:, :], 0.0), w=[self.selA])
        P.op(lambda e: e.memset(self.selA.ap[0:64, :], 1.0), w=[self.selA])
        P.op(lambda e: e.memset(self.selB.ap[:, :], 0.0), w=[self.selB])
        P.op(lambda e: e.memset(self.selB.ap[64:128, :], 1.0), w=[self.selB])
        P.op(lambda e: e.memset(self.trimask.ap[:, :], 1.0), w=[self.trimask])
        P.op(lambda e: e.affine_select(out=self.trimask.ap[:, :], in_=self.trimask.ap[:, :], compare_op=ALU.is_ge,
                                       fill=0.0, base=0, pattern=[[1, 64]], channel_multiplier=-1),
             w=[self.trimask])

    def rstd(self, out, in_, inv_n, rtiles, wtile):
        self.act.op(lambda e: e.activation(out=out, in_=in_, func=AF.Ln, scale=inv_n, bias=self.epsc.ap[:out.shape[0], :]),
                    r=list(rtiles) + [self.epsc], w=[wtile])
        self.act.op(lambda e: e.activation(out=out, in_=out, func=AF.Exp, scale=-0.5), r=[wtile], w=[wtile])

    def psb(self):
        t = self.ps[self._ps_rr % 6]
        self._ps_rr += 1
        return t

    def load_pvec(self, l):
        self.sp.dma(out=self.pv.ap[:, :], in_=self.pvec[l], S=self.pv.sem, w=[self.pv])

    def phase_transpose_in(self):
        TT = self.TT
        xTv = self.xT.ap.rearrange("(k p) t -> p k t", p=128)
        with ExitStack() as es:
            xin = [self.sb(es, f"p0i{i}", [128, D], F32, dma=True) for i in range(2)]
            xo = [self.sb(es, f"p0o{i}", [128, 16, 128], F32, dma=True) for i in range(2)]
            for i in range((TT + 127) // 128):
                r0 = i * 128
                n = min(128, TT - r0)
                ti, to = xin[i % 2], xo[i % 2]
                self.sp.dma(out=ti.ap[:n, :], in_=self.x_all[r0:r0 + n, :], S=ti.sem, w=[ti])
                for j in range(4):
                    ps = self.psb()
                    for q in range(4):
                        f = j * 4 + q
                        self.pe.op(lambda e: e.transpose(out=ps.ap[:, q * 128:q * 128 + n],
                                                         in_=ti.ap[:n, f * 128:(f + 1) * 128],
                                                         identity=self.ident.ap[:n, :n]),
                                   r=[ti, self.ident], w=[ps])
                    src = ps.ap.rearrange("p (a b) -> p a b", b=128)[:, :, :n]
                    self.copy(self.evac_eng(), to.ap[:, j * 4:(j + 1) * 4, :n], src, r=[ps], wm=[to])
                self.sp.dma(out=xTv[:, :, r0:r0 + n], in_=to.ap[:, :, :n], S=to.sem, r=[to], wm=self.xTt)
            self.barrier()

    def phase_norm(self, gcol, final=False):
        TT = self.TT
        xTv = self.xT.ap.rearrange("(k p) t -> p k t", p=128)
        hTv = self.hT.ap.rearrange("(k p) t -> p k t", p=128)
        with ExitStack() as es:
            xb = [self.sb(es, f"n_x{i}", [128, 16, 512], F32, dma=True) for i in range(2)]
            sq = [self.sb(es, f"n_sq{i}", [128, 16, 512], F32) for i in range(1)]
            rs = [self.sb(es, f"n_rs{i}", [128, 512], F32) for i in range(2)]
            if final:
                ho = [self.sb(es, f"n_h{i}", [128, 16, 512], F32) for i in range(2)]
                yo = [self.sb(es, f"n_y{i}", [128, D], F32, dma=True) for i in range(2)]
            else:
                ho = [self.sb(es, f"n_h{i}", [128, 16, 512], BF16, dma=True) for i in range(2)]
            nyo = 0
            for bi, (c0, cw) in enumerate(split_even(TT, 512)):
                x, s, r, h = xb[bi % 2], sq[0], rs[bi % 2], ho[bi % 2]
                self.sp.dma(out=x.ap[:, :, :cw], in_=xTv[:, :, c0:c0 + cw], S=x.sem, r=self.xTt, w=[x])
                self.act.op(lambda e: e.activation(out=s.ap[:, :, :cw], in_=x.ap[:, :, :cw], func=AF.Square),
                            r=[x], w=[s])
                ps = self.psb()
                for k in range(16):
                    self.pe.op(lambda e: e.matmul(ps.ap[:, :cw], lhsT=self.ones32.ap[:, :], rhs=s.ap[:, k, :cw],
                                                  start=(k == 0), stop=(k == 15)),
                               r=[self.ones32, s], w=[ps])
                self.rstd(r.ap[:, :cw], ps.ap[:, :cw], 1.0 / D, [ps], r)
                for k in range(16):
                    eng = self.dve
                    eng.op(lambda e: e.scalar_tensor_tensor(out=h.ap[:, k, :cw], in0=x.ap[:, k, :cw],
                                                            scalar=self.pv.ap[:, gcol + k:gcol + k + 1],
                                                            in1=r.ap[:, :cw], op0=ALU.mult, op1=ALU.mult),
                           r=[x, r, self.pv], wm=[h])
                if not final:
                    self.sp.dma(out=hTv[:, :, c0:c0 + cw], in_=h.ap[:, :, :cw], S=h.sem, r=[h], wm=[self.hT])
                else:
                    for t0 in range(0, cw, 128):
                        n = min(128, cw - t0)
                        y = yo[nyo % 2]
                        nyo += 1
                        for j in range(4):
                            ps2 = self.psb()
                            for q in range(4):
                                f = j * 4 + q
                                self.pe.op(lambda e: e.transpose(out=ps2.ap[:n, q * 128:(q + 1) * 128],
                                                                 in_=h.ap[:, f, t0:t0 + n],
                                                                 identity=self.ident.ap[:, :]),
                                           r=[h, self.ident], w=[ps2])
                            self.copy(self.evac_eng(), y.ap[:n, j * 512:(j + 1) * 512], ps2.ap[:n, :],
                                      r=[ps2], wm=[y])
                        self.sp.dma(out=self.y_all[c0 + t0:c0 + t0 + n, :], in_=y.ap[:n, :], S=y.sem, r=[y])
            self.barrier()

    def seq_ranges(self):
        return [(0, self.TP)] + [(self.TP + 16 * s, 16) for s in range(self.NS)]

    def mm_f(self, wt, wc0, M, xin, n0, nw, KT):
        ps = self.psb()
        for k in range(KT):
            self.pe.op(lambda e: e.matmul(ps.ap[:M, :nw], lhsT=wt.ap[:, k, wc0:wc0 + M], rhs=xin.ap[:, k, n0:n0 + nw],
                                          start=(k == 0), stop=(k == KT - 1)), r=[wt, xin], w=[ps])
        return ps

    def mm_t(self, wt, wc0, w, xin, t0, tn, KT):
        ps = self.psb()
        for k in range(KT):
            self.pe.op(lambda e: e.matmul(ps.ap[:tn, :w], lhsT=xin.ap[:, k, t0:t0 + tn], rhs=wt.ap[:, k, wc0:wc0 + w],
                                          start=(k == 0), stop=(k == KT - 1)), r=[wt, xin], w=[ps])
        return ps

    def linear(self, es, src, KT, blocks, CW, SLOTW, handler, dbl=False):
        TT = self.TT
        srcv = src.ap.rearrange("(k p) t -> p k t", p=128)
        nsb = (TT + CW - 1) // CW
        xins = [self.sb(es, f"lin_x{i}", [128, KT, CW], BF16, dma=True) for i in range(2 if (nsb > 1 and dbl) else 1)]
        wts = [self.sb(es, f"lin_w{i}", [128, KT, SLOTW], BF16, dma=f"sw{i}") for i in range(2)]
        wi = 0

        def load_x(sbi):
            s0 = sbi * CW
            sw = min(CW, TT - s0)
            xin = xins[sbi % len(xins)]
            self.sp.dma(out=xin.ap[:, :, :sw], in_=srcv[:, :, s0:s0 + sw], S=xin.sem, r=[src], w=[xin])

        def load_w(parts, wt):
            off = 0
            for (Wap, c0, w) in parts:
                Wv = Wap.rearrange("(k p) n -> p k n", p=128)
                self.pool.dma(out=wt.ap[:, :, off:off + w], in_=Wv[:, :, c0:c0 + w], S=wt.sem, wm=[wt])
                off += w

        if len(xins) > 1:
            load_x(0)
        for sbi in range(nsb):
            s0 = sbi * CW
            sw = min(CW, TT - s0)
            xin = xins[sbi % len(xins)]
            if len(xins) == 1:
                load_x(sbi)
            elif sbi + 1 < nsb:
                load_x(sbi + 1)
            load_w(blocks[0][0], wts[wi % 2])
            for bi, (parts, tag) in enumerate(blocks):
                wt = wts[wi % 2]
                wi += 1
                if bi + 1 < len(blocks):
                    load_w(blocks[bi + 1][0], wts[wi % 2])
                handler(tag, parts, wt, xin, s0, sw)

    def phase_inproj(self, l):
        W = self.w_in[l]
        TP, TT = self.TP, self.TT
        blocks = []
        for (c0, wd, tag) in [(O_XA, 512, "f"), (O_BG, 512, "f"), (O_CG, 512, "f"), (O_Q, 256, "f"), (O_K, 256, "f"),
                              (O_V, 512, "tg"), (O_G, 512, "f"), (O_R, 16, "f"), (O_CV, 512, "f"), (O_CGT, 512, "f"),
                              (O_DQ, 512, "f"), (O_DK, 512, "fk"), (O_DV, 512, "ta")]:
            blocks.append(([(W, c0, wd)], tag))
        with ExitStack() as es:
            stf = [self.sb(es, f"z_sf{i}", [128, 2080], F32, dma=True) for i in range(2)]
            stt = [self.sb(es, f"z_st{i}", [128, 512], F32, dma=True) for i in range(3)]
            cnt = {"f": 0, "t": 0}

            def handler(tag, parts, wt, xin, s0, sw):
                (_, c0, wd) = parts[0]
                if tag[0] == "f":
                    for m0 in range(0, wd, 128):
                        M = min(128, wd - m0)
                        st = stf[cnt["f"] % 2]
                        cnt["f"] += 1
                        for (n0, nw) in split_even(sw, 512):
                            ps = self.mm_f(wt, m0, M, xin, n0, nw, 16)
                            self.copy(self.evac_eng(), st.ap[:M, n0:n0 + nw], ps.ap[:M, :nw], r=[ps], wm=[st])
                        self.sp.dma(out=self.zT.ap[c0 + m0:c0 + m0 + M, s0:s0 + sw], in_=st.ap[:M, :sw], S=st.sem,
                                    r=[st], wm=[self.zT])
                if tag[0] == "t" or tag == "fk":
                    dst = {"tg": self.vtok_g, "ta": self.vtok_a, "fk": self.ktok}[tag]
                    for t0 in range(0, sw, 128):
                        tn = min(128, sw - t0)
                        if tag == "fk" and (s0 + t0 + tn <= TP - 512):
                            continue
                        st = stt[cnt["t"] % 3]
                        cnt["t"] += 1
                        ps = self.mm_t(wt, 0, wd, xin, t0, tn, 16)
                        self.copy(self.evac_eng(), st.ap[:tn, :wd], ps.ap[:tn, :wd], r=[ps], w=[st])
                        self.sp.dma(out=dst.ap[s0 + t0:s0 + t0 + tn, :], in_=st.ap[:tn, :wd], S=st.sem,
                                    r=[st], wm=[dst])

            self.linear(es, self.hT, 16, blocks, 2080, 512, handler, dbl=True)
            self.barrier()

    def tok2feat(self, es, name, src_ap, R, C):
        nt = C // 128
        stg = self.sb(es, name + "_s", [R, C], F32, dma=True)
        dst = self.sb(es, name + "_d", [128, nt, R], F32)
        self.sp.dma(out=stg.ap[:, :], in_=src_ap, S=stg.sem, w=[stg])
        per = max(1, 512 // R)
        for g0 in range(0, nt, per):
            g = min(per, nt - g0)
            ps = self.psb()
            for q in range(g):
                self.pe.op(lambda e: e.transpose(out=ps.ap[:, q * R:(q + 1) * R],
                                                 in_=stg.ap[:R, (g0 + q) * 128:(g0 + q + 1) * 128],
                                                 identity=self.ident.ap[:R, :R]), r=[stg, self.ident], w=[ps])
            self.copy(self.evac_eng(), dst.ap[:, g0:g0 + g, :],
                      ps.ap[:, :g * R].rearrange("p (a b) -> p a b", b=R), r=[ps], wm=[dst])
        return dst

    def feat2tok(self, es, name, src, R, C, dst_ap):
        nt = C // 128
        stg = self.sb(es, name + "_o", [R, C], F32, dma=True)
        for g0 in range(0, nt, 4):
            g = min(4, nt - g0)
            ps = self.psb()
            for q in range(g):
                self.pe.op(lambda e: e.transpose(out=ps.ap[:R, q * 128:(q + 1) * 128], in_=src.ap[:, g0 + q, :],
                                                 identity=self.ident.ap[:, :]), r=[src, self.ident], w=[ps])
            self.copy(self.evac_eng(), stg.ap[:R, g0 * 128:(g0 + g) * 128], ps.ap[:R, :g * 128], r=[ps], wm=[stg])
        self.sp.dma(out=dst_ap, in_=stg.ap[:R, :], S=stg.sem, r=[stg])

    def mix_short(self, l):
        TP, NS, TT = self.TP, self.NS, self.TT
        zv = self.zT.ap
        pv = self.pv
        import os
        for (is_s, nseq, T, col0) in [(False, 1, TP, 0), (True, NS, 16, TP)]:
            if is_s and os.environ.get('DBG_NOSAMPLE'):
                continue
            with ExitStack() as es:
                BW = min(T, 1024)
                nblk = (T + BW - 1) // BW
                W = nseq * BW
                if is_s:
                    hist = self.tok2feat(es, "a_h", self.st_short[l].rearrange("s r c -> (s r) c"), 2 * NS, GW)
                stout = self.sb(es, "a_so", [128, 4, nseq * 2], F32)
                xa = [self.sb(es, f"a_xa{i}", [128, W], F32, dma=True) for i in range(2)]
                bg = [self.sb(es, f"a_bg{i}", [128, W], F32, dma=True) for i in range(2)]
                cg = [self.sb(es, f"a_cg{i}", [128, W], F32, dma=True) for i in range(2)]
                ue = [self.sb(es, f"a_ue{i}", [128, nseq, BW + 2], F32) for i in range(2)]
                cc = [self.sb(es, f"a_cc{i}", [128, nseq, BW], F32) for i in range(2)]
                yo = [self.sb(es, f"a_yo{i}", [128, nseq, BW], BF16, dma=True) for i in range(2)]
                it = 0
                for i in range(4):
                    for b in range(nblk):
                        c0 = col0 + b * BW
                        cw = min(BW, T - b * BW) if not is_s else BW
                        n = cw * nseq
                        X, Bg, Cg, U, C_, Y = (t[it % 2] for t in (xa, bg, cg, ue, cc, yo))
                        Uprev = ue[(it + 1) % 2]
                        it += 1
                        self.sp.dma(out=X.ap[:, :n], in_=zv[O_XA + i * 128:O_XA + (i + 1) * 128, c0:c0 + n], S=X.sem,
                                    r=[self.zT], w=[X])
                        self.sp.dma(out=Bg.ap[:, :n], in_=zv[O_BG + i * 128:O_BG + (i + 1) * 128, c0:c0 + n], S=Bg.sem,
                                    r=[self.zT], w=[Bg])
                        self.sp.dma(out=Cg.ap[:, :n], in_=zv[O_CG + i * 128:O_CG + (i + 1) * 128, c0:c0 + n], S=Cg.sem,
                                    r=[self.zT], w=[Cg])
                        v3 = lambda t: t.ap[:, :n].rearrange("p (s c) -> p s c", s=nseq)
                        if is_s:
                            self.dve.op(lambda e: e.tensor_copy(out=U.ap[:, :, 0:2],
                                                                in_=hist.ap[:, i, :].rearrange("p (s r) -> p s r", r=2)),
                                        r=[hist], w=[U])
                        elif b == 0:
                            self.dve.op(lambda e: e.memset(U.ap[:, :, 0:2], 0.0), w=[U])
                        else:
                            self.dve.op(lambda e: e.tensor_copy(out=U.ap[:, :, 0:2], in_=Uprev.ap[:, :, BW:BW + 2]),
                                        r=[Uprev], w=[U])
                        self.dve.op(lambda e: e.tensor_tensor(out=U.ap[:, :, 2:2 + cw], in0=v3(Cg), in1=v3(X), op=ALU.mult),
                                    r=[Cg, X], wm=[U])
                        wc = PV_CAW + i * 3
                        self.dve.op(lambda e: e.tensor_scalar(out=C_.ap[:, :, :cw], in0=U.ap[:, :, 0:cw],
                                                              scalar1=pv.ap[:, wc:wc + 1], scalar2=None, op0=ALU.mult),
                                    r=[U, pv], w=[C_])
                        for j in (1, 2):
                            self.dve.op(lambda e: e.scalar_tensor_tensor(out=C_.ap[:, :, :cw], in0=U.ap[:, :, j:j + cw],
                                                                         scalar=pv.ap[:, wc + j:wc + j + 1],
                                                                         in1=C_.ap[:, :, :cw], op0=ALU.mult, op1=ALU.add),
                                        r=[U, pv, C_], w=[C_])
                        self.dve.op(lambda e: e.tensor_tensor(out=Y.ap[:, :, :cw], in0=C_.ap[:, :, :cw], in1=v3(Bg),
                                                              op=ALU.mult), r=[C_, Bg], w=[Y])
                        if nseq == 1:
                            self.sp.dma(out=self.yT.ap[i * 128:(i + 1) * 128, c0:c0 + cw], in_=Y.ap[:, 0, :cw], S=Y.sem,
                                        r=[Y], wm=[self.yT])
                        else:
                            self.sp.dma(out=self.yT.ap[i * 128:(i + 1) * 128, c0:c0 + n].rearrange("p (s c) -> p s c", s=nseq),
                                        in_=Y.ap[:, :, :cw], S=Y.sem, r=[Y], wm=[self.yT])
                        if b == nblk - 1:
                            self.dve.op(lambda e: e.tensor_copy(out=stout.ap[:, i, :].rearrange("p (s r) -> p s r", r=2),
                                                                in_=U.ap[:, :, cw:cw + 2]), r=[U], wm=[stout])
                dst = (self.o_s["short"][l].rearrange("s r c -> (s r) c") if is_s else self.o_p["short"][l])
                if not (os.environ.get('DBG_NOF2T') == '1' or (os.environ.get('DBG_NOF2T') == 'p' and not is_s)):
                    self.feat2tok(es, "a_fo", stout, 2 * nseq, GW, dst)
                self.barrier()

    def build_diag(self, es):
        dg = self.sb(es, "c_diag", [128, 4, 31, 128], BF16)
        for i in range(4):
            for j in range(31):
                c = PV_CCW + i * 31 + j
                self.pool.op(lambda e: e.tensor_scalar(out=dg.ap[:, i, j, :], in0=self.identb.ap[:, :],
                                                       scalar1=self.pv.ap[:, c:c + 1], scalar2=None, op0=ALU.mult),
                             r=[self.identb, self.pv], wm=[dg])
        return dg

    def mix_cconv(self, l):
        TP, NS, TT = self.TP, self.NS, self.TT
        pv = self.pv
        zcv = self.zT.ap[O_CV:O_CV + 512, :].rearrange("(i p) t -> p i t", p=128)
        zcg = self.zT.ap[O_CGT:O_CGT + 512, :].rearrange("(i p) t -> p i t", p=128)
        yv = self.yT.ap[1024:1536, :].rearrange("(i p) t -> p i t", p=128)
        with ExitStack() as es0:
            dg = self.build_diag(es0)
            import os
            clv = int(os.environ.get('DBG_CC', '9'))
            if clv < 2:
                self.barrier()
                return
            for (is_s, nseq, T, col0) in [(False, 1, TP, 0), (True, NS, 16, TP)]:
                with ExitStack() as es:
                    BW = min(T, 512 // nseq)
                    nblk = (T + BW - 1) // BW
                    W = nseq * BW
                    if is_s:
                        hist = self.tok2feat(es, "c_h", self.st_cconv[l].rearrange("s r c -> (s r) c"), 30 * NS, GW)
                    stout = self.sb(es, "c_so", [128, 4, nseq * 30], F32)
                    cv = [self.sb(es, f"c_cv{i}", [128, 4, W], F32, dma=True) for i in range(2)]
                    cg = [self.sb(es, f"c_cg{i}", [128, 4, W], F32, dma=True) for i in range(2)]
                    ue = [self.sb(es, f"c_ue{i}", [128, 4, nseq, BW + 30], BF16) for i in range(2)]
                    uf = [self.sb(es, f"c_uf{i}", [128, 4, nseq, BW + 30], F32) for i in range(2)]
                    cc = [self.sb(es, f"c_cc{i}", [128, 4, W], F32) for i in range(2)]
                    sq = [self.sb(es, f"c_sq{i}", [128, 4, W], F32) for i in range(2)]
                    mean = self.sb(es, "c_mean", [128, W], F32)
                    msq = self.sb(es, "c_msq", [128, W], F32)
                    rs = self.sb(es, "c_rs", [128, W], F32)
                    tt = [self.sb(es, f"c_t{i}", [128, W], F32) for i in range(2)]
                    yo = [self.sb(es, f"c_yo{i}", [128, 4, W], BF16, dma=True) for i in range(2)]
                    def blk_ctx(b):
                        c0 = col0 + b * BW
                        cw = min(BW, T - b * BW)
                        n = cw * nseq
                        return (c0, cw, n) + tuple(t[b % 2] for t in (cv, cg, ue, uf, cc, sq, yo)) + (ue[(b + 1) % 2], uf[(b + 1) % 2])

                    def front(b):
                        (c0, cw, n, CV, CG, UE, UF, CC, SQ, Y, UEp, UFp) = blk_ctx(b)
                        self.sp.dma(out=CV.ap[:, :, :n], in_=zcv[:, :, c0:c0 + n], S=CV.sem, r=[self.zT], w=[CV])
                        self.sp.dma(out=CG.ap[:, :, :n], in_=zcg[:, :, c0:c0 + n], S=CG.sem, r=[self.zT], w=[CG])
                        self.act.op(lambda e: e.activation(out=CG.ap[:, :, :n], in_=CG.ap[:, :, :n], func=AF.Sigmoid),
                                    r=[CG], w=[CG])
                        v4 = lambda t: t.ap[:, :, :n].rearrange("p i (s c) -> p i s c", s=nseq)
                        if is_s:
                            self.dve.op(lambda e: e.tensor_copy(out=UF.ap[:, :, :, 0:30],
                                                                in_=hist.ap[:, :, :].rearrange("p i (s r) -> p i s r", r=30)),
                                        r=[hist], w=[UF])
                        elif b == 0:
                            self.dve.op(lambda e: e.memset(UF.ap[:, :, :, 0:30], 0.0), w=[UF])
                        else:
                            self.dve.op(lambda e: e.tensor_copy(out=UF.ap[:, :, :, 0:30], in_=UFp.ap[:, :, :, BW:BW + 30]),
                                        r=[UFp], w=[UF])
                        for i in range(4):
                            self.dve.op(lambda e: e.tensor_tensor(out=UF.ap[:, i, :, 30:30 + cw], in0=v4(CV)[:, i],
                                                                  in1=v4(CG)[:, i], op=ALU.mult), r=[CV, CG], wm=[UF])
                        self.act.op(lambda e: e.activation(out=UE.ap[:, :, :, :cw + 30], in_=UF.ap[:, :, :, :cw + 30],
                                                           func=AF.Copy), r=[UF], w=[UE])

                    def back(b):
                        (c0, cw, n, CV, CG, UE, UF, CC, SQ, Y, UEp, UFp) = blk_ctx(b)
                        v4 = lambda t: t.ap[:, :, :n].rearrange("p i (s c) -> p i s c", s=nseq)
                        pss = []
                        for i in range(4):
                            ps = self.psb()
                            pss.append(ps)
                            out = ps.ap[:, :n] if nseq == 1 else ps.ap[:, :n].rearrange("p (s c) -> p s c", s=nseq)
                            for j in range(31):
                                rhs = UE.ap[:, i, 0, j:j + cw] if nseq == 1 else UE.ap[:, i, :, j:j + cw]
                                self.pe.op(lambda e: e.matmul(out, lhsT=dg.ap[:, i, j, :], rhs=rhs,
                                                              start=(j == 0), stop=(j == 30)), r=[dg, UE], w=[ps])
                            bcol = pv.ap[:, PV_CCB + i:PV_CCB + i + 1]
                            self.act.op(lambda e: e.activation(out=CC.ap[:, i, :n], in_=ps.ap[:, :n], func=AF.Identity,
                                                               bias=bcol), r=[ps, pv], wm=[CC])
                            self.act.op(lambda e: e.activation(out=SQ.ap[:, i, :n], in_=ps.ap[:, :n], func=AF.Square,
                                                               bias=bcol), r=[ps, pv], wm=[SQ])
                        ps1, ps2 = self.psb(), self.psb()
                        for i in range(4):
                            self.pe.op(lambda e: e.matmul(ps1.ap[:, :n], lhsT=self.ones32.ap[:, :], rhs=CC.ap[:, i, :n],
                                                          start=(i == 0), stop=(i == 3)), r=[self.ones32, CC], w=[ps1])
                        for i in range(4):
                            self.pe.op(lambda e: e.matmul(ps2.ap[:, :n], lhsT=self.ones32.ap[:, :], rhs=SQ.ap[:, i, :n],
                                                          start=(i == 0), stop=(i == 3)), r=[self.ones32, SQ], w=[ps2])
                        self.dve.op(lambda e: e.tensor_scalar(out=mean.ap[:, :n], in0=ps1.ap[:, :n], scalar1=1.0 / 512,
                                                              scalar2=None, op0=ALU.mult), r=[ps1], w=[mean])
                        self.dve.op(lambda e: e.tensor_tensor(out=msq.ap[:, :n], in0=mean.ap[:, :n], in1=mean.ap[:, :n],
                                                              op=ALU.mult), r=[mean], w=[msq])
                        self.dve.op(lambda e: e.scalar_tensor_tensor(out=msq.ap[:, :n], in0=ps2.ap[:, :n], scalar=1.0 / 512,
                                                                     in1=msq.ap[:, :n], op0=ALU.mult, op1=ALU.subtract),
                                    r=[ps2, msq], w=[msq])
                        self.rstd(rs.ap[:, :n], msq.ap[:, :n], 1.0, [msq], rs)
                        for i in range(4):
                            T_ = tt[i % 2]
                            self.dve.op(lambda e: e.tensor_tensor(out=T_.ap[:, :n], in0=CC.ap[:, i, :n], in1=mean.ap[:, :n],
                                                                  op=ALU.subtract), r=[CC, mean], w=[T_])
                            self.dve.op(lambda e: e.tensor_tensor(out=T_.ap[:, :n], in0=T_.ap[:, :n], in1=rs.ap[:, :n],
                                                                  op=ALU.mult), r=[T_, rs], w=[T_])
                            self.act.op(lambda e: e.activation(out=Y.ap[:, i, :n], in_=T_.ap[:, :n], func=AF.Silu,
                                                               scale=pv.ap[:, PV_CLG + i:PV_CLG + i + 1],
                                                               bias=pv.ap[:, PV_CLB + i:PV_CLB + i + 1]),
                                        r=[T_, pv], wm=[Y])
                        self.sp.dma(out=yv[:, :, c0:c0 + n], in_=Y.ap[:, :, :n], S=Y.sem, r=[Y], wm=[self.yT])
                        if b == nblk - 1:
                            for i in range(4):
                                self.dve.op(lambda e: e.tensor_copy(out=stout.ap[:, i, :].rearrange("p (s r) -> p s r", r=30),
                                                                    in_=UF.ap[:, i, :, cw:cw + 30]), r=[UF], wm=[stout])

                    front(0)
                    for b in range(nblk):
                        if b + 1 < nblk:
                            front(b + 1)
                        back(b)
                    dst = (self.o_s["cconv"][l].rearrange("s r c -> (s r) c") if is_s else self.o_p["cconv"][l])
                    self.feat2tok(es, "c_fo", stout, 30 * nseq, GW, dst)
                    self.barrier()

    def mix_attn(self, l):
        TP, NS, TT = self.TP, self.NS, self.TT
        TBIDX = {128: 0, 0: 1, 192: 2, 64: 3}
        with ExitStack() as es0:
            tb = self.sb(es0, "d_tb", [128, 8, 5, 64], F32, dma=True)
            rbr = self.sb(es0, "d_rbr", [128, 8, 257], F32, dma=True)
            bmax = self.sb(es0, "d_bmax", [128, 8], F32)
            self.sp.dma(out=tb.ap[:, :, :, :], in_=self.biasT[l].rearrange("p (h t q) -> p h t q", h=8, t=5), S=tb.sem, w=[tb])
            self.sp.dma(out=rbr.ap[:, :, :], in_=self.rb_rep[l].rearrange("p (h r) -> p h r", h=8), S=rbr.sem, w=[rbr])
            self.dve.op(lambda e: e.scalar_tensor_tensor(out=rbr.ap[:, :, :], in0=rbr.ap[:, :, :], scalar=-1.0, in1=rbr.ap[:, :, :],
                                                         op0=ALU.mult, op1=ALU.max), r=[rbr], w=[rbr])
            self.dve.op(lambda e: e.tensor_reduce(out=bmax.ap[:, :], in_=rbr.ap[:, :, :], axis=AX.X, op=ALU.max),
                        r=[rbr], w=[bmax])
            if TP >= 512:
                self.sp.dma(out=self.o_p["k"][l], in_=self.ktok.ap[TP - 512:TP, :], S=self.scr_sem, r=[self.ktok])
                self.sp.dma(out=self.o_p["v"][l], in_=self.vtok_a.ap[TP - 512:TP, :], S=self.scr_sem, r=[self.vtok_a])
            self.sp.dma(out=self.o_s["k"][l].rearrange("s r c -> (s r) c"), in_=self.ktok.ap[TP:TT, :], S=self.scr_sem,
                        r=[self.ktok])
            self.sp.dma(out=self.o_s["v"][l].rearrange("s r c -> (s r) c"), in_=self.vtok_a.ap[TP:TT, :], S=self.scr_sem,
                        r=[self.vtok_a])
            seqs = [(False, 0, TP, 0, 64, 0)] + [(True, s, 16, 512, 16, TP + 16 * s) for s in range(NS)]
            import os
            self.alv = int(os.environ.get('DBG_AT', '9'))
            if self.alv < 1:
                self.barrier()
                return
            for (is_s, sidx, T, Lc, CL, col0) in seqs:
                self._attn_seq(l, tb, bmax, is_s, sidx, T, Lc, CL, col0, TBIDX)

    def _attn_seq(self, l, tb, bmax, is_s, sidx, T, Lc, CL, col0, TBIDX):
        import os
        NK = Lc + T
        NKT = (NK + 127) // 128
        zq = self.zT.ap[O_DQ:O_DQ + 512, :].rearrange("(i p) t -> p i t", p=128)
        zk = self.zT.ap[O_DK:O_DK + 512, :].rearrange("(i p) t -> p i t", p=128)
        yv = self.yT.ap[1536:2048, :].rearrange("(i p) t -> p i t", p=128)
        with ExitStack() as es:
            QT = self.sb(es, "d_qt", [128, 4, T], BF16)
            KT = self.sb(es, "d_kt", [128, 4, NK], BF16)
            VA = self.sb(es, "d_va", [128, NKT, 8, 65], BF16)
            ld = [self.sb(es, f"d_ld{i}", [128, 4, 512], F32, dma=True) for i in range(2)]
            sqb = [self.sb(es, f"d_sq{i}", [128, 4, 512], BF16) for i in range(2)]
            nb_k = (NK + 511) // 512 + 1
            nb_q = (T + 511) // 512
            kmx = self.sb(es, "d_kmx", [128, 8, nb_k], F32)
            qmx = self.sb(es, "d_qmx", [128, 8, nb_q], F32)
            k2 = self.sb(es, "d_k2", [128, 8], F32)
            q2 = self.sb(es, "d_q2", [128, 8], F32)
            negM = self.sb(es, "d_negm", [128, 8], F32)
            cM = self.sb(es, "d_cm", [128, 8], F32)
            nld = [0]

            def stat_block(L, ncols, mx, blk):
                SQ = sqb[nld[0] % 2]
                self.act.op(lambda e: e.activation(out=SQ.ap[:, :, :ncols], in_=L.ap[:, :, :ncols], func=AF.Square),
                            r=[L], w=[SQ])
                for pr in range(4):
                    for hb, sel in ((0, self.selA), (1, self.selB)):
                        ps = self.psb()
                        self.pe.op(lambda e: e.matmul(ps.ap[:, :ncols], lhsT=sel.ap[:, :], rhs=SQ.ap[:, pr, :ncols],
                                                      start=True, stop=True), r=[sel, SQ], w=[ps])
                        h = pr * 2 + hb
                        self.dve.op(lambda e: e.tensor_reduce(out=mx.ap[:, h, blk:blk + 1], in_=ps.ap[:, :ncols],
                                                              axis=AX.X, op=ALU.max), r=[ps], wm=[mx])

            kb = 0
            if is_s:
                with ExitStack() as es2:
                    kc = self.sb(es2, "d_kc", [128, 4, 512], F32, dma=True)
                    kf = self.sb(es2, "d_kf", [128, 4, 512], F32)
                    self.sp.dma(out=kc.ap[:, :, :], in_=self.st_k[l, sidx].rearrange("(t p) c -> p t c", p=128), S=kc.sem, w=[kc])
                    for pr in range(4):
                        ps = self.psb()
                        for t in range(4):
                            self.pe.op(lambda e: e.transpose(out=ps.ap[:, t * 128:(t + 1) * 128],
                                                             in_=kc.ap[:, t, pr * 128:(pr + 1) * 128],
                                                             identity=self.ident.ap[:, :]), r=[kc, self.ident], w=[ps])
                        self.copy(self.evac_eng(), kf.ap[:, pr, :], ps.ap[:, :], r=[ps], wm=[kf])
                    self.copy(self.dve, KT.ap[:, :, 0:512], kf.ap[:, :, :], r=[kf], wm=[KT])
                    nld[0] += 1
                    stat_block(kf, 512, kmx, kb)
                    kb += 1
                    self.barrier()
            for (c0, cw) in split_even(T, 512):
                L = ld[nld[0] % 2]
                nld[0] += 1
                self.sp.dma(out=L.ap[:, :, :cw], in_=zk[:, :, col0 + c0:col0 + c0 + cw], S=L.sem, r=[self.zT], w=[L])
                self.copy(self.dve, KT.ap[:, :, Lc + c0:Lc + c0 + cw], L.ap[:, :, :cw], r=[L], wm=[KT])
                stat_block(L, cw, kmx, kb)
                kb += 1
            qb_ = 0
            for (c0, cw) in split_even(T, 512):
                L = ld[nld[0] % 2]
                nld[0] += 1
                self.sp.dma(out=L.ap[:, :, :cw], in_=zq[:, :, col0 + c0:col0 + c0 + cw], S=L.sem, r=[self.zT], w=[L])
                self.act.op(lambda e: e.activation(out=L.ap[:, :, :cw], in_=L.ap[:, :, :cw], func=AF.Copy, scale=0.125),
                            r=[L], w=[L])
                self.copy(self.dve, QT.ap[:, :, c0:c0 + cw], L.ap[:, :, :cw], r=[L], wm=[QT])
                stat_block(L, cw, qmx, qb_)
                qb_ += 1
            self.dve.op(lambda e: e.tensor_reduce(out=k2.ap[:, :], in_=kmx.ap[:, :, :kb], axis=AX.X, op=ALU.max), r=[kmx], w=[k2])
            self.dve.op(lambda e: e.tensor_reduce(out=q2.ap[:, :], in_=qmx.ap[:, :, :qb_], axis=AX.X, op=ALU.max), r=[qmx], w=[q2])
            self.dve.op(lambda e: e.tensor_tensor(out=k2.ap[:, :], in0=k2.ap[:, :], in1=q2.ap[:, :], op=ALU.mult), r=[k2, q2], w=[k2])
            self.act.op(lambda e: e.activation(out=k2.ap[:, :], in_=k2.ap[:, :], func=AF.Sqrt), r=[k2], w=[k2])
            self.dve.op(lambda e: e.scalar_tensor_tensor(out=negM.ap[:, :], in0=k2.ap[:, :], scalar=-1.0, in1=bmax.ap[:, :],
                                                         op0=ALU.mult, op1=ALU.subtract), r=[k2, bmax], w=[negM])
            self.dve.op(lambda e: e.tensor_tensor(out=cM.ap[:, :], in0=tb.ap[:, :, 4, 0], in1=negM.ap[:, :], op=ALU.add),
                        r=[tb, negM], w=[cM])
            self.pool.op(lambda e: e.memset(VA.ap[:, :, :, 64:65], 1.0), wm=[VA])
            vsrcs = []
            if is_s:
                vsrcs.append((self.st_v[l, sidx], 512, 0, None))
            vsrcs.append((self.vtok_a.ap[col0:col0 + T, :], T, Lc // 128, self.vtok_a))
            vl = [self.sb(es, f"d_vl{i}", [128, 4, 512], F32, dma=True) for i in range(2)]
            nv = 0
            for (vap, rows, t0, dep) in vsrcs:
                for r0 in range(0, rows, 512):
                    rn = min(512, rows - r0)
                    V = vl[nv % 2]
                    nv += 1
                    rd = [dep] if dep is not None else []
                    if rn >= 128:
                        nt = rn // 128
                        self.sp.dma(out=V.ap[:, :nt, :], in_=vap[r0:r0 + rn, :].rearrange("(t p) c -> p t c", p=128),
                                    S=V.sem, r=rd, w=[V])
                        self.copy(self.act, VA.ap[:, t0 + r0 // 128:t0 + r0 // 128 + nt, :, 0:64],
                                  V.ap[:, :nt, :].rearrange("p t (h d) -> p t h d", h=8), r=[V], wm=[VA])
                    else:
                        self.sp.dma(out=V.ap[:rn, 0, :], in_=vap[r0:r0 + rn, :], S=V.sem, r=rd, w=[V])
                        self.copy(self.act, VA.ap[:rn, t0 + r0 // 128, :, 0:64],
                                  V.ap[:rn, 0, :].rearrange("p (h d) -> p h d", h=8), r=[V], wm=[VA])
            NB = 4
            ET = [self.sb(es, f"d_et{i}", [128, 5 * CL], BF16) for i in range(NB)]
            TM = [self.sb(es, f"d_tm{i}", [128, 2 * CL], F32) for i in range(NB)]
            rden = [self.sb(es, f"d_rd{i}", [128, 8], F32) for i in range(2)]
            otok = [self.sb(es, f"d_ot{i}", [128, 8, 64], F32) for i in range(2)]
            GB = 512 // CL if not is_s else 1
            yst = [self.sb(es, f"d_ys{i}", [128, 4, GB * CL], BF16, dma=True) for i in range(2)]
            pso = [self.ps[6], self.ps[7]]
            nchunk = T // CL

            def chunk_tiles(c):
                qs = Lc + c * CL
                kmin = max(0, qs - 512)
                kend = qs + CL
                tiles = []
                for kt in range(kmin // 128, (kend - 1) // 128 + 1):
                    p0 = max(kmin, kt * 128) - kt * 128
                    p1 = min(kend, kt * 128 + 128) - kt * 128
                    tiles.append((kt, p0, p1, qs - kt * 128))
                return tiles, sum(1 for t in tiles if t[3] >= 256)

            def qk(c, h):
                tiles, nconst = chunk_tiles(c)
                pr, hb = h // 2, (h % 2) * 64
                ps = self.psb()
                for si, (kt, p0, p1, d0) in enumerate(tiles):
                    mend = p1 if p0 == 0 else 128
                    self.pe.op(lambda e: e.matmul(ps.ap[:mend, si * CL:(si + 1) * CL],
                                                  lhsT=KT.ap[hb:hb + 64, pr, kt * 128:kt * 128 + mend],
                                                  rhs=QT.ap[hb:hb + 64, pr, c * CL:(c + 1) * CL],
                                                  start=True, stop=True), r=[KT, QT], w=[ps])
                return ps

            def softmax_pv(c, h, ps, E, Tm):
                tiles, nconst = chunk_tiles(c)
                if nconst:
                    self.act.op(lambda e: e.activation(out=E.ap[:, :nconst * CL], in_=ps.ap[:, :nconst * CL], func=AF.Exp,
                                                       bias=cM.ap[:, h:h + 1]), r=[ps, cM], w=[E])
                if tiles[0][1] == 64:
                    self.pool.op(lambda e: e.memset(E.ap[0:64, 0:CL], 0.0), w=[E])
                for si, (kt, p0, p1, d0) in enumerate(tiles):
                    if d0 >= 256:
                        continue
                    j = si - nconst
                    mend = p1 if p0 == 0 else 128
                    self.dve.op(lambda e: e.tensor_tensor(out=Tm.ap[:mend, j * CL:(j + 1) * CL], in0=ps.ap[:mend, si * CL:(si + 1) * CL],
                                                          in1=tb.ap[:mend, h, TBIDX[d0], 0:CL], op=ALU.add),
                                r=[ps, tb], wm=[Tm])
                    self.act.op(lambda e: e.activation(out=E.ap[:mend, si * CL:(si + 1) * CL], in_=Tm.ap[:mend, j * CL:(j + 1) * CL],
                                                       func=AF.Exp, bias=negM.ap[:mend, h:h + 1]), r=[Tm, negM], wm=[E])
                po = pso[h // 4]
                hh = h % 4
                for si, (kt, p0, p1, d0) in enumerate(tiles):
                    if p0 == 64:
                        p0 = 0
                    self.pe.op(lambda e: e.matmul(po.ap[:CL, hh * 65:(hh + 1) * 65], lhsT=E.ap[p0:p1, si * CL:(si + 1) * CL],
                                                  rhs=VA.ap[p0:p1, kt, h, :], start=(si == 0), stop=(si == len(tiles) - 1)),
                               r=[E, VA], w=[po])

            def finalize(c):
                RD, OT = rden[c % 2], otok[c % 2]
                for g in range(2):
                    pov = pso[g].ap[:CL, :260].rearrange("p (h d) -> p h d", d=65)
                    self.dve.op(lambda e: e.reciprocal(out=RD.ap[:CL, g * 4:(g + 1) * 4], in_=pov[:, :, 64]), r=[pso[g]], wm=[RD])
                    self.dve.op(lambda e: e.tensor_tensor(out=OT.ap[:CL, g * 4:(g + 1) * 4, :], in0=pov[:, :, 0:64],
                                                          in1=RD.ap[:CL, g * 4:(g + 1) * 4].unsqueeze(2).to_broadcast([CL, 4, 64]),
                                                          op=ALU.mult), r=[pso[g], RD], wm=[OT])
                Y = yst[(c // GB) % 2]
                pst = self.psb()
                otv = OT.ap[:CL, :, :].rearrange("p h d -> p (h d)")
                for i in range(4):
                    self.pe.op(lambda e: e.transpose(out=pst.ap[:, i * CL:(i + 1) * CL], in_=otv[:, i * 128:(i + 1) * 128],
                                                     identity=self.ident.ap[:CL, :CL]), r=[OT, self.ident], w=[pst])
                cc_ = (c % GB) * CL
                self.copy(self.act, Y.ap[:, :, cc_:cc_ + CL], pst.ap[:, :4 * CL].rearrange("p (i c) -> p i c", i=4),
                          r=[pst], wm=[Y])
                if (c % GB) == GB - 1 or c == nchunk - 1:
                    g0 = (c // GB) * GB * CL
                    gw = (c % GB + 1) * CL
                    self.sp.dma(out=yv[:, :, col0 + g0:col0 + g0 + gw], in_=Y.ap[:, :, :gw], S=Y.sem, r=[Y], wm=[self.yT])

            SKEW = 2
            items = [(c, h) for c in range(nchunk) for h in range(8)]
            pend = []
            for i, (c, h) in enumerate(items):
                pend.append((c, h, qk(c, h), ET[i % NB], TM[i % NB]))
                if len(pend) > SKEW:
                    it = pend.pop(0)
                    softmax_pv(*it)
                    if it[1] == 7:
                        finalize(it[0])
            while pend:
                it = pend.pop(0)
                softmax_pv(*it)
                if it[1] == 7:
                    finalize(it[0])
            self.barrier()

    def mix_gla(self, l):
        TP, NS = self.TP, self.NS
        with ExitStack() as es0:
            w2f = self.sb(es0, "b_w2f", [16, 256], F32, dma=True)
            w2 = self.sb(es0, "b_w2", [16, 256], BF16)
            negb = self.sb(es0, "b_negb", [64, 4], F32)
            self.sp.dma(out=w2f.ap[:, :], in_=self.gla_w2[l], S=w2f.sem, w=[w2f])
            self.copy(self.dve, w2.ap[:, :], w2f.ap[:, :], r=[w2f], w=[w2])
            self.dve.op(lambda e: e.tensor_scalar(out=negb.ap[:, :], in0=self.pv.ap[:64, PV_GLAB4:PV_GLAB4 + 4], scalar1=-1.0,
                                                  scalar2=None, op0=ALU.mult), r=[self.pv], w=[negb])
            seqs = [(False, 0, TP, 64, 0)] + [(True, s, 16, 16, TP + 16 * s) for s in range(NS)]
            for (is_s, sidx, T, L, col0) in seqs:
                self._gla_seq(l, w2, negb, is_s, sidx, T, L, col0)

    def _gla_seq(self, l, w2, negb, is_s, sidx, T, L, col0):
        pv = self.pv
        zT = self.zT.ap
        zq = zT[O_Q:O_Q + 256, :].rearrange("(h p) t -> p h t", p=64)
        zk = zT[O_K:O_K + 256, :].rearrange("(h p) t -> p h t", p=64)
        zg = zT[O_G:O_G + 512, :].rearrange("(i p) t -> p i t", p=128)
        yv = self.yT.ap[512:1024, :].rearrange("(i p) t -> p i t", p=128)
        BW = min(T, 512)
        CPB = BW // L
        bc = lambda ap, shape: ap.unsqueeze(2).to_broadcast(shape)
        with ExitStack() as es:
            S = self.sb(es, "b_S", [64, 4, 128], F32, dma=True)
            Sb = [self.sb(es, f"b_Sb{i}", [64, 4, 128], BF16) for i in range(2)]
            if is_s:
                self.sp.dma(out=S.ap[:, :, :], in_=self.st_gla[l, sidx].rearrange("(h p) v -> p h v", p=64), S=S.sem, w=[S])
            else:
                self.dve.op(lambda e: e.memset(S.ap[:, :, :], 0.0), w=[S])
            self.copy(self.act, Sb[0].ap[:, :, :], S.ap[:, :, :], r=[S], w=[Sb[0]])
            nS = 0
            rmask = self.sb(es, "b_rm", [64, BW], F32)
            self.dve.op(lambda e: e.memset(rmask.ap[:, :], 1.0), w=[rmask])
            self.dve.op(lambda e: e.memset(rmask.ap[:, :].rearrange("p (c t) -> p c t", t=L)[:, :, 0:1], 0.0), w=[rmask])
            rT = self.sb(es, "b_rT", [16, BW], F32, dma=True)
            rTb = self.sb(es, "b_rTb", [16, BW], BF16)
            qf = self.sb(es, "b_qf", [64, 4, BW], F32, dma=True)
            kf = self.sb(es, "b_kf", [64, 4, BW], F32, dma=True)
            gf = self.sb(es, "b_gf", [128, 4, BW], F32, dma=True)
            vf = self.sb(es, "b_vf", [64, CPB, 512], F32, dma=True)
            vb = self.sb(es, "b_vb", [64, CPB, 512], BF16)
            yy = self.sb(es, "b_y", [64, BW], F32)
            ay = self.sb(es, "b_ay", [64, BW], F32)
            la = self.sb(es, "b_la", [64, 4, BW], F32)
            bb = self.sb(es, "b_b", [64, 4, BW], F32)
            e3 = self.sb(es, "b_e3", [64, 4, BW], F32)
            ei = self.sb(es, "b_ei", [64, 4, BW], F32)
            qb = self.sb(es, "b_qb", [64, 4, BW], BF16)
            kinv = self.sb(es, "b_kinv", [64, 4, BW], F32)
            qd = [self.sb(es, f"b_qd{i}", [64, 4, L], BF16) for i in range(2)]
            kd = [self.sb(es, f"b_kd{i}", [64, 4, L], BF16) for i in range(2)]
            kl = [self.sb(es, f"b_kl{i}", [64, 4, L], F32) for i in range(2)]
            klt = [self.sb(es, f"b_klt{i}", [64, 256], BF16) for i in range(2)]
            am = [self.sb(es, f"b_am{i}", [64, 4, L], BF16) for i in range(2)]
            oT = self.sb(es, "b_oT", [128, 4, BW], F32)
            osq = self.sb(es, "b_osq", [128, 4, BW], F32)
            rs = self.sb(es, "b_rs", [128, BW], F32)
            ot = self.sb(es, "b_ot", [128, BW], F32)
            yo = [self.sb(es, f"b_yo{i}", [128, 4, BW], BF16, dma=True) for i in range(2)]
            psu = self.ps[6]
            for bi in range((T + BW - 1) // BW):
                c0 = col0 + bi * BW
                self.sp.dma(out=rT.ap[:, :], in_=zT[O_R:O_R + 16, c0:c0 + BW], S=rT.sem, r=[self.zT], w=[rT])
                self.sp.dma(out=qf.ap[:, :, :], in_=zq[:, :, c0:c0 + BW], S=qf.sem, r=[self.zT], w=[qf])
                self.sp.dma(out=kf.ap[:, :, :], in_=zk[:, :, c0:c0 + BW], S=kf.sem, r=[self.zT], w=[kf])
                self.sp.dma(out=gf.ap[:, :, :], in_=zg[:, :, c0:c0 + BW], S=gf.sem, r=[self.zT], w=[gf])
                self.sp.dma(out=vf.ap[:L, :, :], in_=self.vtok_g.ap[c0:c0 + BW, :].rearrange("(c p) v -> p c v", p=L),
                            S=vf.sem, r=[self.vtok_g], w=[vf])
                self.copy(self.act, vb.ap[:L, :, :], vf.ap[:L, :, :], r=[vf], w=[vb])
                self.copy(self.dve, rTb.ap[:, :], rT.ap[:, :], r=[rT], w=[rTb])
                for h in range(4):
                    ps = self.psb()
                    self.pe.op(lambda e: e.matmul(ps.ap[:64, :BW], lhsT=w2.ap[:, h * 64:(h + 1) * 64], rhs=rTb.ap[:, :],
                                                  start=True, stop=True), r=[w2, rTb], w=[ps])
                    self.dve.op(lambda e: e.tensor_scalar(out=yy.ap[:, :], in0=ps.ap[:64, :BW], scalar1=-1.0,
                                                          scalar2=negb.ap[:, h:h + 1], op0=ALU.mult, op1=ALU.add),
                                r=[ps, negb], w=[yy])
                    self.dve.op(lambda e: e.scalar_tensor_tensor(out=ay.ap[:, :], in0=yy.ap[:, :], scalar=-1.0, in1=yy.ap[:, :],
                                                                 op0=ALU.mult, op1=ALU.max), r=[yy], w=[ay])
                    self.act.op(lambda e: e.activation(out=ay.ap[:, :], in_=ay.ap[:, :], func=AF.Exp, scale=-1.0), r=[ay], w=[ay])
                    self.act.op(lambda e: e.activation(out=ay.ap[:, :], in_=ay.ap[:, :], func=AF.Ln, bias=self.onec.ap[:64, :]),
                                r=[ay, self.onec], w=[ay])
                    self.dve.op(lambda e: e.scalar_tensor_tensor(out=yy.ap[:, :], in0=yy.ap[:, :], scalar=0.0, in1=ay.ap[:, :],
                                                                 op0=ALU.max, op1=ALU.add), r=[yy, ay], w=[yy])
                    self.dve.op(lambda e: e.tensor_scalar(out=la.ap[:, h, :], in0=yy.ap[:, :], scalar1=-1.0 / 16.0, scalar2=None,
                                                          op0=ALU.mult), r=[yy], wm=[la])
                    self.dve.op(lambda e: e.tensor_tensor_scan(out=bb.ap[:, h, :], data0=rmask.ap[:, :], data1=la.ap[:, h, :],
                                                               initial=0.0, op0=ALU.mult, op1=ALU.add), r=[rmask, la], wm=[bb])
                self.act.op(lambda e: e.activation(out=e3.ap[:, :, :], in_=bb.ap[:, :, :], func=AF.Exp), r=[bb], w=[e3])
                self.act.op(lambda e: e.activation(out=ei.ap[:, :, :], in_=bb.ap[:, :, :], func=AF.Exp, scale=-1.0), r=[bb], w=[ei])
                self.dve.op(lambda e: e.scalar_tensor_tensor(out=qf.ap[:, :, :], in0=qf.ap[:, :, :], scalar=0.125, in1=e3.ap[:, :, :],
                                                             op0=ALU.mult, op1=ALU.mult), r=[qf, e3], w=[qf])
                self.copy(self.act, qb.ap[:, :, :], qf.ap[:, :, :], r=[qf], w=[qb])
                self.dve.op(lambda e: e.tensor_tensor(out=kinv.ap[:, :, :], in0=kf.ap[:, :, :], in1=ei.ap[:, :, :], op=ALU.mult),
                            r=[kf, ei], w=[kinv])
                def stage_a(c):
                    t0, t1, mid = c * L, (c + 1) * L, c * L + L // 2
                    QD, KD, KL, KLT, AM = (x[c % 2] for x in (qd, kd, kl, klt, am))
                    self.dve.op(lambda e: e.tensor_tensor(out=QD.ap[:, :, :], in0=qf.ap[:, :, t0:t1], in1=bc(ei.ap[:, :, mid], [64, 4, L]),
                                                          op=ALU.mult), r=[qf, ei], w=[QD])
                    self.dve.op(lambda e: e.tensor_tensor(out=KD.ap[:, :, :], in0=kinv.ap[:, :, t0:t1], in1=bc(e3.ap[:, :, mid], [64, 4, L]),
                                                          op=ALU.mult), r=[kinv, e3], w=[KD])
                    self.dve.op(lambda e: e.tensor_tensor(out=KL.ap[:, :, :], in0=kinv.ap[:, :, t0:t1], in1=bc(e3.ap[:, :, t1 - 1], [64, 4, L]),
                                                          op=ALU.mult), r=[kinv, e3], w=[KL])
                    pst = self.psb()
                    for h in range(4):
                        self.pe.op(lambda e: e.transpose(out=pst.ap[:L, h * 64:(h + 1) * 64], in_=KL.ap[:, h, :],
                                                         identity=self.ident.ap[:64, :64]), r=[KL, self.ident], w=[pst])
                    self.copy(self.act, KLT.ap[:L, :], pst.ap[:L, :256], r=[pst], w=[KLT])
                    psa = self.psb()
                    for h in range(4):
                        self.pe.op(lambda e: e.matmul(psa.ap[:L, h * L:(h + 1) * L], lhsT=KD.ap[:, h, :], rhs=QD.ap[:, h, :],
                                                      start=True, stop=True), r=[KD, QD], w=[psa])
                    self.dve.op(lambda e: e.tensor_tensor(out=AM.ap[:L, :, :], in0=psa.ap[:L, :4 * L].rearrange("p (h t) -> p h t", h=4),
                                                          in1=self.trimask.ap[:L, :L].unsqueeze(1).to_broadcast([L, 4, L]), op=ALU.mult),
                                r=[psa, self.trimask], w=[AM])

                def stage_b(c, nS):
                    t0, t1 = c * L, (c + 1) * L
                    KLT, AM = klt[c % 2], am[c % 2]
                    pso = self.psb()
                    SB = Sb[nS % 2]
                    for h in range(4):
                        self.pe.op(lambda e: e.matmul(pso.ap[:, h * L:(h + 1) * L], lhsT=vb.ap[:L, c, h * 128:(h + 1) * 128], rhs=AM.ap[:L, h, :],
                                                      start=True, stop=False), r=[vb, AM], w=[pso])
                        self.pe.op(lambda e: e.matmul(pso.ap[:, h * L:(h + 1) * L], lhsT=SB.ap[:, h, :], rhs=qb.ap[:, h, t0:t1],
                                                      start=False, stop=True), r=[SB, qb], w=[pso])
                    self.copy(self.act, oT.ap[:, :, t0:t1], pso.ap[:, :4 * L].rearrange("p (h t) -> p h t", h=4), r=[pso], wm=[oT])
                    for h in range(4):
                        self.pe.op(lambda e: e.matmul(psu.ap[:64, h * 128:(h + 1) * 128], lhsT=KLT.ap[:L, h * 64:(h + 1) * 64],
                                                      rhs=vb.ap[:L, c, h * 128:(h + 1) * 128], start=True, stop=True), r=[KLT, vb], w=[psu])
                    self.dve.op(lambda e: e.tensor_tensor(out=S.ap[:, :, :], in0=S.ap[:, :, :], in1=bc(e3.ap[:, :, t1 - 1], [64, 4, 128]),
                                                          op=ALU.mult), r=[S, e3], w=[S])
                    self.dve.op(lambda e: e.tensor_tensor(out=S.ap[:, :, :], in0=S.ap[:, :, :],
                                                          in1=psu.ap[:64, :].rearrange("p (h v) -> p h v", h=4), op=ALU.add),
                                r=[S, psu], w=[S])
                    self.copy(self.act, Sb[(nS + 1) % 2].ap[:, :, :], S.ap[:, :, :], r=[S], w=[Sb[(nS + 1) % 2]])

                stage_a(0)
                for c in range(CPB):
                    if c + 1 < CPB:
                        stage_a(c + 1)
                    stage_b(c, nS)
                    nS += 1
                self.act.op(lambda e: e.activation(out=osq.ap[:, :, :], in_=oT.ap[:, :, :], func=AF.Square), r=[oT], w=[osq])
                Y = yo[bi % 2]
                for h in range(4):
                    ps = self.psb()
                    self.pe.op(lambda e: e.matmul(ps.ap[:, :BW], lhsT=self.ones32.ap[:, :], rhs=osq.ap[:, h, :], start=True, stop=True),
                               r=[self.ones32, osq], w=[ps])
                    self.rstd(rs.ap[:, :], ps.ap[:, :BW], 1.0 / 128, [ps], rs)
                    self.dve.op(lambda e: e.tensor_tensor(out=ot.ap[:, :], in0=oT.ap[:, h, :], in1=rs.ap[:, :], op=ALU.mult),
                                r=[oT, rs], w=[ot])
                    self.act.op(lambda e: e.activation(out=gf.ap[:, h, :], in_=gf.ap[:, h, :], func=AF.Silu), r=[gf], w=[gf])
                    self.dve.op(lambda e: e.scalar_tensor_tensor(out=Y.ap[:, h, :], in0=ot.ap[:, :], scalar=pv.ap[:, PV_GLAG:PV_GLAG + 1],
                                                                 in1=gf.ap[:, h, :], op0=ALU.mult, op1=ALU.mult), r=[ot, pv, gf], wm=[Y])
                self.sp.dma(out=yv[:, :, c0:c0 + BW], in_=Y.ap[:, :, :], S=Y.sem, r=[Y], wm=[self.yT])
            dst = self.o_s["gla"][l, sidx] if is_s else self.o_p["gla"][l]
            self.sp.dma(out=dst.rearrange("(h p) v -> p h v", p=64), in_=S.ap[:, :, :], S=S.sem, r=[S])
            self.barrier()

    def phase_resproj(self, src, KT, W, CW, BWID, dbl=False):
        blocks = [([(W, c0, BWID)], "f") for c0 in range(0, D, BWID)]
        with ExitStack() as es:
            xr = [self.sb(es, f"r_x{i}", [128, CW], F32, dma=True) for i in range(2)]
            cnt = [0]

            def handler(tag, parts, wt, xin, s0, sw):
                (_, c0, wd) = parts[0]
                for m0 in range(0, wd, 128):
                    kt = (c0 + m0) // 128
                    X = xr[cnt[0] % 2]
                    cnt[0] += 1
                    xt = self.xTt[kt]
                    self.sp.dma(out=X.ap[:, :sw], in_=self.xT.ap[kt * 128:(kt + 1) * 128, s0:s0 + sw], S=X.sem, r=[xt], w=[X])
                    for (n0, nw) in split_even(sw, 512):
                        ps = self.mm_f(wt, m0, 128, xin, n0, nw, KT)
                        self.dve.op(lambda e: e.tensor_tensor(out=X.ap[:, n0:n0 + nw], in0=ps.ap[:, :nw], in1=X.ap[:, n0:n0 + nw],
                                                              op=ALU.add), r=[ps, X], w=[X])
                    self.sp.dma(out=self.xT.ap[kt * 128:(kt + 1) * 128, s0:s0 + sw], in_=X.ap[:, :sw], S=X.sem, r=[X], wm=[xt])

            self.linear(es, src, KT, blocks, CW, BWID, handler, dbl=dbl)
            self.barrier()

    def phase_ffn_hidden(self, l):
        TP, NS, TT = self.TP, self.NS, self.TT
        pv = self.pv
        CW = 2080
        Wg, Wu = self.w_gate[l], self.w_up[l]
        blocks = [([(Wg, c0, 256), (Wu, c0, 256)], "g") for c0 in range(0, DFF, 256)]
        seqs = self.seq_ranges()
        with ExitStack() as es:
            ghp = self.sb(es, "f_ghp", [128, 44, 2], F32)
            self.dve.op(lambda e: e.memset(ghp.ap[:, :, :], 0.0), w=[ghp])
            ghs = self.sb(es, "f_ghs", [128, 44, 2 * NS], F32)
            with ExitStack() as es2:
                t = self.tok2feat(es2, "f_hi", self.st_ffn[l].rearrange("s r c -> (s r) c"), 2 * NS, DFF)
                self.copy(self.dve, ghs.ap[:, :, :], t.ap[:, :, :], r=[t], w=[ghs])
                self.barrier()
            with ExitStack() as es3:
                WX = CW + 2 * (1 + NS)
                ge = [self.sb(es3, f"f_ge{i}", [128, WX], F32) for i in range(2)]
                ub = [self.sb(es3, f"f_ub{i}", [128, WX], F32) for i in range(2)]
                cb = [self.sb(es3, f"f_cb{i}", [128, WX], F32) for i in range(2)]
                ab = [self.sb(es3, f"f_ab{i}", [128, WX], BF16, dma=True) for i in range(2)]
                for u_ in ub:
                    self.pool.op(lambda e: e.memset(u_.ap[:, :], 0.0), w=[u_])
                cnt = [0]

                def hist_ap(j, q):
                    return ghp.ap[:, j, :] if q == 0 else ghs.ap[:, j, 2 * (q - 1):2 * q]

                import os
                flv = int(os.environ.get('DBG_FF', '9'))

                def handler(tag, parts, wt, xin, s0, sw):
                    c0 = parts[0][1]
                    if flv < 2:
                        return
                    segs = []
                    o = 0
                    for q, (a0, ln) in enumerate(seqs):
                        lo, hi = max(a0, s0), min(a0 + ln, s0 + sw)
                        if lo < hi:
                            segs.append((q, lo - s0, hi - lo, o))
                            o += hi - lo + 2
                    Wc = o - 2
                    for m in range(2):
                        j = (c0 + m * 128) // 128
                        G, U, C_, A = (x[cnt[0] % 2] for x in (ge, ub, cb, ab))
                        cnt[0] += 1
                        hq = hist_ap
                        for (q, a, ln, og) in segs:
                            self.dve.op(lambda e: e.tensor_copy(out=G.ap[:, og:og + 2], in_=hq(j, q)),
                                        r=[ghp if q == 0 else ghs], wm=[G])
                        f2 = int(os.environ.get('DBG_FF2', '9'))
                        for which, dstt, off in ((0, G, 2), (1, U, 0)):
                            if f2 < 2:
                                break
                            for (n0, nw) in split_even(sw, 512):
                                ps = self.mm_f(wt, which * 256 + m * 128, 128, xin, n0, nw, 16)
                                if f2 < 3:
                                    continue
                                for (q, a, ln, og) in segs:
                                    lo, hi = max(n0, a), min(n0 + nw, a + ln)
                                    if lo < hi:
                                        self.copy(self.evac_eng(), dstt.ap[:, og + off + lo - a:og + off + hi - a], ps.ap[:, lo - n0:hi - n0],
                                                  r=[ps], wm=[dstt])
                        if flv < 3:
                            continue
                        wc = PV_FCW + j * 3
                        self.dve.op(lambda e: e.tensor_scalar(out=C_.ap[:, :Wc], in0=G.ap[:, 0:Wc], scalar1=pv.ap[:, wc:wc + 1], scalar2=None,
                                                              op0=ALU.mult), r=[G, pv], w=[C_])
                        for t_ in (1, 2):
                            self.dve.op(lambda e: e.scalar_tensor_tensor(out=C_.ap[:, :Wc], in0=G.ap[:, t_:t_ + Wc], scalar=pv.ap[:, wc + t_:wc + t_ + 1],
                                                                         in1=C_.ap[:, :Wc], op0=ALU.mult, op1=ALU.add), r=[G, pv, C_], w=[C_])
                        self.act.op(lambda e: e.activation(out=C_.ap[:, :Wc], in_=C_.ap[:, :Wc], func=AF.Silu), r=[C_], w=[C_])
                        self.dve.op(lambda e: e.tensor_tensor(out=A.ap[:, :Wc], in0=C_.ap[:, :Wc], in1=U.ap[:, :Wc], op=ALU.mult),
                                    r=[C_, U], w=[A])
                        if flv < 4:
                            continue
                        for (q, a, ln, og) in segs:
                            self.sp.dma(out=self.aT.ap[j * 128:(j + 1) * 128, s0 + a:s0 + a + ln], in_=A.ap[:, og:og + ln], S=A.sem,
                                        r=[A], wm=[self.aT])
                            self.dve.op(lambda e: e.tensor_copy(out=hq(j, q), in_=G.ap[:, og + ln:og + ln + 2]), r=[G],
                                        wm=[ghp if q == 0 else ghs])

                self.linear(es3, self.hT, 16, blocks, CW, 512, handler)
                self.barrier()
            if flv < 5:
                self.barrier()
                return
            self.feat2tok(es, "f_op", ghp, 2, DFF, self.o_p["ffn"][l])
            self.feat2tok(es, "f_os", ghs, 2 * NS, DFF, self.o_s["ffn"][l].rearrange("s r c -> (s r) c"))
            self.barrier()

    def build(self):
        self.setup()
        self.phase_transpose_in()
        for l in range(self.depth):
            self.load_pvec(l)
            self.phase_norm(PV_GMIX)
            self.phase_inproj(l)
            self.mix_short(l)
            self.mix_gla(l)
            self.mix_cconv(l)
            self.mix_attn(l)
            self.phase_resproj(self.yT, 16, self.w_out[l], 2080, 512, dbl=True)
            self.phase_norm(PV_GFFN)
            self.phase_ffn_hidden(l)
            self.phase_resproj(self.aT, 44, self.w_down[l], 1040, 256)
        self.phase_norm(PV_GFIN, final=True)
        return self.finish()

    def finish(self):
        self.barrier()
        self.es.close()
        return self.nc


def _fm(v, ntile):
    return np.ascontiguousarray(np.asarray(v, np.float32).reshape(ntile, 128).T)


def prep_pvec(inp, depth):
    out = np.zeros((depth, 128, NV), np.float32)
    for l in range(depth):
        o = out[l]
        o[:, PV_GMIX:PV_GMIX + 16] = _fm(inp["g_mix"][l], 16)
        o[:, PV_GFFN:PV_GFFN + 16] = _fm(inp["g_ffn"][l], 16)
        for j in range(3):
            o[:, PV_CAW + j:PV_CAW + 12:3] = _fm(inp["conv_a_w"][l, j], 4)
        for j in range(31):
            o[:, PV_CCW + j:PV_CCW + 124:31] = _fm(inp["cconv_w"][l, j], 4)
        o[:, PV_CCB:PV_CCB + 4] = _fm(inp["cconv_b"][l], 4)
        o[:, PV_CLG:PV_CLG + 4] = _fm(inp["cln_g"][l], 4)
        o[:, PV_CLB:PV_CLB + 4] = _fm(inp["cln_b"][l], 4)
        o[:, PV_GLAB:PV_GLAB + 2] = _fm(inp["gla_b_gate"][l], 2)
        o[:, PV_GLAG:PV_GLAG + 1] = _fm(inp["gla_g_norm"][l], 1)
        o[:64, PV_GLAB4:PV_GLAB4 + 4] = np.asarray(inp["gla_b_gate"][l], np.float32).reshape(4, 64).T
        for j in range(3):
            o[:, PV_FCW + j:PV_FCW + 132:3] = _fm(inp["ffn_conv_w"][l, j], 44)
        o[:, PV_GFIN:PV_GFIN + 16] = _fm(inp["g_final"], 16)
    return out


def prep_bias(inp, depth):
    rb = np.asarray(inp["rel_bias"], np.float32)[:depth]
    p = np.arange(128)[:, None]
    q = np.arange(64)[None, :]
    tabs = []
    for d0 in (128, 0, 192, 64, 1024):
        idx = np.clip(d0 + q - p, -128, 128) + 128
        tabs.append(rb[:, :, idx])
    t = np.stack(tabs, axis=2)
    t = np.transpose(t, (0, 3, 1, 2, 4))
    biasT = np.ascontiguousarray(t.reshape(depth, 128, 8 * 5 * 64))
    rb_rep = np.ascontiguousarray(np.broadcast_to(rb.reshape(depth, 1, 8 * 257), (depth, 128, 8 * 257)))
    return biasT, rb_rep


_NC_CACHE = {}


def make_in_maps(inp, n_cores, TP, NS, depth):
    f32 = lambda a: np.ascontiguousarray(np.asarray(a, np.float32))
    B = inp["x_prompt"].shape[0]
    pvec = prep_pvec(inp, depth)
    biasT, rb_rep = prep_bias(inp, depth)
    shared = {
        "pvec": pvec, "biasT": biasT, "rb_rep": rb_rep,
        "w_in": f32(inp["w_in"][:depth]), "w_out": f32(inp["w_out"][:depth]),
        "w_gate": f32(inp["w_ffn_gate"][:depth]), "w_up": f32(inp["w_ffn_up"][:depth]),
        "w_down": f32(inp["w_ffn_down"][:depth]), "gla_w2": f32(inp["gla_w_gate2"][:depth]),
    }
    maps = []
    for c in range(n_cores):
        b = c % B
        s0, s1 = c * NS, (c + 1) * NS
        m = dict(shared)
        m["x_all"] = f32(np.concatenate([inp["x_prompt"][b, :TP], np.asarray(inp["x_sample"][s0:s1]).reshape(NS * 16, D)], 0))
        m["st_short"] = f32(inp["state_short_conv"][:depth, s0:s1])
        m["st_gla"] = f32(np.asarray(inp["state_gla"][:depth, s0:s1]).reshape(depth, NS, 256, 128))
        m["st_cconv"] = f32(inp["state_conformer_conv"][:depth, s0:s1])
        m["st_k"] = f32(np.asarray(inp["cache_attn_k"][:depth, s0:s1]).reshape(depth, NS, 512, GW))
        m["st_v"] = f32(np.asarray(inp["cache_attn_v"][:depth, s0:s1]).reshape(depth, NS, 512, GW))
        m["st_ffn"] = f32(inp["state_ffn_conv"][:depth, s0:s1])
        maps.append(m)
    return maps


def gather_outputs(results, B, TP, NS, depth):
    n = len(results)
    r = results
    yp = np.stack([r[b]["y_all"][:TP] for b in range(B)], 0)
    ys = np.concatenate([r[c]["y_all"][TP:].reshape(NS, 16, D) for c in range(n)], 0)
    P = lambda k: np.stack([r[b][k] for b in range(B)], 1)
    Sx = lambda k: np.concatenate([r[c][k] for c in range(n)], 1)
    kr = min(512, TP)
    outs = (
        yp, ys,
        P("p_short"), P("p_gla").reshape(depth, B, 4, 64, 128), P("p_cconv"),
        P("p_k")[:, :, :kr].reshape(depth, B, kr, 8, 64), P("p_v")[:, :, :kr].reshape(depth, B, kr, 8, 64), P("p_ffn"),
        Sx("s_short"), Sx("s_gla").reshape(depth, n * NS, 4, 64, 128), Sx("s_cconv"),
        Sx("s_k").reshape(depth, n * NS, 16, 8, 64), Sx("s_v").reshape(depth, n * NS, 16, 8, 64), Sx("s_ffn"),
    )
    return tuple(np.ascontiguousarray(o, dtype=np.float32) for o in outs)


def kernel(**inputs):
    n_cores = 8
    TP, NS, depth = 4096, 4, DEPTH
    key = (TP, NS, depth)
    if key not in _NC_CACHE:
        _NC_CACHE[key] = K(TP, NS, depth).build()
    nc = _NC_CACHE[key]
    maps = make_in_maps(inputs, n_cores, TP, NS, depth)
    res = run_bass_kernel_spmd(nc, maps, core_ids=list(range(n_cores)))
    return gather_outputs(res.results, 4, TP, NS, depth)
```

```python
import math
from contextlib import ExitStack

import numpy as np
import concourse.bass as bass
import concourse.mybir as mybir
from concourse.bass_utils import run_bass_kernel_spmd

F32 = mybir.dt.float32
BF16 = mybir.dt.bfloat16
AF = mybir.ActivationFunctionType
ALU = mybir.AluOpType
AX = mybir.AxisListType

D = 2048
GW = 512
DFF = 5632
INC = 5648
EPS = 1e-6
DEPTH = 4
O_XA, O_BG, O_CG, O_Q, O_K, O_V, O_G, O_R, O_CV, O_CGT, O_DQ, O_DK, O_DV = (
    0, 512, 1024, 1536, 1792, 2048, 2560, 3072, 3088, 3600, 4112, 4624, 5136)

PV_GMIX, PV_GFFN, PV_CAW, PV_CCW, PV_CCB, PV_CLG, PV_CLB, PV_GLAB, PV_GLAG, PV_FCW, PV_GFIN = (
    0, 16, 32, 44, 168, 172, 176, 180, 182, 183, 315)
PV_GLAB4 = 331
NV = 335


class Sem:
    __slots__ = ("h", "n")

    def __init__(self, h):
        self.h = h
        self.n = 0


class Tile:
    __slots__ = ("ap", "w", "r", "war", "sem", "excl")

    def __init__(self, ap, sem=None, excl=False):
        self.ap = ap
        self.excl = excl
        self.w = []
        self.r = []
        self.war = []
        self.sem = sem

    def __getitem__(self, idx):
        return self.ap[idx]


def _merge(evs):
    d = {}
    for S, v in evs:
        if v > d.get(S, 0):
            d[S] = v
    return list(d.items())


class Eng:
    def __init__(self, eng, S, selfsync):
        self.eng = eng
        self.S = S
        self.selfsync = selfsync
        self.seen = {}

    def wait_for(self, evs):
        for S, v in _merge(evs):
            if S is self.S and not self.selfsync:
                continue
            if self.seen.get(S, 0) >= v:
                continue
            self.eng.wait_ge(S.h, v)
            self.seen[S] = v

    @staticmethod
    def _deps(r, w, wm):
        evs = []
        for t in r:
            evs += t.w
            if t.excl:
                evs += t.r
        for t in w:
            evs += t.w
            evs += t.r
            evs += t.war
        for t in wm:
            if t.r:
                t.war = _merge(t.war + t.r + t.w)
                t.w = []
                t.r = []
            evs += t.war
        return evs

    @staticmethod
    def _post(ev, r, w, wm):
        for t in r:
            t.r = _merge(t.r + [ev])
        for t in w:
            t.w = [ev]
            t.r = []
            t.war = []
        for t in wm:
            t.w = _merge(t.w + [ev])

    def op(self, fn, r=(), w=(), wm=()):
        self.wait_for(self._deps(r, w, wm))
        inst = fn(self.eng)
        self.S.n += 1
        inst.then_inc(self.S.h, 1)
        self._post((self.S, self.S.n), r, w, wm)

    def dma(self, out, in_, S, r=(), w=(), wm=(), **kw):
        evs = self._deps(r, w, wm)
        if S.n:
            evs.append((S, S.n))
        self.wait_for(evs)
        inst = self.eng.dma_start(out=out, in_=in_, **kw)
        S.n += 16
        inst.then_inc(S.h, 16)
        self._post((S, S.n), r, w, wm)


def split_even(n, maxw, mult=16):
    k = (n + maxw - 1) // maxw
    base = ((n + k - 1) // k + mult - 1) // mult * mult
    out = []
    s = 0
    while s < n:
        w = min(base, n - s)
        out.append((s, w))
        s += w
    return out


class K:
    def __init__(self, TP, NS, depth, debug=()):
        self.TP, self.NS, self.depth = TP, NS, depth
        self.TS = 16 * NS
        self.TT = TP + self.TS
        self.debug = set(debug)
        self.nc = nc = bass.Bass("TRN2", target_bir_lowering=False)
        self.es = ExitStack()
        self.sems_free = []
        self.all_sems = []
        S = lambda: self._new_sem()
        self.pe = Eng(nc.tensor, S(), False)
        self.act = Eng(nc.scalar, S(), True)
        self.dve = Eng(nc.vector, S(), True)
        self.pool = Eng(nc.gpsimd, S(), True)
        self.sp = Eng(nc.sync, S(), True)
        self.engs = [self.pe, self.act, self.dve, self.pool, self.sp]
        self.sw_sems = [S() for _ in range(2)]
        self.scr_sem = S()
        self.dma_sems = [S() for _ in range(21)]
        self._dma_rr = 0
        self._evac_rr = 0

    def _new_sem(self):
        h = self.es.enter_context(self.nc.semaphore(f"s{len(self.all_sems)}"))
        s = Sem(h)
        self.all_sems.append(s)
        return s

    def dsem(self):
        s = self.dma_sems[self._dma_rr % len(self.dma_sems)]
        self._dma_rr += 1
        return s

    def sb(self, es, name, shape, dtype, dma=False):
        reuse = getattr(self, '_reuse', None)
        if reuse is not None:
            if name in reuse:
                return reuse[name]
            t = self._sb_new(self._reuse_es, name, shape, dtype, dma)
            reuse[name] = t
            return t
        return self._sb_new(es, name, shape, dtype, dma)

    def _sb_new(self, es, name, shape, dtype, dma=False):
        self._uid = getattr(self, '_uid', 0) + 1
        t = es.enter_context(self.nc.sbuf_tensor(f"{name}_{self._uid}", list(shape), dtype))
        if dma == "sw0" or dma == "sw1":
            return Tile(t, self.sw_sems[int(dma[2])])
        return Tile(t, self.dsem() if dma else None)

    def dram_in(self, name, shape, dtype=F32):
        return self.nc.dram_tensor(name, list(shape), dtype, kind="ExternalInput").ap()

    def dram_out(self, name, shape, dtype=F32):
        return self.nc.dram_tensor(name, list(shape), dtype, kind="ExternalOutput").ap()

    def scratch(self, name, shape, dtype):
        kind = "ExternalOutput" if name in self.debug else "Internal"
        ap = self.nc.dram_tensor(name, list(shape), dtype, kind=kind).ap()
        return Tile(ap)

    def barrier(self):
        if getattr(self, '_reuse', None) is not None:
            return
        evs = [(s, s.n) for s in self.all_sems if s.n]
        for e in self.engs:
            e.wait_for(evs)

    def evac_eng(self):
        self._evac_rr += 1
        return self.act if self._evac_rr % 2 else self.dve

    def copy(self, eng, out, in_, **kw):
        if eng is self.act:
            eng.op(lambda e: e.activation(out=out, in_=in_, func=AF.Copy), **kw)
        else:
            eng.op(lambda e: e.tensor_copy(out=out, in_=in_), **kw)

    def setup(self):
        nc, es = self.nc, self.es
        TT, NS, dp = self.TT, self.NS, self.depth
        self.ps = [Tile(es.enter_context(nc.psum_tensor(f"ps{i}", [128, 512], F32)), excl=True) for i in range(8)]
        self._ps_rr = 0
        self.x_all = self.dram_in("x_all", [TT, D])
        self.pvec = self.dram_in("pvec", [dp, 128, NV])
        self.w_in = self.dram_in("w_in", [dp, D, INC])
        self.w_out = self.dram_in("w_out", [dp, D, D])
        self.w_gate = self.dram_in("w_gate", [dp, D, DFF])
        self.w_up = self.dram_in("w_up", [dp, D, DFF])
        self.w_down = self.dram_in("w_down", [dp, DFF, D])
        self.gla_w2 = self.dram_in("gla_w2", [dp, 16, 256])
        self.biasT = self.dram_in("biasT", [dp, 128, 8 * 5 * 64])
        self.rb_rep = self.dram_in("rb_rep", [dp, 128, 8 * 257])
        self.st_short = self.dram_in("st_short", [dp, NS, 2, GW])
        self.st_gla = self.dram_in("st_gla", [dp, NS, 256, 128])
        self.st_cconv = self.dram_in("st_cconv", [dp, NS, 30, GW])
        self.st_k = self.dram_in("st_k", [dp, NS, 512, GW])
        self.st_v = self.dram_in("st_v", [dp, NS, 512, GW])
        self.st_ffn = self.dram_in("st_ffn", [dp, NS, 2, DFF])
        self.y_all = self.dram_out("y_all", [TT, D])
        self.o_p = {
            "short": self.dram_out("p_short", [dp, 2, GW]),
            "gla": self.dram_out("p_gla", [dp, 256, 128]),
            "cconv": self.dram_out("p_cconv", [dp, 30, GW]),
            "k": self.dram_out("p_k", [dp, 512, GW]),
            "v": self.dram_out("p_v", [dp, 512, GW]),
            "ffn": self.dram_out("p_ffn", [dp, 2, DFF]),
        }
        self.o_s = {
            "short": self.dram_out("s_short", [dp, NS, 2, GW]),
            "gla": self.dram_out("s_gla", [dp, NS, 256, 128]),
            "cconv": self.dram_out("s_cconv", [dp, NS, 30, GW]),
            "k": self.dram_out("s_k", [dp, NS, 16, GW]),
            "v": self.dram_out("s_v", [dp, NS, 16, GW]),
            "ffn": self.dram_out("s_ffn", [dp, NS, 2, DFF]),
        }
        self.xT = self.scratch("xT", [D, TT], F32)
        self.xTt = [Tile(self.xT.ap[k * 128:(k + 1) * 128, :]) for k in range(16)]
        self.hT = self.scratch("hT", [D, TT], BF16)
        self.zT = self.scratch("zT", [INC, TT], F32)
        self.vtok_a = self.scratch("vtok_a", [TT, GW], F32)
        self.vtok_g = self.scratch("vtok_g", [TT, GW], F32)
        self.ktok = self.scratch("ktok", [TT, GW], F32)
        self.yT = self.scratch("yT", [D, TT], BF16)
        self.aT = self.scratch("aT", [DFF, TT], BF16)
        self.ident = self.sb(es, "ident", [128, 128], F32)
        self.ones32 = self.sb(es, "ones32", [128, 128], F32)
        self.identb = self.sb(es, "identb", [128, 128], BF16)
        self.onesb = self.sb(es, "onesb", [128, 128], BF16)
        self.selA = self.sb(es, "selA", [128, 128], BF16)
        self.selB = self.sb(es, "selB", [128, 128], BF16)
        self.trimask = self.sb(es, "trimask", [64, 64], F32)
        self.pv = self.sb(es, "pv", [128, NV], F32, dma=True)
        P = self.pool
        self.epsc = self.sb(es, 'epsc', [128, 1], F32)
        P.op(lambda e: e.memset(self.epsc.ap[:, :], EPS), w=[self.epsc])
        self.onec = self.sb(es, 'onec', [128, 1], F32)
        P.op(lambda e: e.memset(self.onec.ap[:, :], 1.0), w=[self.onec])
        P.op(lambda e: e.memset(self.ident.ap[:, :], 0.0), w=[self.ident])
        P.op(lambda e: e.affine_select(out=self.ident.ap[:, :], in_=self.ident.ap[:, :], compare_op=ALU.not_equal,
                                       fill=1.0, base=0, pattern=[[-1, 128]], channel_multiplier=1),
             w=[self.ident])
        P.op(lambda e: e.memset(self.ones32.ap[:, :], 1.0), w=[self.ones32])
        P.op(lambda e: e.memset(self.onesb.ap[:, :], 1.0), w=[self.onesb])
        P.op(lambda e: e.tensor_copy(out=self.identb.ap[:, :], in_=self.ident.ap[:, :]), r=[self.ident], w=[self.identb])
        P.op(lambda e: e.memset(self.selA.ap[:, :], 0.0), w=[self.selA])
        P.op(lambda e: e.memset(self.selA.ap[0:64, :], 1.0), w=[self.selA])
        P.op(lambda e: e.memset(self.selB.ap[:, :], 0.0), w=[self.selB])
        P.op(lambda e: e.memset(self.selB.ap[64:128, :], 1.0), w=[self.selB])
        P.op(lambda e: e.memset(self.trimask.ap[:, :], 1.0), w=[self.trimask])
        P.op(lambda e: e.affine_select(out=self.trimask.ap[:, :], in_=self.trimask.ap[:, :], compare_op=ALU.is_ge,
                                       fill=0.0, base=0, pattern=[[1, 64]], channel_multiplier=-1),
             w=[self.trimask])

    def rstd(self, out, in_, inv_n, rtiles, wtile):
        self.act.op(lambda e: e.activation(out=out, in_=in_, func=AF.Ln, scale=inv_n, bias=self.epsc.ap[:out.shape[0], :]),
                    r=list(rtiles) + [self.epsc], w=[wtile])
        self.act.op(lambda e: e.activation(out=out, in_=out, func=AF.Exp, scale=-0.5), r=[wtile], w=[wtile])

    def psb(self):
        t = self.ps[self._ps_rr % 6]
        self._ps_rr += 1
        return t

    def load_pvec(self, l):
        self.sp.dma(out=self.pv.ap[:, :], in_=self.pvec[l], S=self.pv.sem, w=[self.pv])

    def phase_transpose_in(self):
        TT = self.TT
        xTv = self.xT.ap.rearrange("(k p) t -> p k t", p=128)
        with ExitStack() as es:
            xin = [self.sb(es, f"p0i{i}", [128, D], F32, dma=True) for i in range(2)]
            xo = [self.sb(es, f"p0o{i}", [128, 16, 128], F32, dma=True) for i in range(2)]
            for i in range((TT + 127) // 128):
                r0 = i * 128
                n = min(128, TT - r0)
                ti, to = xin[i % 2], xo[i % 2]
                self.sp.dma(out=ti.ap[:n, :], in_=self.x_all[r0:r0 + n, :], S=ti.sem, w=[ti])
                for j in range(4):
                    ps = self.psb()
                    for q in range(4):
                        f = j * 4 + q
                        self.pe.op(lambda e: e.transpose(out=ps.ap[:, q * 128:q * 128 + n],
                                                         in_=ti.ap[:n, f * 128:(f + 1) * 128],
                                                         identity=self.ident.ap[:n, :n]),
                                   r=[ti, self.ident], w=[ps])
                    src = ps.ap.rearrange("p (a b) -> p a b", b=128)[:, :, :n]
                    self.copy(self.evac_eng(), to.ap[:, j * 4:(j + 1) * 4, :n], src, r=[ps], wm=[to])
                self.sp.dma(out=xTv[:, :, r0:r0 + n], in_=to.ap[:, :, :n], S=to.sem, r=[to], wm=self.xTt)
            self.barrier()

    def phase_norm(self, gcol, final=False):
        TT = self.TT
        xTv = self.xT.ap.rearrange("(k p) t -> p k t", p=128)
        hTv = self.hT.ap.rearrange("(k p) t -> p k t", p=128)
        with ExitStack() as es:
            xb = [self.sb(es, f"n_x{i}", [128, 16, 512], F32, dma=True) for i in range(2)]
            sq = [self.sb(es, f"n_sq{i}", [128, 16, 512], F32) for i in range(1)]
            rs = [self.sb(es, f"n_rs{i}", [128, 512], F32) for i in range(2)]
            if final:
                ho = [self.sb(es, f"n_h{i}", [128, 16, 512], F32) for i in range(2)]
                yo = [self.sb(es, f"n_y{i}", [128, D], F32, dma=True) for i in range(2)]
            else:
                ho = [self.sb(es, f"n_h{i}", [128, 16, 512], BF16, dma=True) for i in range(2)]
            nyo = 0
            for bi, (c0, cw) in enumerate(split_even(TT, 512)):
                x, s, r, h = xb[bi % 2], sq[0], rs[bi % 2], ho[bi % 2]
                self.sp.dma(out=x.ap[:, :, :cw], in_=xTv[:, :, c0:c0 + cw], S=x.sem, r=self.xTt, w=[x])
                self.act.op(lambda e: e.activation(out=s.ap[:, :, :cw], in_=x.ap[:, :, :cw], func=AF.Square),
                            r=[x], w=[s])
                ps = self.psb()
                for k in range(16):
                    self.pe.op(lambda e: e.matmul(ps.ap[:, :cw], lhsT=self.ones32.ap[:, :], rhs=s.ap[:, k, :cw],
                                                  start=(k == 0), stop=(k == 15)),
                               r=[self.ones32, s], w=[ps])
                self.rstd(r.ap[:, :cw], ps.ap[:, :cw], 1.0 / D, [ps], r)
                for k in range(16):
                    eng = self.dve
                    eng.op(lambda e: e.scalar_tensor_tensor(out=h.ap[:, k, :cw], in0=x.ap[:, k, :cw],
                                                            scalar=self.pv.ap[:, gcol + k:gcol + k + 1],
                                                            in1=r.ap[:, :cw], op0=ALU.mult, op1=ALU.mult),
                           r=[x, r, self.pv], wm=[h])
                if not final:
                    self.sp.dma(out=hTv[:, :, c0:c0 + cw], in_=h.ap[:, :, :cw], S=h.sem, r=[h], wm=[self.hT])
                else:
                    for t0 in range(0, cw, 128):
                        n = min(128, cw - t0)
                        y = yo[nyo % 2]
                        nyo += 1
                        for j in range(4):
                            ps2 = self.psb()
                            for q in range(4):
                                f = j * 4 + q
                                self.pe.op(lambda e: e.transpose(out=ps2.ap[:n, q * 128:(q + 1) * 128],
                                                                 in_=h.ap[:, f, t0:t0 + n],
                                                                 identity=self.ident.ap[:, :]),
                                           r=[h, self.ident], w=[ps2])
                            self.copy(self.evac_eng(), y.ap[:n, j * 512:(j + 1) * 512], ps2.ap[:n, :],
                                      r=[ps2], wm=[y])
                        self.sp.dma(out=self.y_all[c0 + t0:c0 + t0 + n, :], in_=y.ap[:n, :], S=y.sem, r=[y])
            self.barrier()

    def seq_ranges(self):
        return [(0, self.TP)] + [(self.TP + 16 * s, 16) for s in range(self.NS)]

    def mm_f(self, wt, wc0, M, xin, n0, nw, KT):
        ps = self.psb()
        for k in range(KT):
            self.pe.op(lambda e: e.matmul(ps.ap[:M, :nw], lhsT=wt.ap[:, k, wc0:wc0 + M], rhs=xin.ap[:, k, n0:n0 + nw],
                                          start=(k == 0), stop=(k == KT - 1)), r=[wt, xin], w=[ps])
        return ps

    def mm_t(self, wt, wc0, w, xin, t0, tn, KT):
        ps = self.psb()
        for k in range(KT):
            self.pe.op(lambda e: e.matmul(ps.ap[:tn, :w], lhsT=xin.ap[:, k, t0:t0 + tn], rhs=wt.ap[:, k, wc0:wc0 + w],
                                          start=(k == 0), stop=(k == KT - 1)), r=[wt, xin], w=[ps])
        return ps

    def linear(self, es, src, KT, blocks, CW, SLOTW, handler, dbl=False, src_r0=0):
        TT = self.TT
        srcv = src.ap[src_r0:src_r0 + KT * 128, :].rearrange("(k p) t -> p k t", p=128)
        nsb = (TT + CW - 1) // CW
        xins = [self.sb(es, f"lin_x{i}", [128, KT, CW], BF16, dma=True) for i in range(2 if (nsb > 1 and dbl) else 1)]
        wts = [self.sb(es, f"lin_w{i}", [128, KT, SLOTW], BF16, dma=f"sw{i}") for i in range(2)]
        wi = 0

        def load_x(sbi):
            s0 = sbi * CW
            sw = min(CW, TT - s0)
            xin = xins[sbi % len(xins)]
            self.sp.dma(out=xin.ap[:, :, :sw], in_=srcv[:, :, s0:s0 + sw], S=xin.sem, r=[src], w=[xin])

        def load_w(parts, wt):
            off = 0
            for (Wap, c0, w) in parts:
                Wv = Wap.rearrange("(k p) n -> p k n", p=128)
                self.pool.dma(out=wt.ap[:, :, off:off + w], in_=Wv[:, :, c0:c0 + w], S=wt.sem, wm=[wt])
                off += w

        if len(xins) > 1:
            load_x(0)
        for sbi in range(nsb):
            s0 = sbi * CW
            sw = min(CW, TT - s0)
            xin = xins[sbi % len(xins)]
            if len(xins) == 1:
                load_x(sbi)
            elif sbi + 1 < nsb:
                load_x(sbi + 1)
            load_w(blocks[0][0], wts[wi % 2])
            for bi, (parts, tag) in enumerate(blocks):
                wt = wts[wi % 2]
                wi += 1
                if bi + 1 < len(blocks):
                    load_w(blocks[bi + 1][0], wts[wi % 2])
                handler(tag, parts, wt, xin, s0, sw)

    def phase_inproj(self, l):
        W = self.w_in[l]
        TP, TT = self.TP, self.TT
        blocks = []
        for (c0, wd, tag) in [(O_XA, 512, "f"), (O_BG, 512, "f"), (O_CG, 512, "f"), (O_Q, 256, "f"), (O_K, 256, "f"),
                              (O_V, 512, "tg"), (O_G, 512, "f"), (O_R, 16, "f"), (O_CV, 512, "f"), (O_CGT, 512, "f"),
                              (O_DQ, 512, "f"), (O_DK, 512, "fk"), (O_DV, 512, "ta")]:
            blocks.append(([(W, c0, wd)], tag))
        with ExitStack() as es:
            stf = [self.sb(es, f"z_sf{i}", [128, 2080], F32, dma=True) for i in range(2)]
            stt = [self.sb(es, f"z_st{i}", [128, 512], F32, dma=True) for i in range(3)]
            cnt = {"f": 0, "t": 0}

            def handler(tag, parts, wt, xin, s0, sw):
                (_, c0, wd) = parts[0]
                if tag[0] == "f":
                    for m0 in range(0, wd, 128):
                        M = min(128, wd - m0)
                        st = stf[cnt["f"] % 2]
                        cnt["f"] += 1
                        for (n0, nw) in split_even(sw, 512):
                            ps = self.mm_f(wt, m0, M, xin, n0, nw, 16)
                            self.copy(self.evac_eng(), st.ap[:M, n0:n0 + nw], ps.ap[:M, :nw], r=[ps], wm=[st])
                        self.sp.dma(out=self.zT.ap[c0 + m0:c0 + m0 + M, s0:s0 + sw], in_=st.ap[:M, :sw], S=st.sem,
                                    r=[st], wm=[self.zT])
                if tag[0] == "t" or tag == "fk":
                    dst = {"tg": self.vtok_g, "ta": self.vtok_a, "fk": self.ktok}[tag]
                    for t0 in range(0, sw, 128):
                        tn = min(128, sw - t0)
                        if tag == "fk" and (s0 + t0 + tn <= TP - 512):
                            continue
                        st = stt[cnt["t"] % 3]
                        cnt["t"] += 1
                        ps = self.mm_t(wt, 0, wd, xin, t0, tn, 16)
                        self.copy(self.evac_eng(), st.ap[:tn, :wd], ps.ap[:tn, :wd], r=[ps], w=[st])
                        self.sp.dma(out=dst.ap[s0 + t0:s0 + t0 + tn, :], in_=st.ap[:tn, :wd], S=st.sem,
                                    r=[st], wm=[dst])

            self.linear(es, self.hT, 16, blocks, 2080, 512, handler, dbl=True)
            self.barrier()

    def tok2feat(self, es, name, src_ap, R, C):
        nt = C // 128
        stg = self.sb(es, name + "_s", [R, C], F32, dma=True)
        dst = self.sb(es, name + "_d", [128, nt, R], F32)
        self.sp.dma(out=stg.ap[:, :], in_=src_ap, S=stg.sem, w=[stg])
        per = max(1, 512 // R)
        for g0 in range(0, nt, per):
            g = min(per, nt - g0)
            ps = self.psb()
            for q in range(g):
                self.pe.op(lambda e: e.transpose(out=ps.ap[:, q * R:(q + 1) * R],
                                                 in_=stg.ap[:R, (g0 + q) * 128:(g0 + q + 1) * 128],
                                                 identity=self.ident.ap[:R, :R]), r=[stg, self.ident], w=[ps])
            self.copy(self.evac_eng(), dst.ap[:, g0:g0 + g, :],
                      ps.ap[:, :g * R].rearrange("p (a b) -> p a b", b=R), r=[ps], wm=[dst])
        return dst

    def feat2tok(self, es, name, src, R, C, dst_ap):
        nt = C // 128
        stg = self.sb(es, name + "_o", [R, C], F32, dma=True)
        for g0 in range(0, nt, 4):
            g = min(4, nt - g0)
            ps = self.psb()
            for q in range(g):
                self.pe.op(lambda e: e.transpose(out=ps.ap[:R, q * 128:(q + 1) * 128], in_=src.ap[:, g0 + q, :],
                                                 identity=self.ident.ap[:, :]), r=[src, self.ident], w=[ps])
            self.copy(self.evac_eng(), stg.ap[:R, g0 * 128:(g0 + g) * 128], ps.ap[:R, :g * 128], r=[ps], wm=[stg])
        self.sp.dma(out=dst_ap, in_=stg.ap[:R, :], S=stg.sem, r=[stg])

    def mix_short(self, l):
        TP, NS, TT = self.TP, self.NS, self.TT
        zv = self.zT.ap
        pv = self.pv
        import os
        for (is_s, nseq, T, col0) in [(False, 1, TP, 0), (True, NS, 16, TP)]:
            if is_s and os.environ.get('DBG_NOSAMPLE'):
                continue
            with ExitStack() as es:
                BW = min(T, 1024)
                nblk = (T + BW - 1) // BW
                W = nseq * BW
                if is_s:
                    hist = self.tok2feat(es, "a_h", self.st_short[l].rearrange("s r c -> (s r) c"), 2 * NS, GW)
                stout = self.sb(es, "a_so", [128, 4, nseq * 2], F32)
                xa = [self.sb(es, f"a_xa{i}", [128, W], F32, dma=True) for i in range(2)]
                bg = [self.sb(es, f"a_bg{i}", [128, W], F32, dma=True) for i in range(2)]
                cg = [self.sb(es, f"a_cg{i}", [128, W], F32, dma=True) for i in range(2)]
                ue = [self.sb(es, f"a_ue{i}", [128, nseq, BW + 2], F32) for i in range(2)]
                cc = [self.sb(es, f"a_cc{i}", [128, nseq, BW], F32) for i in range(2)]
                yo = [self.sb(es, f"a_yo{i}", [128, nseq, BW], BF16, dma=True) for i in range(2)]
                it = 0
                for i in range(4):
                    for b in range(nblk):
                        c0 = col0 + b * BW
                        cw = min(BW, T - b * BW) if not is_s else BW
                        n = cw * nseq
                        X, Bg, Cg, U, C_, Y = (t[it % 2] for t in (xa, bg, cg, ue, cc, yo))
                        Uprev = ue[(it + 1) % 2]
                        it += 1
                        self.sp.dma(out=X.ap[:, :n], in_=zv[O_XA + i * 128:O_XA + (i + 1) * 128, c0:c0 + n], S=X.sem,
                                    r=[self.zT], w=[X])
                        self.sp.dma(out=Bg.ap[:, :n], in_=zv[O_BG + i * 128:O_BG + (i + 1) * 128, c0:c0 + n], S=Bg.sem,
                                    r=[self.zT], w=[Bg])
                        self.sp.dma(out=Cg.ap[:, :n], in_=zv[O_CG + i * 128:O_CG + (i + 1) * 128, c0:c0 + n], S=Cg.sem,
                                    r=[self.zT], w=[Cg])
                        v3 = lambda t: t.ap[:, :n].rearrange("p (s c) -> p s c", s=nseq)
                        if is_s:
                            self.dve.op(lambda e: e.tensor_copy(out=U.ap[:, :, 0:2],
                                                                in_=hist.ap[:, i, :].rearrange("p (s r) -> p s r", r=2)),
                                        r=[hist], w=[U])
                        elif b == 0:
                            self.dve.op(lambda e: e.memset(U.ap[:, :, 0:2], 0.0), w=[U])
                        else:
                            self.dve.op(lambda e: e.tensor_copy(out=U.ap[:, :, 0:2], in_=Uprev.ap[:, :, BW:BW + 2]),
                                        r=[Uprev], w=[U])
                        self.dve.op(lambda e: e.tensor_tensor(out=U.ap[:, :, 2:2 + cw], in0=v3(Cg), in1=v3(X), op=ALU.mult),
                                    r=[Cg, X], wm=[U])
                        wc = PV_CAW + i * 3
                        self.dve.op(lambda e: e.tensor_scalar(out=C_.ap[:, :, :cw], in0=U.ap[:, :, 0:cw],
                                                              scalar1=pv.ap[:, wc:wc + 1], scalar2=None, op0=ALU.mult),
                                    r=[U, pv], w=[C_])
                        for j in (1, 2):
                            self.dve.op(lambda e: e.scalar_tensor_tensor(out=C_.ap[:, :, :cw], in0=U.ap[:, :, j:j + cw],
                                                                         scalar=pv.ap[:, wc + j:wc + j + 1],
                                                                         in1=C_.ap[:, :, :cw], op0=ALU.mult, op1=ALU.add),
                                        r=[U, pv, C_], w=[C_])
                        self.dve.op(lambda e: e.tensor_tensor(out=Y.ap[:, :, :cw], in0=C_.ap[:, :, :cw], in1=v3(Bg),
                                                              op=ALU.mult), r=[C_, Bg], w=[Y])
                        if nseq == 1:
                            self.sp.dma(out=self.yT.ap[i * 128:(i + 1) * 128, c0:c0 + cw], in_=Y.ap[:, 0, :cw], S=Y.sem,
                                        r=[Y], wm=[self.yT])
                        else:
                            self.sp.dma(out=self.yT.ap[i * 128:(i + 1) * 128, c0:c0 + n].rearrange("p (s c) -> p s c", s=nseq),
                                        in_=Y.ap[:, :, :cw], S=Y.sem, r=[Y], wm=[self.yT])
                        if b == nblk - 1:
                            self.dve.op(lambda e: e.tensor_copy(out=stout.ap[:, i, :].rearrange("p (s r) -> p s r", r=2),
                                                                in_=U.ap[:, :, cw:cw + 2]), r=[U], wm=[stout])
                dst = (self.o_s["short"][l].rearrange("s r c -> (s r) c") if is_s else self.o_p["short"][l])
                if not (os.environ.get('DBG_NOF2T') == '1' or (os.environ.get('DBG_NOF2T') == 'p' and not is_s)):
                    self.feat2tok(es, "a_fo", stout, 2 * nseq, GW, dst)
                self.barrier()

    def build_diag(self, es):
        dg = self.sb(es, "c_diag", [128, 4, 31, 128], BF16)
        for i in range(4):
            for j in range(31):
                c = PV_CCW + i * 31 + j
                self.pool.op(lambda e: e.tensor_scalar(out=dg.ap[:, i, j, :], in0=self.identb.ap[:, :],
                                                       scalar1=self.pv.ap[:, c:c + 1], scalar2=None, op0=ALU.mult),
                             r=[self.identb, self.pv], wm=[dg])
        return dg

    def mix_cconv(self, l):
        TP, NS, TT = self.TP, self.NS, self.TT
        pv = self.pv
        zcv = self.zT.ap[O_CV:O_CV + 512, :].rearrange("(i p) t -> p i t", p=128)
        zcg = self.zT.ap[O_CGT:O_CGT + 512, :].rearrange("(i p) t -> p i t", p=128)
        yv = self.yT.ap[1024:1536, :].rearrange("(i p) t -> p i t", p=128)
        with ExitStack() as es0:
            dg = self.build_diag(es0)
            import os
            clv = int(os.environ.get('DBG_CC', '9'))
            if clv < 2:
                self.barrier()
                return
            for (is_s, nseq, T, col0) in [(False, 1, TP, 0), (True, NS, 16, TP)]:
                with ExitStack() as es:
                    BW = min(T, 512 // nseq)
                    nblk = (T + BW - 1) // BW
                    W = nseq * BW
                    if is_s:
                        hist = self.tok2feat(es, "c_h", self.st_cconv[l].rearrange("s r c -> (s r) c"), 30 * NS, GW)
                    stout = self.sb(es, "c_so", [128, 4, nseq * 30], F32)
                    cv = [self.sb(es, f"c_cv{i}", [128, 4, W], F32, dma=True) for i in range(2)]
                    cg = [self.sb(es, f"c_cg{i}", [128, 4, W], F32, dma=True) for i in range(2)]
                    ue = [self.sb(es, f"c_ue{i}", [128, 4, nseq, BW + 30], BF16) for i in range(2)]
                    uf = [self.sb(es, f"c_uf{i}", [128, 4, nseq, BW + 30], F32) for i in range(2)]
                    cc = [self.sb(es, f"c_cc{i}", [128, 4, W], F32) for i in range(2)]
                    sq = [self.sb(es, f"c_sq{i}", [128, 4, W], F32) for i in range(2)]
                    mean = self.sb(es, "c_mean", [128, W], F32)
                    msq = self.sb(es, "c_msq", [128, W], F32)
                    rs = self.sb(es, "c_rs", [128, W], F32)
                    tt = [self.sb(es, f"c_t{i}", [128, W], F32) for i in range(2)]
                    yo = [self.sb(es, f"c_yo{i}", [128, 4, W], BF16, dma=True) for i in range(2)]
                    def blk_ctx(b):
                        c0 = col0 + b * BW
                        cw = min(BW, T - b * BW)
                        n = cw * nseq
                        return (c0, cw, n) + tuple(t[b % 2] for t in (cv, cg, ue, uf, cc, sq, yo)) + (ue[(b + 1) % 2], uf[(b + 1) % 2])

                    def front(b):
                        (c0, cw, n, CV, CG, UE, UF, CC, SQ, Y, UEp, UFp) = blk_ctx(b)
                        self.sp.dma(out=CV.ap[:, :, :n], in_=zcv[:, :, c0:c0 + n], S=CV.sem, r=[self.zT], w=[CV])
                        self.sp.dma(out=CG.ap[:, :, :n], in_=zcg[:, :, c0:c0 + n], S=CG.sem, r=[self.zT], w=[CG])
                        self.act.op(lambda e: e.activation(out=CG.ap[:, :, :n], in_=CG.ap[:, :, :n], func=AF.Sigmoid),
                                    r=[CG], w=[CG])
                        v4 = lambda t: t.ap[:, :, :n].rearrange("p i (s c) -> p i s c", s=nseq)
                        if is_s:
                            self.dve.op(lambda e: e.tensor_copy(out=UF.ap[:, :, :, 0:30],
                                                                in_=hist.ap[:, :, :].rearrange("p i (s r) -> p i s r", r=30)),
                                        r=[hist], w=[UF])
                        elif b == 0:
                            self.dve.op(lambda e: e.memset(UF.ap[:, :, :, 0:30], 0.0), w=[UF])
                        else:
                            self.dve.op(lambda e: e.tensor_copy(out=UF.ap[:, :, :, 0:30], in_=UFp.ap[:, :, :, BW:BW + 30]),
                                        r=[UFp], w=[UF])
                        for i in range(4):
                            self.dve.op(lambda e: e.tensor_tensor(out=UF.ap[:, i, :, 30:30 + cw], in0=v4(CV)[:, i],
                                                                  in1=v4(CG)[:, i], op=ALU.mult), r=[CV, CG], wm=[UF])
                        self.act.op(lambda e: e.activation(out=UE.ap[:, :, :, :cw + 30], in_=UF.ap[:, :, :, :cw + 30],
                                                           func=AF.Copy), r=[UF], w=[UE])

                    def back(b):
                        (c0, cw, n, CV, CG, UE, UF, CC, SQ, Y, UEp, UFp) = blk_ctx(b)
                        v4 = lambda t: t.ap[:, :, :n].rearrange("p i (s c) -> p i s c", s=nseq)
                        pss = []
                        for i in range(4):
                            ps = self.psb()
                            pss.append(ps)
                            out = ps.ap[:, :n] if nseq == 1 else ps.ap[:, :n].rearrange("p (s c) -> p s c", s=nseq)
                            for j in range(31):
                                rhs = UE.ap[:, i, 0, j:j + cw] if nseq == 1 else UE.ap[:, i, :, j:j + cw]
                                self.pe.op(lambda e: e.matmul(out, lhsT=dg.ap[:, i, j, :], rhs=rhs,
                                                              start=(j == 0), stop=(j == 30)), r=[dg, UE], w=[ps])
                            bcol = pv.ap[:, PV_CCB + i:PV_CCB + i + 1]
                            self.act.op(lambda e: e.activation(out=CC.ap[:, i, :n], in_=ps.ap[:, :n], func=AF.Identity,
                                                               bias=bcol), r=[ps, pv], wm=[CC])
                            self.act.op(lambda e: e.activation(out=SQ.ap[:, i, :n], in_=ps.ap[:, :n], func=AF.Square,
                                                               bias=bcol), r=[ps, pv], wm=[SQ])
                        ps1, ps2 = self.psb(), self.psb()
                        for i in range(4):
                            self.pe.op(lambda e: e.matmul(ps1.ap[:, :n], lhsT=self.ones32.ap[:, :], rhs=CC.ap[:, i, :n],
                                                          start=(i == 0), stop=(i == 3)), r=[self.ones32, CC], w=[ps1])
                        for i in range(4):
                            self.pe.op(lambda e: e.matmul(ps2.ap[:, :n], lhsT=self.ones32.ap[:, :], rhs=SQ.ap[:, i, :n],
                                                          start=(i == 0), stop=(i == 3)), r=[self.ones32, SQ], w=[ps2])
                        self.dve.op(lambda e: e.tensor_scalar(out=mean.ap[:, :n], in0=ps1.ap[:, :n], scalar1=1.0 / 512,
                                                              scalar2=None, op0=ALU.mult), r=[ps1], w=[mean])
                        self.dve.op(lambda e: e.tensor_tensor(out=msq.ap[:, :n], in0=mean.ap[:, :n], in1=mean.ap[:, :n],
                                                              op=ALU.mult), r=[mean], w=[msq])
                        self.dve.op(lambda e: e.scalar_tensor_tensor(out=msq.ap[:, :n], in0=ps2.ap[:, :n], scalar=1.0 / 512,
                                                                     in1=msq.ap[:, :n], op0=ALU.mult, op1=ALU.subtract),
                                    r=[ps2, msq], w=[msq])
                        self.rstd(rs.ap[:, :n], msq.ap[:, :n], 1.0, [msq], rs)
                        for i in range(4):
                            T_ = tt[i % 2]
                            self.dve.op(lambda e: e.tensor_tensor(out=T_.ap[:, :n], in0=CC.ap[:, i, :n], in1=mean.ap[:, :n],
                                                                  op=ALU.subtract), r=[CC, mean], w=[T_])
                            self.dve.op(lambda e: e.tensor_tensor(out=T_.ap[:, :n], in0=T_.ap[:, :n], in1=rs.ap[:, :n],
                                                                  op=ALU.mult), r=[T_, rs], w=[T_])
                            self.act.op(lambda e: e.activation(out=Y.ap[:, i, :n], in_=T_.ap[:, :n], func=AF.Silu,
                                                               scale=pv.ap[:, PV_CLG + i:PV_CLG + i + 1],
                                                               bias=pv.ap[:, PV_CLB + i:PV_CLB + i + 1]),
                                        r=[T_, pv], wm=[Y])
                        self.sp.dma(out=yv[:, :, c0:c0 + n], in_=Y.ap[:, :, :n], S=Y.sem, r=[Y], wm=[self.yT])
                        if b == nblk - 1:
                            for i in range(4):
                                self.dve.op(lambda e: e.tensor_copy(out=stout.ap[:, i, :].rearrange("p (s r) -> p s r", r=30),
                                                                    in_=UF.ap[:, i, :, cw:cw + 30]), r=[UF], wm=[stout])

                    front(0)
                    for b in range(nblk):
                        if b + 1 < nblk:
                            front(b + 1)
                        back(b)
                    dst = (self.o_s["cconv"][l].rearrange("s r c -> (s r) c") if is_s else self.o_p["cconv"][l])
                    self.feat2tok(es, "c_fo", stout, 30 * nseq, GW, dst)
                    self.barrier()

    def mix_attn(self, l):
        TP, NS, TT = self.TP, self.NS, self.TT
        TBIDX = {128: 0, 0: 1, 192: 2, 64: 3}
        with ExitStack() as es0:
            tb = self.sb(es0, "d_tb", [128, 8, 5, 64], F32, dma=True)
            rbr = self.sb(es0, "d_rbr", [128, 8, 257], F32, dma=True)
            bmax = self.sb(es0, "d_bmax", [128, 8], F32)
            self.sp.dma(out=tb.ap[:, :, :, :], in_=self.biasT[l].rearrange("p (h t q) -> p h t q", h=8, t=5), S=tb.sem, w=[tb])
            self.sp.dma(out=rbr.ap[:, :, :], in_=self.rb_rep[l].rearrange("p (h r) -> p h r", h=8), S=rbr.sem, w=[rbr])
            self.dve.op(lambda e: e.scalar_tensor_tensor(out=rbr.ap[:, :, :], in0=rbr.ap[:, :, :], scalar=-1.0, in1=rbr.ap[:, :, :],
                                                         op0=ALU.mult, op1=ALU.max), r=[rbr], w=[rbr])
            self.dve.op(lambda e: e.tensor_reduce(out=bmax.ap[:, :], in_=rbr.ap[:, :, :], axis=AX.X, op=ALU.max),
                        r=[rbr], w=[bmax])
            if TP >= 512:
                self.sp.dma(out=self.o_p["k"][l], in_=self.ktok.ap[TP - 512:TP, :], S=self.scr_sem, r=[self.ktok])
                self.sp.dma(out=self.o_p["v"][l], in_=self.vtok_a.ap[TP - 512:TP, :], S=self.scr_sem, r=[self.vtok_a])
            self.sp.dma(out=self.o_s["k"][l].rearrange("s r c -> (s r) c"), in_=self.ktok.ap[TP:TT, :], S=self.scr_sem,
                        r=[self.ktok])
            self.sp.dma(out=self.o_s["v"][l].rearrange("s r c -> (s r) c"), in_=self.vtok_a.ap[TP:TT, :], S=self.scr_sem,
                        r=[self.vtok_a])
            seqs = [(False, 0, TP, 0, 64, 0)] + [(True, s, 16, 512, 16, TP + 16 * s) for s in range(NS)]
            import os
            self.alv = int(os.environ.get('DBG_AT', '9'))
            if self.alv < 1:
                self.barrier()
                return
            self._attn_seq(l, tb, bmax, *seqs[0], TBIDX)
            with ExitStack() as es_s:
                self._reuse, self._reuse_es = {}, es_s
                for sq_ in seqs[1:]:
                    self._attn_seq(l, tb, bmax, *sq_, TBIDX)
                self._reuse = None
                self.barrier()

    def _attn_seq(self, l, tb, bmax, is_s, sidx, T, Lc, CL, col0, TBIDX):
        import os
        NK = Lc + T
        NKT = (NK + 127) // 128
        zq = self.zT.ap[O_DQ:O_DQ + 512, :].rearrange("(i p) t -> p i t", p=128)
        zk = self.zT.ap[O_DK:O_DK + 512, :].rearrange("(i p) t -> p i t", p=128)
        yv = self.yT.ap[1536:2048, :].rearrange("(i p) t -> p i t", p=128)
        with ExitStack() as es:
            QT = self.sb(es, "d_qt", [128, 4, T], BF16)
            KT = self.sb(es, "d_kt", [128, 4, NK], BF16)
            VA = self.sb(es, "d_va", [128, NKT, 8, 65], BF16)
            ld = [self.sb(es, f"d_ld{i}", [128, 4, 512], F32, dma=True) for i in range(2)]
            sqb = [self.sb(es, f"d_sq{i}", [128, 4, 512], BF16) for i in range(2)]
            nb_k = (NK + 511) // 512 + 1
            nb_q = (T + 511) // 512
            kmx = self.sb(es, "d_kmx", [128, 8, nb_k], F32)
            qmx = self.sb(es, "d_qmx", [128, 8, nb_q], F32)
            k2 = self.sb(es, "d_k2", [128, 8], F32)
            q2 = self.sb(es, "d_q2", [128, 8], F32)
            negM = self.sb(es, "d_negm", [128, 8], F32)
            cM = self.sb(es, "d_cm", [128, 8], F32)
            nld = [0]

            def stat_block(L, ncols, mx, blk):
                SQ = sqb[nld[0] % 2]
                self.act.op(lambda e: e.activation(out=SQ.ap[:, :, :ncols], in_=L.ap[:, :, :ncols], func=AF.Square),
                            r=[L], w=[SQ])
                for pr in range(4):
                    for hb, sel in ((0, self.selA), (1, self.selB)):
                        ps = self.psb()
                        self.pe.op(lambda e: e.matmul(ps.ap[:, :ncols], lhsT=sel.ap[:, :], rhs=SQ.ap[:, pr, :ncols],
                                                      start=True, stop=True), r=[sel, SQ], w=[ps])
                        h = pr * 2 + hb
                        self.dve.op(lambda e: e.tensor_reduce(out=mx.ap[:, h, blk:blk + 1], in_=ps.ap[:, :ncols],
                                                              axis=AX.X, op=ALU.max), r=[ps], wm=[mx])

            kb = 0
            if is_s:
                with ExitStack() as es2:
                    kc = self.sb(es2, "d_kc", [128, 4, 512], F32, dma=True)
                    kf = self.sb(es2, "d_kf", [128, 4, 512], F32)
                    self.sp.dma(out=kc.ap[:, :, :], in_=self.st_k[l, sidx].rearrange("(t p) c -> p t c", p=128), S=kc.sem, w=[kc])
                    for pr in range(4):
                        ps = self.psb()
                        for t in range(4):
                            self.pe.op(lambda e: e.transpose(out=ps.ap[:, t * 128:(t + 1) * 128],
                                                             in_=kc.ap[:, t, pr * 128:(pr + 1) * 128],
                                                             identity=self.ident.ap[:, :]), r=[kc, self.ident], w=[ps])
                        self.copy(self.evac_eng(), kf.ap[:, pr, :], ps.ap[:, :], r=[ps], wm=[kf])
                    self.copy(self.dve, KT.ap[:, :, 0:512], kf.ap[:, :, :], r=[kf], wm=[KT])
                    nld[0] += 1
                    stat_block(kf, 512, kmx, kb)
                    kb += 1
                    self.barrier()
            for (c0, cw) in split_even(T, 512):
                L = ld[nld[0] % 2]
                nld[0] += 1
                self.sp.dma(out=L.ap[:, :, :cw], in_=zk[:, :, col0 + c0:col0 + c0 + cw], S=L.sem, r=[self.zT], w=[L])
                self.copy(self.dve, KT.ap[:, :, Lc + c0:Lc + c0 + cw], L.ap[:, :, :cw], r=[L], wm=[KT])
                stat_block(L, cw, kmx, kb)
                kb += 1
            qb_ = 0
            for (c0, cw) in split_even(T, 512):
                L = ld[nld[0] % 2]
                nld[0] += 1
                self.sp.dma(out=L.ap[:, :, :cw], in_=zq[:, :, col0 + c0:col0 + c0 + cw], S=L.sem, r=[self.zT], w=[L])
                self.act.op(lambda e: e.activation(out=L.ap[:, :, :cw], in_=L.ap[:, :, :cw], func=AF.Copy, scale=0.125),
                            r=[L], w=[L])
                self.copy(self.dve, QT.ap[:, :, c0:c0 + cw], L.ap[:, :, :cw], r=[L], wm=[QT])
                stat_block(L, cw, qmx, qb_)
                qb_ += 1
            self.dve.op(lambda e: e.tensor_reduce(out=k2.ap[:, :], in_=kmx.ap[:, :, :kb], axis=AX.X, op=ALU.max), r=[kmx], w=[k2])
            self.dve.op(lambda e: e.tensor_reduce(out=q2.ap[:, :], in_=qmx.ap[:, :, :qb_], axis=AX.X, op=ALU.max), r=[qmx], w=[q2])
            self.dve.op(lambda e: e.tensor_tensor(out=k2.ap[:, :], in0=k2.ap[:, :], in1=q2.ap[:, :], op=ALU.mult), r=[k2, q2], w=[k2])
            self.act.op(lambda e: e.activation(out=k2.ap[:, :], in_=k2.ap[:, :], func=AF.Sqrt), r=[k2], w=[k2])
            self.dve.op(lambda e: e.scalar_tensor_tensor(out=negM.ap[:, :], in0=k2.ap[:, :], scalar=-1.0, in1=bmax.ap[:, :],
                                                         op0=ALU.mult, op1=ALU.subtract), r=[k2, bmax], w=[negM])
            self.dve.op(lambda e: e.tensor_tensor(out=cM.ap[:, :], in0=tb.ap[:, :, 4, 0], in1=negM.ap[:, :], op=ALU.add),
                        r=[tb, negM], w=[cM])
            self.pool.op(lambda e: e.memset(VA.ap[:, :, :, 64:65], 1.0), wm=[VA])
            vsrcs = []
            if is_s:
                vsrcs.append((self.st_v[l, sidx], 512, 0, None))
            vsrcs.append((self.vtok_a.ap[col0:col0 + T, :], T, Lc // 128, self.vtok_a))
            vl = [self.sb(es, f"d_vl{i}", [128, 4, 512], F32, dma=True) for i in range(2)]
            nv = 0
            for (vap, rows, t0, dep) in vsrcs:
                for r0 in range(0, rows, 512):
                    rn = min(512, rows - r0)
                    V = vl[nv % 2]
                    nv += 1
                    rd = [dep] if dep is not None else []
                    if rn >= 128:
                        nt = rn // 128
                        self.sp.dma(out=V.ap[:, :nt, :], in_=vap[r0:r0 + rn, :].rearrange("(t p) c -> p t c", p=128),
                                    S=V.sem, r=rd, w=[V])
                        self.copy(self.act, VA.ap[:, t0 + r0 // 128:t0 + r0 // 128 + nt, :, 0:64],
                                  V.ap[:, :nt, :].rearrange("p t (h d) -> p t h d", h=8), r=[V], wm=[VA])
                    else:
                        self.sp.dma(out=V.ap[:rn, 0, :], in_=vap[r0:r0 + rn, :], S=V.sem, r=rd, w=[V])
                        self.copy(self.act, VA.ap[:rn, t0 + r0 // 128, :, 0:64],
                                  V.ap[:rn, 0, :].rearrange("p (h d) -> p h d", h=8), r=[V], wm=[VA])
            NB = 4
            ET = [self.sb(es, f"d_et{i}", [128, 5 * CL], BF16) for i in range(NB)]
            TM = [self.sb(es, f"d_tm{i}", [128, 2 * CL], F32) for i in range(NB)]
            rden = [self.sb(es, f"d_rd{i}", [128, 8], F32) for i in range(2)]
            otok = [self.sb(es, f"d_ot{i}", [128, 8, 64], F32) for i in range(2)]
            GB = 512 // CL if not is_s else 1
            yst = [self.sb(es, f"d_ys{i}", [128, 4, GB * CL], BF16, dma=True) for i in range(2)]
            pso = [self.ps[6], self.ps[7]]
            nchunk = T // CL

            def chunk_tiles(c):
                qs = Lc + c * CL
                kmin = max(0, qs - 512)
                kend = qs + CL
                tiles = []
                for kt in range(kmin // 128, (kend - 1) // 128 + 1):
                    p0 = max(kmin, kt * 128) - kt * 128
                    p1 = min(kend, kt * 128 + 128) - kt * 128
                    tiles.append((kt, p0, p1, qs - kt * 128))
                return tiles, sum(1 for t in tiles if t[3] >= 256)

            def qk(c, h):
                tiles, nconst = chunk_tiles(c)
                pr, hb = h // 2, (h % 2) * 64
                ps = self.psb()
                for si, (kt, p0, p1, d0) in enumerate(tiles):
                    mend = p1 if p0 == 0 else 128
                    self.pe.op(lambda e: e.matmul(ps.ap[:mend, si * CL:(si + 1) * CL],
                                                  lhsT=KT.ap[hb:hb + 64, pr, kt * 128:kt * 128 + mend],
                                                  rhs=QT.ap[hb:hb + 64, pr, c * CL:(c + 1) * CL],
                                                  start=True, stop=True), r=[KT, QT], w=[ps])
                return ps

            def softmax_pv(c, h, ps, E, Tm):
                tiles, nconst = chunk_tiles(c)
                if nconst:
                    self.act.op(lambda e: e.activation(out=E.ap[:, :nconst * CL], in_=ps.ap[:, :nconst * CL], func=AF.Exp,
                                                       bias=cM.ap[:, h:h + 1]), r=[ps, cM], w=[E])
                if tiles[0][1] == 64:
                    self.pool.op(lambda e: e.memset(E.ap[0:64, 0:CL], 0.0), w=[E])
                for si, (kt, p0, p1, d0) in enumerate(tiles):
                    if d0 >= 256:
                        continue
                    j = si - nconst
                    mend = p1 if p0 == 0 else 128
                    self.dve.op(lambda e: e.tensor_tensor(out=Tm.ap[:mend, j * CL:(j + 1) * CL], in0=ps.ap[:mend, si * CL:(si + 1) * CL],
                                                          in1=tb.ap[:mend, h, TBIDX[d0], 0:CL], op=ALU.add),
                                r=[ps, tb], wm=[Tm])
                    self.act.op(lambda e: e.activation(out=E.ap[:mend, si * CL:(si + 1) * CL], in_=Tm.ap[:mend, j * CL:(j + 1) * CL],
                                                       func=AF.Exp, bias=negM.ap[:mend, h:h + 1]), r=[Tm, negM], wm=[E])
                po = pso[h // 4]
                hh = h % 4
                for si, (kt, p0, p1, d0) in enumerate(tiles):
                    if p0 == 64:
                        p0 = 0
                    self.pe.op(lambda e: e.matmul(po.ap[:CL, hh * 65:(hh + 1) * 65], lhsT=E.ap[p0:p1, si * CL:(si + 1) * CL],
                                                  rhs=VA.ap[p0:p1, kt, h, :], start=(si == 0), stop=(si == len(tiles) - 1)),
                               r=[E, VA], w=[po])

            def finalize(c):
                RD, OT = rden[c % 2], otok[c % 2]
                for g in range(2):
                    pov = pso[g].ap[:CL, :260].rearrange("p (h d) -> p h d", d=65)
                    self.dve.op(lambda e: e.reciprocal(out=RD.ap[:CL, g * 4:(g + 1) * 4], in_=pov[:, :, 64]), r=[pso[g]], wm=[RD])
                    self.dve.op(lambda e: e.tensor_tensor(out=OT.ap[:CL, g * 4:(g + 1) * 4, :], in0=pov[:, :, 0:64],
                                                          in1=RD.ap[:CL, g * 4:(g + 1) * 4].unsqueeze(2).to_broadcast([CL, 4, 64]),
                                                          op=ALU.mult), r=[pso[g], RD], wm=[OT])
                Y = yst[(c // GB) % 2]
                pst = self.psb()
                otv = OT.ap[:CL, :, :].rearrange("p h d -> p (h d)")
                for i in range(4):
                    self.pe.op(lambda e: e.transpose(out=pst.ap[:, i * CL:(i + 1) * CL], in_=otv[:, i * 128:(i + 1) * 128],
                                                     identity=self.ident.ap[:CL, :CL]), r=[OT, self.ident], w=[pst])
                cc_ = (c % GB) * CL
                self.copy(self.act, Y.ap[:, :, cc_:cc_ + CL], pst.ap[:, :4 * CL].rearrange("p (i c) -> p i c", i=4),
                          r=[pst], wm=[Y])
                if (c % GB) == GB - 1 or c == nchunk - 1:
                    g0 = (c // GB) * GB * CL
                    gw = (c % GB + 1) * CL
                    self.sp.dma(out=yv[:, :, col0 + g0:col0 + g0 + gw], in_=Y.ap[:, :, :gw], S=Y.sem, r=[Y], wm=[self.yT])

            SKEW = 2
            items = [(c, h) for c in range(nchunk) for h in range(8)]
            pend = []
            for i, (c, h) in enumerate(items):
                pend.append((c, h, qk(c, h), ET[i % NB], TM[i % NB]))
                if len(pend) > SKEW:
                    it = pend.pop(0)
                    softmax_pv(*it)
                    if it[1] == 7:
                        finalize(it[0])
            while pend:
                it = pend.pop(0)
                softmax_pv(*it)
                if it[1] == 7:
                    finalize(it[0])
            self.barrier()

    def mix_gla(self, l):
        TP, NS = self.TP, self.NS
        with ExitStack() as es0:
            w2f = self.sb(es0, "b_w2f", [16, 256], F32, dma=True)
            w2 = self.sb(es0, "b_w2", [16, 256], BF16)
            negb = self.sb(es0, "b_negb", [64, 4], F32)
            self.sp.dma(out=w2f.ap[:, :], in_=self.gla_w2[l], S=w2f.sem, w=[w2f])
            self.copy(self.dve, w2.ap[:, :], w2f.ap[:, :], r=[w2f], w=[w2])
            self.dve.op(lambda e: e.tensor_scalar(out=negb.ap[:, :], in0=self.pv.ap[:64, PV_GLAB4:PV_GLAB4 + 4], scalar1=-1.0,
                                                  scalar2=None, op0=ALU.mult), r=[self.pv], w=[negb])
            seqs = [(False, 0, TP, 64, 0)] + [(True, s, 16, 16, TP + 16 * s) for s in range(NS)]
            self._gla_seq(l, w2, negb, *seqs[0])
            with ExitStack() as es_s:
                self._reuse, self._reuse_es = {}, es_s
                for sq_ in seqs[1:]:
                    self._gla_seq(l, w2, negb, *sq_)
                self._reuse = None
                self.barrier()

    def _gla_seq(self, l, w2, negb, is_s, sidx, T, L, col0):
        pv = self.pv
        zT = self.zT.ap
        zq = zT[O_Q:O_Q + 256, :].rearrange("(h p) t -> p h t", p=64)
        zk = zT[O_K:O_K + 256, :].rearrange("(h p) t -> p h t", p=64)
        zg = zT[O_G:O_G + 512, :].rearrange("(i p) t -> p i t", p=128)
        yv = self.yT.ap[512:1024, :].rearrange("(i p) t -> p i t", p=128)
        BW = min(T, 512)
        CPB = BW // L
        bc = lambda ap, shape: ap.unsqueeze(2).to_broadcast(shape)
        with ExitStack() as es:
            S = self.sb(es, "b_S", [64, 4, 128], F32, dma=True)
            Sb = [self.sb(es, f"b_Sb{i}", [64, 4, 128], BF16) for i in range(2)]
            if is_s:
                self.sp.dma(out=S.ap[:, :, :], in_=self.st_gla[l, sidx].rearrange("(h p) v -> p h v", p=64), S=S.sem, w=[S])
            else:
                self.dve.op(lambda e: e.memset(S.ap[:, :, :], 0.0), w=[S])
            self.copy(self.act, Sb[0].ap[:, :, :], S.ap[:, :, :], r=[S], w=[Sb[0]])
            nS = 0
            rmask = self.sb(es, "b_rm", [64, BW], F32)
            self.dve.op(lambda e: e.memset(rmask.ap[:, :], 1.0), w=[rmask])
            self.dve.op(lambda e: e.memset(rmask.ap[:, :].rearrange("p (c t) -> p c t", t=L)[:, :, 0:1], 0.0), w=[rmask])
            rT = self.sb(es, "b_rT", [16, BW], F32, dma=True)
            rTb = self.sb(es, "b_rTb", [16, BW], BF16)
            qf = self.sb(es, "b_qf", [64, 4, BW], F32, dma=True)
            kf = self.sb(es, "b_kf", [64, 4, BW], F32, dma=True)
            gf = self.sb(es, "b_gf", [128, 4, BW], F32, dma=True)
            vf = self.sb(es, "b_vf", [64, CPB, 512], F32, dma=True)
            vb = self.sb(es, "b_vb", [64, CPB, 512], BF16)
            yy = self.sb(es, "b_y", [64, BW], F32)
            ay = self.sb(es, "b_ay", [64, BW], F32)
            la = self.sb(es, "b_la", [64, 4, BW], F32)
            bb = self.sb(es, "b_b", [64, 4, BW], F32)
            e3 = self.sb(es, "b_e3", [64, 4, BW], F32)
            ei = self.sb(es, "b_ei", [64, 4, BW], F32)
            qb = self.sb(es, "b_qb", [64, 4, BW], BF16)
            kinv = self.sb(es, "b_kinv", [64, 4, BW], F32)
            qd = [self.sb(es, f"b_qd{i}", [64, 4, L], BF16) for i in range(2)]
            kd = [self.sb(es, f"b_kd{i}", [64, 4, L], BF16) for i in range(2)]
            kl = [self.sb(es, f"b_kl{i}", [64, 4, L], F32) for i in range(2)]
            klt = [self.sb(es, f"b_klt{i}", [64, 256], BF16) for i in range(2)]
            am = [self.sb(es, f"b_am{i}", [64, 4, L], BF16) for i in range(2)]
            oT = self.sb(es, "b_oT", [128, 4, BW], F32)
            osq = self.sb(es, "b_osq", [128, 4, BW], F32)
            rs = self.sb(es, "b_rs", [128, BW], F32)
            ot = self.sb(es, "b_ot", [128, BW], F32)
            yo = [self.sb(es, f"b_yo{i}", [128, 4, BW], BF16, dma=True) for i in range(2)]
            psu = self.ps[6]
            for bi in range((T + BW - 1) // BW):
                c0 = col0 + bi * BW
                self.sp.dma(out=rT.ap[:, :], in_=zT[O_R:O_R + 16, c0:c0 + BW], S=rT.sem, r=[self.zT], w=[rT])
                self.sp.dma(out=qf.ap[:, :, :], in_=zq[:, :, c0:c0 + BW], S=qf.sem, r=[self.zT], w=[qf])
                self.sp.dma(out=kf.ap[:, :, :], in_=zk[:, :, c0:c0 + BW], S=kf.sem, r=[self.zT], w=[kf])
                self.sp.dma(out=gf.ap[:, :, :], in_=zg[:, :, c0:c0 + BW], S=gf.sem, r=[self.zT], w=[gf])
                self.sp.dma(out=vf.ap[:L, :, :], in_=self.vtok_g.ap[c0:c0 + BW, :].rearrange("(c p) v -> p c v", p=L),
                            S=vf.sem, r=[self.vtok_g], w=[vf])
                self.copy(self.act, vb.ap[:L, :, :], vf.ap[:L, :, :], r=[vf], w=[vb])
                self.copy(self.dve, rTb.ap[:, :], rT.ap[:, :], r=[rT], w=[rTb])
                for h in range(4):
                    ps = self.psb()
                    self.pe.op(lambda e: e.matmul(ps.ap[:64, :BW], lhsT=w2.ap[:, h * 64:(h + 1) * 64], rhs=rTb.ap[:, :],
                                                  start=True, stop=True), r=[w2, rTb], w=[ps])
                    self.dve.op(lambda e: e.tensor_scalar(out=yy.ap[:, :], in0=ps.ap[:64, :BW], scalar1=-1.0,
                                                          scalar2=negb.ap[:, h:h + 1], op0=ALU.mult, op1=ALU.add),
                                r=[ps, negb], w=[yy])
                    self.dve.op(lambda e: e.scalar_tensor_tensor(out=ay.ap[:, :], in0=yy.ap[:, :], scalar=-1.0, in1=yy.ap[:, :],
                                                                 op0=ALU.mult, op1=ALU.max), r=[yy], w=[ay])
                    self.act.op(lambda e: e.activation(out=ay.ap[:, :], in_=ay.ap[:, :], func=AF.Exp, scale=-1.0), r=[ay], w=[ay])
                    self.act.op(lambda e: e.activation(out=ay.ap[:, :], in_=ay.ap[:, :], func=AF.Ln, bias=self.onec.ap[:64, :]),
                                r=[ay, self.onec], w=[ay])
                    self.dve.op(lambda e: e.scalar_tensor_tensor(out=yy.ap[:, :], in0=yy.ap[:, :], scalar=0.0, in1=ay.ap[:, :],
                                                                 op0=ALU.max, op1=ALU.add), r=[yy, ay], w=[yy])
                    self.dve.op(lambda e: e.tensor_scalar(out=la.ap[:, h, :], in0=yy.ap[:, :], scalar1=-1.0 / 16.0, scalar2=None,
                                                          op0=ALU.mult), r=[yy], wm=[la])
                    self.dve.op(lambda e: e.tensor_tensor_scan(out=bb.ap[:, h, :], data0=rmask.ap[:, :], data1=la.ap[:, h, :],
                                                               initial=0.0, op0=ALU.mult, op1=ALU.add), r=[rmask, la], wm=[bb])
                self.act.op(lambda e: e.activation(out=e3.ap[:, :, :], in_=bb.ap[:, :, :], func=AF.Exp), r=[bb], w=[e3])
                self.act.op(lambda e: e.activation(out=ei.ap[:, :, :], in_=bb.ap[:, :, :], func=AF.Exp, scale=-1.0), r=[bb], w=[ei])
                self.dve.op(lambda e: e.scalar_tensor_tensor(out=qf.ap[:, :, :], in0=qf.ap[:, :, :], scalar=0.125, in1=e3.ap[:, :, :],
                                                             op0=ALU.mult, op1=ALU.mult), r=[qf, e3], w=[qf])
                self.copy(self.act, qb.ap[:, :, :], qf.ap[:, :, :], r=[qf], w=[qb])
                self.dve.op(lambda e: e.tensor_tensor(out=kinv.ap[:, :, :], in0=kf.ap[:, :, :], in1=ei.ap[:, :, :], op=ALU.mult),
                            r=[kf, ei], w=[kinv])
                def stage_a(c):
                    t0, t1, mid = c * L, (c + 1) * L, c * L + L // 2
                    QD, KD, KL, KLT, AM = (x[c % 2] for x in (qd, kd, kl, klt, am))
                    self.dve.op(lambda e: e.tensor_tensor(out=QD.ap[:, :, :], in0=qf.ap[:, :, t0:t1], in1=bc(ei.ap[:, :, mid], [64, 4, L]),
                                                          op=ALU.mult), r=[qf, ei], w=[QD])
                    self.dve.op(lambda e: e.tensor_tensor(out=KD.ap[:, :, :], in0=kinv.ap[:, :, t0:t1], in1=bc(e3.ap[:, :, mid], [64, 4, L]),
                                                          op=ALU.mult), r=[kinv, e3], w=[KD])
                    self.dve.op(lambda e: e.tensor_tensor(out=KL.ap[:, :, :], in0=kinv.ap[:, :, t0:t1], in1=bc(e3.ap[:, :, t1 - 1], [64, 4, L]),
                                                          op=ALU.mult), r=[kinv, e3], w=[KL])
                    pst = self.psb()
                    for h in range(4):
                        self.pe.op(lambda e: e.transpose(out=pst.ap[:L, h * 64:(h + 1) * 64], in_=KL.ap[:, h, :],
                                                         identity=self.ident.ap[:64, :64]), r=[KL, self.ident], w=[pst])
                    self.copy(self.act, KLT.ap[:L, :], pst.ap[:L, :256], r=[pst], w=[KLT])
                    psa = self.psb()
                    for h in range(4):
                        self.pe.op(lambda e: e.matmul(psa.ap[:L, h * L:(h + 1) * L], lhsT=KD.ap[:, h, :], rhs=QD.ap[:, h, :],
                                                      start=True, stop=True), r=[KD, QD], w=[psa])
                    self.dve.op(lambda e: e.tensor_tensor(out=AM.ap[:L, :, :], in0=psa.ap[:L, :4 * L].rearrange("p (h t) -> p h t", h=4),
                                                          in1=self.trimask.ap[:L, :L].unsqueeze(1).to_broadcast([L, 4, L]), op=ALU.mult),
                                r=[psa, self.trimask], w=[AM])

                def stage_b(c, nS):
                    t0, t1 = c * L, (c + 1) * L
                    KLT, AM = klt[c % 2], am[c % 2]
                    pso = self.psb()
                    SB = Sb[nS % 2]
                    for h in range(4):
                        self.pe.op(lambda e: e.matmul(pso.ap[:, h * L:(h + 1) * L], lhsT=vb.ap[:L, c, h * 128:(h + 1) * 128], rhs=AM.ap[:L, h, :],
                                                      start=True, stop=False), r=[vb, AM], w=[pso])
                        self.pe.op(lambda e: e.matmul(pso.ap[:, h * L:(h + 1) * L], lhsT=SB.ap[:, h, :], rhs=qb.ap[:, h, t0:t1],
                                                      start=False, stop=True), r=[SB, qb], w=[pso])
                    self.copy(self.act, oT.ap[:, :, t0:t1], pso.ap[:, :4 * L].rearrange("p (h t) -> p h t", h=4), r=[pso], wm=[oT])
                    for h in range(4):
                        self.pe.op(lambda e: e.matmul(psu.ap[:64, h * 128:(h + 1) * 128], lhsT=KLT.ap[:L, h * 64:(h + 1) * 64],
                                                      rhs=vb.ap[:L, c, h * 128:(h + 1) * 128], start=True, stop=True), r=[KLT, vb], w=[psu])
                    self.dve.op(lambda e: e.tensor_tensor(out=S.ap[:, :, :], in0=S.ap[:, :, :], in1=bc(e3.ap[:, :, t1 - 1], [64, 4, 128]),
                                                          op=ALU.mult), r=[S, e3], w=[S])
                    self.dve.op(lambda e: e.tensor_tensor(out=S.ap[:, :, :], in0=S.ap[:, :, :],
                                                          in1=psu.ap[:64, :].rearrange("p (h v) -> p h v", h=4), op=ALU.add),
                                r=[S, psu], w=[S])
                    self.copy(self.act, Sb[(nS + 1) % 2].ap[:, :, :], S.ap[:, :, :], r=[S], w=[Sb[(nS + 1) % 2]])

                stage_a(0)
                for c in range(CPB):
                    if c + 1 < CPB:
                        stage_a(c + 1)
                    stage_b(c, nS)
                    nS += 1
                self.act.op(lambda e: e.activation(out=osq.ap[:, :, :], in_=oT.ap[:, :, :], func=AF.Square), r=[oT], w=[osq])
                Y = yo[bi % 2]
                for h in range(4):
                    ps = self.psb()
                    self.pe.op(lambda e: e.matmul(ps.ap[:, :BW], lhsT=self.ones32.ap[:, :], rhs=osq.ap[:, h, :], start=True, stop=True),
                               r=[self.ones32, osq], w=[ps])
                    self.rstd(rs.ap[:, :], ps.ap[:, :BW], 1.0 / 128, [ps], rs)
                    self.dve.op(lambda e: e.tensor_tensor(out=ot.ap[:, :], in0=oT.ap[:, h, :], in1=rs.ap[:, :], op=ALU.mult),
                                r=[oT, rs], w=[ot])
                    self.act.op(lambda e: e.activation(out=gf.ap[:, h, :], in_=gf.ap[:, h, :], func=AF.Silu), r=[gf], w=[gf])
                    self.dve.op(lambda e: e.scalar_tensor_tensor(out=Y.ap[:, h, :], in0=ot.ap[:, :], scalar=pv.ap[:, PV_GLAG:PV_GLAG + 1],
                                                                 in1=gf.ap[:, h, :], op0=ALU.mult, op1=ALU.mult), r=[ot, pv, gf], wm=[Y])
                self.sp.dma(out=yv[:, :, c0:c0 + BW], in_=Y.ap[:, :, :], S=Y.sem, r=[Y], wm=[self.yT])
            dst = self.o_s["gla"][l, sidx] if is_s else self.o_p["gla"][l]
            self.sp.dma(out=dst.rearrange("(h p) v -> p h v", p=64), in_=S.ap[:, :, :], S=S.sem, r=[S])
            self.barrier()

    def phase_resproj(self, src, KT, W, CW, BWID, dbl=False, src_r0=0):
        blocks = [([(W, c0, BWID)], "f") for c0 in range(0, D, BWID)]
        with ExitStack() as es:
            xr = [self.sb(es, f"r_x{i}", [128, CW], F32, dma=True) for i in range(2)]
            cnt = [0]

            def handler(tag, parts, wt, xin, s0, sw):
                (_, c0, wd) = parts[0]
                for m0 in range(0, wd, 128):
                    kt = (c0 + m0) // 128
                    X = xr[cnt[0] % 2]
                    cnt[0] += 1
                    xt = self.xTt[kt]
                    self.sp.dma(out=X.ap[:, :sw], in_=self.xT.ap[kt * 128:(kt + 1) * 128, s0:s0 + sw], S=X.sem, r=[xt], w=[X])
                    for (n0, nw) in split_even(sw, 512):
                        ps = self.mm_f(wt, m0, 128, xin, n0, nw, KT)
                        self.dve.op(lambda e: e.tensor_tensor(out=X.ap[:, n0:n0 + nw], in0=ps.ap[:, :nw], in1=X.ap[:, n0:n0 + nw],
                                                              op=ALU.add), r=[ps, X], w=[X])
                    self.sp.dma(out=self.xT.ap[kt * 128:(kt + 1) * 128, s0:s0 + sw], in_=X.ap[:, :sw], S=X.sem, r=[X], wm=[xt])

            self.linear(es, src, KT, blocks, CW, BWID, handler, dbl=dbl, src_r0=src_r0)
            self.barrier()

    def phase_ffn_hidden(self, l):
        TP, NS, TT = self.TP, self.NS, self.TT
        pv = self.pv
        CW = 2080
        Wg, Wu = self.w_gate[l], self.w_up[l]
        blocks = [([(Wg, c0, 256), (Wu, c0, 256)], "g") for c0 in range(0, DFF, 256)]
        seqs = self.seq_ranges()
        with ExitStack() as es:
            ghp = self.sb(es, "f_ghp", [128, 44, 2], F32)
            self.dve.op(lambda e: e.memset(ghp.ap[:, :, :], 0.0), w=[ghp])
            ghs = self.sb(es, "f_ghs", [128, 44, 2 * NS], F32)
            with ExitStack() as es2:
                t = self.tok2feat(es2, "f_hi", self.st_ffn[l].rearrange("s r c -> (s r) c"), 2 * NS, DFF)
                self.copy(self.dve, ghs.ap[:, :, :], t.ap[:, :, :], r=[t], w=[ghs])
                self.barrier()
            with ExitStack() as es3:
                WX = CW + 2 * (1 + NS)
                ge = [self.sb(es3, f"f_ge{i}", [128, WX], F32) for i in range(2)]
                ub = [self.sb(es3, f"f_ub{i}", [128, WX], F32) for i in range(2)]
                cb = [self.sb(es3, f"f_cb{i}", [128, WX], F32) for i in range(2)]
                ab = [self.sb(es3, f"f_ab{i}", [128, WX], BF16, dma=True) for i in range(2)]
                for u_ in ub:
                    self.pool.op(lambda e: e.memset(u_.ap[:, :], 0.0), w=[u_])
                cnt = [0]

                def hist_ap(j, q):
                    return ghp.ap[:, j, :] if q == 0 else ghs.ap[:, j, 2 * (q - 1):2 * q]

                import os
                flv = int(os.environ.get('DBG_FF', '9'))

                def handler(tag, parts, wt, xin, s0, sw):
                    c0 = parts[0][1]
                    if flv < 2:
                        return
                    segs = []
                    o = 0
                    for q, (a0, ln) in enumerate(seqs):
                        lo, hi = max(a0, s0), min(a0 + ln, s0 + sw)
                        if lo < hi:
                            segs.append((q, lo - s0, hi - lo, o))
                            o += hi - lo + 2
                    Wc = o - 2
                    for m in range(2):
                        j = (c0 + m * 128) // 128
                        G, U, C_, A = (x[cnt[0] % 2] for x in (ge, ub, cb, ab))
                        cnt[0] += 1
                        hq = hist_ap
                        for (q, a, ln, og) in segs:
                            self.dve.op(lambda e: e.tensor_copy(out=G.ap[:, og:og + 2], in_=hq(j, q)),
                                        r=[ghp if q == 0 else ghs], wm=[G])
                        f2 = int(os.environ.get('DBG_FF2', '9'))
                        for which, dstt, off in ((0, G, 2), (1, U, 0)):
                            if f2 < 2:
                                break
                            for (n0, nw) in split_even(sw, 512):
                                ps = self.mm_f(wt, which * 256 + m * 128, 128, xin, n0, nw, 16)
                                if f2 < 3:
                                    continue
                                for (q, a, ln, og) in segs:
                                    lo, hi = max(n0, a), min(n0 + nw, a + ln)
                                    if lo < hi:
                                        self.copy(self.evac_eng(), dstt.ap[:, og + off + lo - a:og + off + hi - a], ps.ap[:, lo - n0:hi - n0],
                                                  r=[ps], wm=[dstt])
                        if flv < 3:
                            continue
                        wc = PV_FCW + j * 3
                        self.dve.op(lambda e: e.tensor_scalar(out=C_.ap[:, :Wc], in0=G.ap[:, 0:Wc], scalar1=pv.ap[:, wc:wc + 1], scalar2=None,
                                                              op0=ALU.mult), r=[G, pv], w=[C_])
                        for t_ in (1, 2):
                            self.dve.op(lambda e: e.scalar_tensor_tensor(out=C_.ap[:, :Wc], in0=G.ap[:, t_:t_ + Wc], scalar=pv.ap[:, wc + t_:wc + t_ + 1],
                                                                         in1=C_.ap[:, :Wc], op0=ALU.mult, op1=ALU.add), r=[G, pv, C_], w=[C_])
                        self.act.op(lambda e: e.activation(out=C_.ap[:, :Wc], in_=C_.ap[:, :Wc], func=AF.Silu), r=[C_], w=[C_])
                        self.dve.op(lambda e: e.tensor_tensor(out=A.ap[:, :Wc], in0=C_.ap[:, :Wc], in1=U.ap[:, :Wc], op=ALU.mult),
                                    r=[C_, U], w=[A])
                        if flv < 4:
                            continue
                        for (q, a, ln, og) in segs:
                            self.sp.dma(out=self.aT.ap[j * 128:(j + 1) * 128, s0 + a:s0 + a + ln], in_=A.ap[:, og:og + ln], S=A.sem,
                                        r=[A], wm=[self.aT])
                            self.dve.op(lambda e: e.tensor_copy(out=hq(j, q), in_=G.ap[:, og + ln:og + ln + 2]), r=[G],
                                        wm=[ghp if q == 0 else ghs])

                self.linear(es3, self.hT, 16, blocks, CW, 512, handler)
                self.barrier()
            if flv < 5:
                self.barrier()
                return
            self.feat2tok(es, "f_op", ghp, 2, DFF, self.o_p["ffn"][l])
            self.feat2tok(es, "f_os", ghs, 2 * NS, DFF, self.o_s["ffn"][l].rearrange("s r c -> (s r) c"))
            self.barrier()

    def build(self):
        self.setup()
        self.phase_transpose_in()
        for l in range(self.depth):
            self.load_pvec(l)
            self.phase_norm(PV_GMIX)
            self.phase_inproj(l)
            self.mix_short(l)
            self.mix_gla(l)
            self.mix_cconv(l)
            self.mix_attn(l)
            self.phase_resproj(self.yT, 16, self.w_out[l], 2080, 512, dbl=True)
            self.phase_norm(PV_GFFN)
            self.phase_ffn_hidden(l)
            self.phase_resproj(self.aT, 22, self.w_down[l][0:2816, :], 2080, 512, src_r0=0)
            self.phase_resproj(self.aT, 22, self.w_down[l][2816:5632, :], 2080, 512, src_r0=2816)
        self.phase_norm(PV_GFIN, final=True)
        return self.finish()

    def finish(self):
        self.barrier()
        self.es.close()
        return self.nc


def _fm(v, ntile):
    return np.ascontiguousarray(np.asarray(v, np.float32).reshape(ntile, 128).T)


def prep_pvec(inp, depth):
    out = np.zeros((depth, 128, NV), np.float32)
    for l in range(depth):
        o = out[l]
        o[:, PV_GMIX:PV_GMIX + 16] = _fm(inp["g_mix"][l], 16)
        o[:, PV_GFFN:PV_GFFN + 16] = _fm(inp["g_ffn"][l], 16)
        for j in range(3):
            o[:, PV_CAW + j:PV_CAW + 12:3] = _fm(inp["conv_a_w"][l, j], 4)
        for j in range(31):
            o[:, PV_CCW + j:PV_CCW + 124:31] = _fm(inp["cconv_w"][l, j], 4)
        o[:, PV_CCB:PV_CCB + 4] = _fm(inp["cconv_b"][l], 4)
        o[:, PV_CLG:PV_CLG + 4] = _fm(inp["cln_g"][l], 4)
        o[:, PV_CLB:PV_CLB + 4] = _fm(inp["cln_b"][l], 4)
        o[:, PV_GLAB:PV_GLAB + 2] = _fm(inp["gla_b_gate"][l], 2)
        o[:, PV_GLAG:PV_GLAG + 1] = _fm(inp["gla_g_norm"][l], 1)
        o[:64, PV_GLAB4:PV_GLAB4 + 4] = np.asarray(inp["gla_b_gate"][l], np.float32).reshape(4, 64).T
        for j in range(3):
            o[:, PV_FCW + j:PV_FCW + 132:3] = _fm(inp["ffn_conv_w"][l, j], 44)
        o[:, PV_GFIN:PV_GFIN + 16] = _fm(inp["g_final"], 16)
    return out


def prep_bias(inp, depth):
    rb = np.asarray(inp["rel_bias"], np.float32)[:depth]
    p = np.arange(128)[:, None]
    q = np.arange(64)[None, :]
    tabs = []
    for d0 in (128, 0, 192, 64, 1024):
        idx = np.clip(d0 + q - p, -128, 128) + 128
        tabs.append(rb[:, :, idx])
    t = np.stack(tabs, axis=2)
    t = np.transpose(t, (0, 3, 1, 2, 4))
    biasT = np.ascontiguousarray(t.reshape(depth, 128, 8 * 5 * 64))
    rb_rep = np.ascontiguousarray(np.broadcast_to(rb.reshape(depth, 1, 8 * 257), (depth, 128, 8 * 257)))
    return biasT, rb_rep


_NC_CACHE = {}


def make_in_maps(inp, n_cores, TP, NS, depth):
    f32 = lambda a: np.ascontiguousarray(np.asarray(a, np.float32))
    B = inp["x_prompt"].shape[0]
    pvec = prep_pvec(inp, depth)
    biasT, rb_rep = prep_bias(inp, depth)
    shared = {
        "pvec": pvec, "biasT": biasT, "rb_rep": rb_rep,
        "w_in": f32(inp["w_in"][:depth]), "w_out": f32(inp["w_out"][:depth]),
        "w_gate": f32(inp["w_ffn_gate"][:depth]), "w_up": f32(inp["w_ffn_up"][:depth]),
        "w_down": f32(inp["w_ffn_down"][:depth]), "gla_w2": f32(inp["gla_w_gate2"][:depth]),
    }
    maps = []
    for c in range(n_cores):
        b = c % B
        s0, s1 = c * NS, (c + 1) * NS
        m = dict(shared)
        m["x_all"] = f32(np.concatenate([inp["x_prompt"][b, :TP], np.asarray(inp["x_sample"][s0:s1]).reshape(NS * 16, D)], 0))
        m["st_short"] = f32(inp["state_short_conv"][:depth, s0:s1])
        m["st_gla"] = f32(np.asarray(inp["state_gla"][:depth, s0:s1]).reshape(depth, NS, 256, 128))
        m["st_cconv"] = f32(inp["state_conformer_conv"][:depth, s0:s1])
        m["st_k"] = f32(np.asarray(inp["cache_attn_k"][:depth, s0:s1]).reshape(depth, NS, 512, GW))
        m["st_v"] = f32(np.asarray(inp["cache_attn_v"][:depth, s0:s1]).reshape(depth, NS, 512, GW))
        m["st_ffn"] = f32(inp["state_ffn_conv"][:depth, s0:s1])
        maps.append(m)
    return maps


def gather_outputs(results, B, TP, NS, depth):
    n = len(results)
    r = results
    yp = np.stack([r[b]["y_all"][:TP] for b in range(B)], 0)
    ys = np.concatenate([r[c]["y_all"][TP:].reshape(NS, 16, D) for c in range(n)], 0)
    P = lambda k: np.stack([r[b][k] for b in range(B)], 1)
    Sx = lambda k: np.concatenate([r[c][k] for c in range(n)], 1)
    kr = min(512, TP)
    outs = (
        yp, ys,
        P("p_short"), P("p_gla").reshape(depth, B, 4, 64, 128), P("p_cconv"),
        P("p_k")[:, :, :kr].reshape(depth, B, kr, 8, 64), P("p_v")[:, :, :kr].reshape(depth, B, kr, 8, 64), P("p_ffn"),
        Sx("s_short"), Sx("s_gla").reshape(depth, n * NS, 4, 64, 128), Sx("s_cconv"),
        Sx("s_k").reshape(depth, n * NS, 16, 8, 64), Sx("s_v").reshape(depth, n * NS, 16, 8, 64), Sx("s_ffn"),
    )
    return tuple(np.ascontiguousarray(o, dtype=np.float32) for o in outs)


def kernel(**inputs):
    n_cores = 8
    TP, NS, depth = 4096, 4, DEPTH
    key = (TP, NS, depth)
    if key not in _NC_CACHE:
        _NC_CACHE[key] = K(TP, NS, depth).build()
    nc = _NC_CACHE[key]
    maps = make_in_maps(inputs, n_cores, TP, NS, depth)
    res = run_bass_kernel_spmd(nc, maps, core_ids=list(range(n_cores)))
    return gather_outputs(res.results, 4, TP, NS, depth)
```

```python
import math
from contextlib import ExitStack

import numpy as np
import concourse.bass as bass
import concourse.mybir as mybir
from concourse.bass_utils import run_bass_kernel_spmd

F32 = mybir.dt.float32
BF16 = mybir.dt.bfloat16
AF = mybir.ActivationFunctionType
ALU = mybir.AluOpType
AX = mybir.AxisListType

D = 2048
GW = 512
DFF = 5632
INC = 5648
EPS = 1e-6
DEPTH = 4
O_XA, O_BG, O_CG, O_Q, O_K, O_V, O_G, O_R, O_CV, O_CGT, O_DQ, O_DK, O_DV = (
    0, 512, 1024, 1536, 1792, 2048, 2560, 3072, 3088, 3600, 4112, 4624, 5136)

PV_GMIX, PV_GFFN, PV_CAW, PV_CCW, PV_CCB, PV_CLG, PV_CLB, PV_GLAB, PV_GLAG, PV_FCW, PV_GFIN = (
    0, 16, 32, 44, 168, 172, 176, 180, 182, 183, 315)
PV_GLAB4 = 331
NV = 335


class Sem:
    __slots__ = ("h", "n")

    def __init__(self, h):
        self.h = h
        self.n = 0


class Tile:
    __slots__ = ("ap", "w", "r", "war", "sem", "excl")

    def __init__(self, ap, sem=None, excl=False):
        self.ap = ap
        self.excl = excl
        self.w = []
        self.r = []
        self.war = []
        self.sem = sem

    def __getitem__(self, idx):
        return self.ap[idx]


def _merge(evs):
    d = {}
    for S, v in evs:
        if v > d.get(S, 0):
            d[S] = v
    return list(d.items())


class Eng:
    def __init__(self, eng, S, selfsync):
        self.eng = eng
        self.S = S
        self.selfsync = selfsync
        self.seen = {}

    def wait_for(self, evs):
        for S, v in _merge(evs):
            if S is self.S and not self.selfsync:
                continue
            if self.seen.get(S, 0) >= v:
                continue
            self.eng.wait_ge(S.h, v)
            self.seen[S] = v

    @staticmethod
    def _deps(r, w, wm):
        evs = []
        for t in r:
            evs += t.w
            if t.excl:
                evs += t.r
        for t in w:
            evs += t.w
            evs += t.r
            evs += t.war
        for t in wm:
            if t.r:
                t.war = _merge(t.war + t.r + t.w)
                t.w = []
                t.r = []
            evs += t.war
        return evs

    @staticmethod
    def _post(ev, r, w, wm):
        for t in r:
            t.r = _merge(t.r + [ev])
        for t in w:
            t.w = [ev]
            t.r = []
            t.war = []
        for t in wm:
            t.w = _merge(t.w + [ev])

    def op(self, fn, r=(), w=(), wm=()):
        self.wait_for(self._deps(r, w, wm))
        inst = fn(self.eng)
        self.S.n += 1
        inst.then_inc(self.S.h, 1)
        self._post((self.S, self.S.n), r, w, wm)

    def dma(self, out, in_, S, r=(), w=(), wm=(), **kw):
        evs = self._deps(r, w, wm)
        if S.n:
            evs.append((S, S.n))
        self.wait_for(evs)
        inst = self.eng.dma_start(out=out, in_=in_, **kw)
        S.n += 16
        inst.then_inc(S.h, 16)
        self._post((S, S.n), r, w, wm)


def split_even(n, maxw, mult=16):
    k = (n + maxw - 1) // maxw
    base = ((n + k - 1) // k + mult - 1) // mult * mult
    out = []
    s = 0
    while s < n:
        w = min(base, n - s)
        out.append((s, w))
        s += w
    return out


class K:
    def __init__(self, TP, NS, depth, debug=()):
        self.TP, self.NS, self.depth = TP, NS, depth
        self.TS = 16 * NS
        self.TT = TP + self.TS
        self.debug = set(debug)
        self.nc = nc = bass.Bass("TRN2", target_bir_lowering=False)
        self.es = ExitStack()
        self.sems_free = []
        self.all_sems = []
        S = lambda: self._new_sem()
        self.pe = Eng(nc.tensor, S(), False)
        self.act = Eng(nc.scalar, S(), True)
        self.dve = Eng(nc.vector, S(), True)
        self.pool = Eng(nc.gpsimd, S(), True)
        self.sp = Eng(nc.sync, S(), True)
        self.engs = [self.pe, self.act, self.dve, self.pool, self.sp]
        self.sw_sems = [S() for _ in range(2)]
        self.scr_sem = S()
        self.dma_sems = [S() for _ in range(21)]
        self._dma_rr = 0
        self._evac_rr = 0

    def _new_sem(self):
        h = self.es.enter_context(self.nc.semaphore(f"s{len(self.all_sems)}"))
        s = Sem(h)
        self.all_sems.append(s)
        return s

    def dsem(self):
        s = self.dma_sems[self._dma_rr % len(self.dma_sems)]
        self._dma_rr += 1
        return s

    def sb(self, es, name, shape, dtype, dma=False):
        reuse = getattr(self, '_reuse', None)
        if reuse is not None:
            if name in reuse:
                return reuse[name]
            t = self._sb_new(self._reuse_es, name, shape, dtype, dma)
            reuse[name] = t
            return t
        return self._sb_new(es, name, shape, dtype, dma)

    def _sb_new(self, es, name, shape, dtype, dma=False):
        self._uid = getattr(self, '_uid', 0) + 1
        t = es.enter_context(self.nc.sbuf_tensor(f"{name}_{self._uid}", list(shape), dtype))
        if dma == "sw0" or dma == "sw1":
            return Tile(t, self.sw_sems[int(dma[2])])
        return Tile(t, self.dsem() if dma else None)

    def dram_in(self, name, shape, dtype=F32):
        return self.nc.dram_tensor(name, list(shape), dtype, kind="ExternalInput").ap()

    def dram_out(self, name, shape, dtype=F32):
        return self.nc.dram_tensor(name, list(shape), dtype, kind="ExternalOutput").ap()

    def scratch(self, name, shape, dtype):
        kind = "ExternalOutput" if name in self.debug else "Internal"
        ap = self.nc.dram_tensor(name, list(shape), dtype, kind=kind).ap()
        return Tile(ap)

    def barrier(self):
        if getattr(self, '_reuse', None) is not None:
            return
        evs = [(s, s.n) for s in self.all_sems if s.n]
        for e in self.engs:
            e.wait_for(evs)

    def evac_eng(self):
        self._evac_rr += 1
        return self.act if self._evac_rr % 2 else self.dve

    def copy(self, eng, out, in_, **kw):
        if eng is self.act:
            eng.op(lambda e: e.activation(out=out, in_=in_, func=AF.Copy), **kw)
        else:
            eng.op(lambda e: e.tensor_copy(out=out, in_=in_), **kw)

    def setup(self):
        nc, es = self.nc, self.es
        TT, NS, dp = self.TT, self.NS, self.depth
        self.ps = [Tile(es.enter_context(nc.psum_tensor(f"ps{i}", [128, 512], F32)), excl=True) for i in range(8)]
        self._ps_rr = 0
        self.x_all = self.dram_in("x_all", [TT, D])
        self.pvec = self.dram_in("pvec", [dp, 128, NV])
        self.w_in = self.dram_in("w_in", [dp, D, INC])
        self.w_out = self.dram_in("w_out", [dp, D, D])
        self.w_gate = self.dram_in("w_gate", [dp, D, DFF])
        self.w_up = self.dram_in("w_up", [dp, D, DFF])
        self.w_down = self.dram_in("w_down", [dp, DFF, D])
        self.gla_w2 = self.dram_in("gla_w2", [dp, 16, 256])
        self.biasT = self.dram_in("biasT", [dp, 128, 8 * 5 * 64])
        self.rb_rep = self.dram_in("rb_rep", [dp, 128, 8 * 257])
        self.st_short = self.dram_in("st_short", [dp, NS, 2, GW])
        self.st_gla = self.dram_in("st_gla", [dp, NS, 256, 128])
        self.st_cconv = self.dram_in("st_cconv", [dp, NS, 30, GW])
        self.st_k = self.dram_in("st_k", [dp, NS, 512, GW])
        self.st_v = self.dram_in("st_v", [dp, NS, 512, GW])
        self.st_ffn = self.dram_in("st_ffn", [dp, NS, 2, DFF])
        self.y_all = self.dram_out("y_all", [TT, D])
        self.o_p = {
            "short": self.dram_out("p_short", [dp, 2, GW]),
            "gla": self.dram_out("p_gla", [dp, 256, 128]),
            "cconv": self.dram_out("p_cconv", [dp, 30, GW]),
            "k": self.dram_out("p_k", [dp, 512, GW]),
            "v": self.dram_out("p_v", [dp, 512, GW]),
            "ffn": self.dram_out("p_ffn", [dp, 2, DFF]),
        }
        self.o_s = {
            "short": self.dram_out("s_short", [dp, NS, 2, GW]),
            "gla": self.dram_out("s_gla", [dp, NS, 256, 128]),
            "cconv": self.dram_out("s_cconv", [dp, NS, 30, GW]),
            "k": self.dram_out("s_k", [dp, NS, 16, GW]),
            "v": self.dram_out("s_v", [dp, NS, 16, GW]),
            "ffn": self.dram_out("s_ffn", [dp, NS, 2, DFF]),
        }
        self.xT = self.scratch("xT", [D, TT], F32)
        self.xTt = [Tile(self.xT.ap[k * 128:(k + 1) * 128, :]) for k in range(16)]
        self.hT = self.scratch("hT", [D, TT], BF16)
        self.zT = self.scratch("zT", [INC, TT], F32)
        self.vtok_a = self.scratch("vtok_a", [TT, GW], F32)
        self.vtok_g = self.scratch("vtok_g", [TT, GW], F32)
        self.ktok = self.scratch("ktok", [TT, GW], F32)
        self.yT = self.scratch("yT", [D, TT], BF16)
        self.aT = self.scratch("aT", [DFF, TT], BF16)
        self.ident = self.sb(es, "ident", [128, 128], F32)
        self.ones32 = self.sb(es, "ones32", [128, 128], F32)
        self.identb = self.sb(es, "identb", [128, 128], BF16)
        self.onesb = self.sb(es, "onesb", [128, 128], BF16)
        self.selA = self.sb(es, "selA", [128, 128], BF16)
        self.selB = self.sb(es, "selB", [128, 128], BF16)
        self.trimask = self.sb(es, "trimask", [64, 64], F32)
        self.pv = self.sb(es, "pv", [128, NV], F32, dma=True)
        P = self.pool
        self.epsc = self.sb(es, 'epsc', [128, 1], F32)
        P.op(lambda e: e.memset(self.epsc.ap[:, :], EPS), w=[self.epsc])
        self.onec = self.sb(es, 'onec', [128, 1], F32)
        P.op(lambda e: e.memset(self.onec.ap[:, :], 1.0), w=[self.onec])
        P.op(lambda e: e.memset(self.ident.ap[:, :], 0.0), w=[self.ident])
        P.op(lambda e: e.affine_select(out=self.ident.ap[:, :], in_=self.ident.ap[:, :], compare_op=ALU.not_equal,
                                       fill=1.0, base=0, pattern=[[-1, 128]], channel_multiplier=1),
             w=[self.ident])
        P.op(lambda e: e.memset(self.ones32.ap[:, :], 1.0), w=[self.ones32])
        P.op(lambda e: e.memset(self.onesb.ap[:, :], 1.0), w=[self.onesb])
        P.op(lambda e: e.tensor_copy(out=self.identb.ap[:, :], in_=self.ident.ap[:, :]), r=[self.ident], w=[self.identb])
        P.op(lambda e: e.memset(self.selA.ap[:, :], 0.0), w=[self.selA])
        P.op(lambda e: e.memset(self.selA.ap[0:64, :], 1.0), w=[self.selA])
        P.op(lambda e: e.memset(self.selB.ap[:, :], 0.0), w=[self.selB])
        P.op(lambda e: e.memset(self.selB.ap[64:128, :], 1.0), w=[self.selB])
        P.op(lambda e: e.memset(self.trimask.ap[:, :], 1.0), w=[self.trimask])
        P.op(lambda e: e.affine_select(out=self.trimask.ap[:, :], in_=self.trimask.ap[:, :], compare_op=ALU.is_ge,
                                       fill=0.0, base=0, pattern=[[1, 64]], channel_multiplier=-1),
             w=[self.trimask])

    def rstd(self, out, in_, inv_n, rtiles, wtile):
        self.act.op(lambda e: e.activation(out=out, in_=in_, func=AF.Ln, scale=inv_n, bias=self.epsc.ap[:out.shape[0], :]),
                    r=list(rtiles) + [self.epsc], w=[wtile])
        self.act.op(lambda e: e.activation(out=out, in_=out, func=AF.Exp, scale=-0.5), r=[wtile], w=[wtile])

    def psb(self):
        t = self.ps[self._ps_rr % 6]
        self._ps_rr += 1
        return t

    def load_pvec(self, l):
        self.sp.dma(out=self.pv.ap[:, :], in_=self.pvec[l], S=self.pv.sem, w=[self.pv])

    def phase_transpose_in(self):
        TT = self.TT
        xTv = self.xT.ap.rearrange("(k p) t -> p k t", p=128)
        with ExitStack() as es:
            xin = [self.sb(es, f"p0i{i}", [128, D], F32, dma=True) for i in range(2)]
            xo = [self.sb(es, f"p0o{i}", [128, 16, 128], F32, dma=True) for i in range(2)]
            for i in range((TT + 127) // 128):
                r0 = i * 128
                n = min(128, TT - r0)
                ti, to = xin[i % 2], xo[i % 2]
                self.sp.dma(out=ti.ap[:n, :], in_=self.x_all[r0:r0 + n, :], S=ti.sem, w=[ti])
                for j in range(4):
                    ps = self.psb()
                    for q in range(4):
                        f = j * 4 + q
                        self.pe.op(lambda e: e.transpose(out=ps.ap[:, q * 128:q * 128 + n],
                                                         in_=ti.ap[:n, f * 128:(f + 1) * 128],
                                                         identity=self.ident.ap[:n, :n]),
                                   r=[ti, self.ident], w=[ps])
                    src = ps.ap.rearrange("p (a b) -> p a b", b=128)[:, :, :n]
                    self.copy(self.evac_eng(), to.ap[:, j * 4:(j + 1) * 4, :n], src, r=[ps], wm=[to])
                self.sp.dma(out=xTv[:, :, r0:r0 + n], in_=to.ap[:, :, :n], S=to.sem, r=[to], wm=self.xTt)
            self.barrier()

    def phase_norm(self, gcol, final=False):
        TT = self.TT
        xTv = self.xT.ap.rearrange("(k p) t -> p k t", p=128)
        hTv = self.hT.ap.rearrange("(k p) t -> p k t", p=128)
        with ExitStack() as es:
            xb = [self.sb(es, f"n_x{i}", [128, 16, 512], F32, dma=True) for i in range(2)]
            sq = [self.sb(es, f"n_sq{i}", [128, 16, 512], F32) for i in range(1)]
            rs = [self.sb(es, f"n_rs{i}", [128, 512], F32) for i in range(2)]
            if final:
                ho = [self.sb(es, f"n_h{i}", [128, 16, 512], F32) for i in range(2)]
                yo = [self.sb(es, f"n_y{i}", [128, D], F32, dma=True) for i in range(2)]
            else:
                ho = [self.sb(es, f"n_h{i}", [128, 16, 512], BF16, dma=True) for i in range(2)]
            nyo = 0
            for bi, (c0, cw) in enumerate(split_even(TT, 512)):
                x, s, r, h = xb[bi % 2], sq[0], rs[bi % 2], ho[bi % 2]
                self.sp.dma(out=x.ap[:, :, :cw], in_=xTv[:, :, c0:c0 + cw], S=x.sem, r=self.xTt, w=[x])
                self.act.op(lambda e: e.activation(out=s.ap[:, :, :cw], in_=x.ap[:, :, :cw], func=AF.Square),
                            r=[x], w=[s])
                ps = self.psb()
                for k in range(16):
                    self.pe.op(lambda e: e.matmul(ps.ap[:, :cw], lhsT=self.ones32.ap[:, :], rhs=s.ap[:, k, :cw],
                                                  start=(k == 0), stop=(k == 15)),
                               r=[self.ones32, s], w=[ps])
                self.rstd(r.ap[:, :cw], ps.ap[:, :cw], 1.0 / D, [ps], r)
                for k in range(16):
                    eng = self.dve
                    eng.op(lambda e: e.scalar_tensor_tensor(out=h.ap[:, k, :cw], in0=x.ap[:, k, :cw],
                                                            scalar=self.pv.ap[:, gcol + k:gcol + k + 1],
                                                            in1=r.ap[:, :cw], op0=ALU.mult, op1=ALU.mult),
                           r=[x, r, self.pv], wm=[h])
                if not final:
                    self.sp.dma(out=hTv[:, :, c0:c0 + cw], in_=h.ap[:, :, :cw], S=h.sem, r=[h], wm=[self.hT])
                else:
                    for t0 in range(0, cw, 128):
                        n = min(128, cw - t0)
                        y = yo[nyo % 2]
                        nyo += 1
                        for j in range(4):
                            ps2 = self.psb()
                            for q in range(4):
                                f = j * 4 + q
                                self.pe.op(lambda e: e.transpose(out=ps2.ap[:n, q * 128:(q + 1) * 128],
                                                                 in_=h.ap[:, f, t0:t0 + n],
                                                                 identity=self.ident.ap[:, :]),
                                           r=[h, self.ident], w=[ps2])
                            self.copy(self.evac_eng(), y.ap[:n, j * 512:(j + 1) * 512], ps2.ap[:n, :],
                                      r=[ps2], wm=[y])
                        self.sp.dma(out=self.y_all[c0 + t0:c0 + t0 + n, :], in_=y.ap[:n, :], S=y.sem, r=[y])
            self.barrier()

    def seq_ranges(self):
        return [(0, self.TP)] + [(self.TP + 16 * s, 16) for s in range(self.NS)]

    def mm_f(self, wt, wc0, M, xin, n0, nw, KT):
        ps = self.psb()
        for k in range(KT):
            self.pe.op(lambda e: e.matmul(ps.ap[:M, :nw], lhsT=wt.ap[:, k, wc0:wc0 + M], rhs=xin.ap[:, k, n0:n0 + nw],
                                          start=(k == 0), stop=(k == KT - 1)), r=[wt, xin], w=[ps])
        return ps

    def mm_t(self, wt, wc0, w, xin, t0, tn, KT):
        ps = self.psb()
        for k in range(KT):
            self.pe.op(lambda e: e.matmul(ps.ap[:tn, :w], lhsT=xin.ap[:, k, t0:t0 + tn], rhs=wt.ap[:, k, wc0:wc0 + w],
                                          start=(k == 0), stop=(k == KT - 1)), r=[wt, xin], w=[ps])
        return ps

    def linear(self, es, src, KT, blocks, CW, SLOTW, handler, dbl=False, src_r0=0):
        TT = self.TT
        srcv = src.ap[src_r0:src_r0 + KT * 128, :].rearrange("(k p) t -> p k t", p=128)
        nsb = (TT + CW - 1) // CW
        xins = [self.sb(es, f"lin_x{i}", [128, KT, CW], BF16, dma=True) for i in range(2 if (nsb > 1 and dbl) else 1)]
        wts = [self.sb(es, f"lin_w{i}", [128, KT, SLOTW], BF16, dma=f"sw{i}") for i in range(2)]
        wi = 0

        def load_x(sbi):
            s0 = sbi * CW
            sw = min(CW, TT - s0)
            xin = xins[sbi % len(xins)]
            self.sp.dma(out=xin.ap[:, :, :sw], in_=srcv[:, :, s0:s0 + sw], S=xin.sem, r=[src], w=[xin])

        def load_w(parts, wt):
            off = 0
            for (Wap, c0, w) in parts:
                Wv = Wap.rearrange("(k p) n -> p k n", p=128)
                self.pool.dma(out=wt.ap[:, :, off:off + w], in_=Wv[:, :, c0:c0 + w], S=wt.sem, wm=[wt])
                off += w

        if len(xins) > 1:
            load_x(0)
        for sbi in range(nsb):
            s0 = sbi * CW
            sw = min(CW, TT - s0)
            xin = xins[sbi % len(xins)]
            if len(xins) == 1:
                load_x(sbi)
            elif sbi + 1 < nsb:
                load_x(sbi + 1)
            load_w(blocks[0][0], wts[wi % 2])
            for bi, (parts, tag) in enumerate(blocks):
                wt = wts[wi % 2]
                wi += 1
                if bi + 1 < len(blocks):
                    load_w(blocks[bi + 1][0], wts[wi % 2])
                handler(tag, parts, wt, xin, s0, sw)

    def phase_inproj(self, l):
        W = self.w_in[l]
        TP, TT = self.TP, self.TT
        blocks = []
        for (c0, wd, tag) in [(O_XA, 512, "f"), (O_BG, 512, "f"), (O_CG, 512, "f"), (O_Q, 256, "f"), (O_K, 256, "f"),
                              (O_V, 512, "tg"), (O_G, 512, "f"), (O_R, 16, "f"), (O_CV, 512, "f"), (O_CGT, 512, "f"),
                              (O_DQ, 512, "f"), (O_DK, 512, "fk"), (O_DV, 512, "ta")]:
            blocks.append(([(W, c0, wd)], tag))
        with ExitStack() as es:
            stf = [self.sb(es, f"z_sf{i}", [128, 2080], F32, dma=True) for i in range(2)]
            stt = [self.sb(es, f"z_st{i}", [128, 512], F32, dma=True) for i in range(3)]
            cnt = {"f": 0, "t": 0}

            def handler(tag, parts, wt, xin, s0, sw):
                (_, c0, wd) = parts[0]
                if tag[0] == "f":
                    for m0 in range(0, wd, 128):
                        M = min(128, wd - m0)
                        st = stf[cnt["f"] % 2]
                        cnt["f"] += 1
                        for (n0, nw) in split_even(sw, 512):
                            ps = self.mm_f(wt, m0, M, xin, n0, nw, 16)
                            self.copy(self.evac_eng(), st.ap[:M, n0:n0 + nw], ps.ap[:M, :nw], r=[ps], wm=[st])
                        self.sp.dma(out=self.zT.ap[c0 + m0:c0 + m0 + M, s0:s0 + sw], in_=st.ap[:M, :sw], S=st.sem,
                                    r=[st], wm=[self.zT])
                if tag[0] == "t" or tag == "fk":
                    dst = {"tg": self.vtok_g, "ta": self.vtok_a, "fk": self.ktok}[tag]
                    for t0 in range(0, sw, 128):
                        tn = min(128, sw - t0)
                        if tag == "fk" and (s0 + t0 + tn <= TP - 512):
                            continue
                        st = stt[cnt["t"] % 3]
                        cnt["t"] += 1
                        ps = self.mm_t(wt, 0, wd, xin, t0, tn, 16)
                        self.copy(self.evac_eng(), st.ap[:tn, :wd], ps.ap[:tn, :wd], r=[ps], w=[st])
                        self.sp.dma(out=dst.ap[s0 + t0:s0 + t0 + tn, :], in_=st.ap[:tn, :wd], S=st.sem,
                                    r=[st], wm=[dst])

            self.linear(es, self.hT, 16, blocks, 2080, 512, handler, dbl=True)
            self.barrier()

    def tok2feat(self, es, name, src_ap, R, C):
        nt = C // 128
        stg = self.sb(es, name + "_s", [R, C], F32, dma=True)
        dst = self.sb(es, name + "_d", [128, nt, R], F32)
        self.sp.dma(out=stg.ap[:, :], in_=src_ap, S=stg.sem, w=[stg])
        per = max(1, 512 // R)
        for g0 in range(0, nt, per):
            g = min(per, nt - g0)
            ps = self.psb()
            for q in range(g):
                self.pe.op(lambda e: e.transpose(out=ps.ap[:, q * R:(q + 1) * R],
                                                 in_=stg.ap[:R, (g0 + q) * 128:(g0 + q + 1) * 128],
                                                 identity=self.ident.ap[:R, :R]), r=[stg, self.ident], w=[ps])
            self.copy(self.evac_eng(), dst.ap[:, g0:g0 + g, :],
                      ps.ap[:, :g * R].rearrange("p (a b) -> p a b", b=R), r=[ps], wm=[dst])
        return dst

    def feat2tok(self, es, name, src, R, C, dst_ap):
        nt = C // 128
        stg = self.sb(es, name + "_o", [R, C], F32, dma=True)
        for g0 in range(0, nt, 4):
            g = min(4, nt - g0)
            ps = self.psb()
            for q in range(g):
                self.pe.op(lambda e: e.transpose(out=ps.ap[:R, q * 128:(q + 1) * 128], in_=src.ap[:, g0 + q, :],
                                                 identity=self.ident.ap[:, :]), r=[src, self.ident], w=[ps])
            self.copy(self.evac_eng(), stg.ap[:R, g0 * 128:(g0 + g) * 128], ps.ap[:R, :g * 128], r=[ps], wm=[stg])
        self.sp.dma(out=dst_ap, in_=stg.ap[:R, :], S=stg.sem, r=[stg])

    def mix_short(self, l):
        TP, NS, TT = self.TP, self.NS, self.TT
        zv = self.zT.ap
        pv = self.pv
        import os
        for (is_s, nseq, T, col0) in [(False, 1, TP, 0), (True, NS, 16, TP)]:
            if is_s and os.environ.get('DBG_NOSAMPLE'):
                continue
            with ExitStack() as es:
                BW = min(T, 1024)
                nblk = (T + BW - 1) // BW
                W = nseq * BW
                if is_s:
                    hist = self.tok2feat(es, "a_h", self.st_short[l].rearrange("s r c -> (s r) c"), 2 * NS, GW)
                stout = self.sb(es, "a_so", [128, 4, nseq * 2], F32)
                xa = [self.sb(es, f"a_xa{i}", [128, W], F32, dma=True) for i in range(2)]
                bg = [self.sb(es, f"a_bg{i}", [128, W], F32, dma=True) for i in range(2)]
                cg = [self.sb(es, f"a_cg{i}", [128, W], F32, dma=True) for i in range(2)]
                ue = [self.sb(es, f"a_ue{i}", [128, nseq, BW + 2], F32) for i in range(2)]
                cc = [self.sb(es, f"a_cc{i}", [128, nseq, BW], F32) for i in range(2)]
                yo = [self.sb(es, f"a_yo{i}", [128, nseq, BW], BF16, dma=True) for i in range(2)]
                it = 0
                for i in range(4):
                    for b in range(nblk):
                        c0 = col0 + b * BW
                        cw = min(BW, T - b * BW) if not is_s else BW
                        n = cw * nseq
                        X, Bg, Cg, U, C_, Y = (t[it % 2] for t in (xa, bg, cg, ue, cc, yo))
                        Uprev = ue[(it + 1) % 2]
                        it += 1
                        self.sp.dma(out=X.ap[:, :n], in_=zv[O_XA + i * 128:O_XA + (i + 1) * 128, c0:c0 + n], S=X.sem,
                                    r=[self.zT], w=[X])
                        self.sp.dma(out=Bg.ap[:, :n], in_=zv[O_BG + i * 128:O_BG + (i + 1) * 128, c0:c0 + n], S=Bg.sem,
                                    r=[self.zT], w=[Bg])
                        self.sp.dma(out=Cg.ap[:, :n], in_=zv[O_CG + i * 128:O_CG + (i + 1) * 128, c0:c0 + n], S=Cg.sem,
                                    r=[self.zT], w=[Cg])
                        v3 = lambda t: t.ap[:, :n].rearrange("p (s c) -> p s c", s=nseq)
                        if is_s:
                            self.dve.op(lambda e: e.tensor_copy(out=U.ap[:, :, 0:2],
                                                                in_=hist.ap[:, i, :].rearrange("p (s r) -> p s r", r=2)),
                                        r=[hist], w=[U])
                        elif b == 0:
                            self.dve.op(lambda e: e.memset(U.ap[:, :, 0:2], 0.0), w=[U])
                        else:
                            self.dve.op(lambda e: e.tensor_copy(out=U.ap[:, :, 0:2], in_=Uprev.ap[:, :, BW:BW + 2]),
                                        r=[Uprev], w=[U])
                        self.dve.op(lambda e: e.tensor_tensor(out=U.ap[:, :, 2:2 + cw], in0=v3(Cg), in1=v3(X), op=ALU.mult),
                                    r=[Cg, X], wm=[U])
                        wc = PV_CAW + i * 3
                        self.dve.op(lambda e: e.tensor_scalar(out=C_.ap[:, :, :cw], in0=U.ap[:, :, 0:cw],
                                                              scalar1=pv.ap[:, wc:wc + 1], scalar2=None, op0=ALU.mult),
                                    r=[U, pv], w=[C_])
                        for j in (1, 2):
                            self.dve.op(lambda e: e.scalar_tensor_tensor(out=C_.ap[:, :, :cw], in0=U.ap[:, :, j:j + cw],
                                                                         scalar=pv.ap[:, wc + j:wc + j + 1],
                                                                         in1=C_.ap[:, :, :cw], op0=ALU.mult, op1=ALU.add),
                                        r=[U, pv, C_], w=[C_])
                        self.dve.op(lambda e: e.tensor_tensor(out=Y.ap[:, :, :cw], in0=C_.ap[:, :, :cw], in1=v3(Bg),
                                                              op=ALU.mult), r=[C_, Bg], w=[Y])
                        if nseq == 1:
                            self.sp.dma(out=self.yT.ap[i * 128:(i + 1) * 128, c0:c0 + cw], in_=Y.ap[:, 0, :cw], S=Y.sem,
                                        r=[Y], wm=[self.yT])
                        else:
                            self.sp.dma(out=self.yT.ap[i * 128:(i + 1) * 128, c0:c0 + n].rearrange("p (s c) -> p s c", s=nseq),
                                        in_=Y.ap[:, :, :cw], S=Y.sem, r=[Y], wm=[self.yT])
                        if b == nblk - 1:
                            self.dve.op(lambda e: e.tensor_copy(out=stout.ap[:, i, :].rearrange("p (s r) -> p s r", r=2),
                                                                in_=U.ap[:, :, cw:cw + 2]), r=[U], wm=[stout])
                dst = (self.o_s["short"][l].rearrange("s r c -> (s r) c") if is_s else self.o_p["short"][l])
                if not (os.environ.get('DBG_NOF2T') == '1' or (os.environ.get('DBG_NOF2T') == 'p' and not is_s)):
                    self.feat2tok(es, "a_fo", stout, 2 * nseq, GW, dst)
                self.barrier()

    def build_diag(self, es):
        dg = self.sb(es, "c_diag", [128, 4, 31, 128], BF16)
        for i in range(4):
            for j in range(31):
                c = PV_CCW + i * 31 + j
                self.pool.op(lambda e: e.tensor_scalar(out=dg.ap[:, i, j, :], in0=self.identb.ap[:, :],
                                                       scalar1=self.pv.ap[:, c:c + 1], scalar2=None, op0=ALU.mult),
                             r=[self.identb, self.pv], wm=[dg])
        return dg

    def mix_cconv(self, l):
        TP, NS, TT = self.TP, self.NS, self.TT
        pv = self.pv
        zcv = self.zT.ap[O_CV:O_CV + 512, :].rearrange("(i p) t -> p i t", p=128)
        zcg = self.zT.ap[O_CGT:O_CGT + 512, :].rearrange("(i p) t -> p i t", p=128)
        yv = self.yT.ap[1024:1536, :].rearrange("(i p) t -> p i t", p=128)
        with ExitStack() as es0:
            dg = self.build_diag(es0)
            import os
            clv = int(os.environ.get('DBG_CC', '9'))
            if clv < 2:
                self.barrier()
                return
            for (is_s, nseq, T, col0) in [(False, 1, TP, 0), (True, NS, 16, TP)]:
                with ExitStack() as es:
                    BW = min(T, 512 // nseq)
                    nblk = (T + BW - 1) // BW
                    W = nseq * BW
                    if is_s:
                        hist = self.tok2feat(es, "c_h", self.st_cconv[l].rearrange("s r c -> (s r) c"), 30 * NS, GW)
                    stout = self.sb(es, "c_so", [128, 4, nseq * 30], F32)
                    cv = [self.sb(es, f"c_cv{i}", [128, 4, W], F32, dma=True) for i in range(2)]
                    cg = [self.sb(es, f"c_cg{i}", [128, 4, W], F32, dma=True) for i in range(2)]
                    ue = [self.sb(es, f"c_ue{i}", [128, 4, nseq, BW + 30], BF16) for i in range(2)]
                    uf = [self.sb(es, f"c_uf{i}", [128, 4, nseq, BW + 30], F32) for i in range(2)]
                    cc = [self.sb(es, f"c_cc{i}", [128, 4, W], F32) for i in range(2)]
                    sq = [self.sb(es, f"c_sq{i}", [128, 4, W], F32) for i in range(2)]
                    mean = self.sb(es, "c_mean", [128, W], F32)
                    msq = self.sb(es, "c_msq", [128, W], F32)
                    rs = self.sb(es, "c_rs", [128, W], F32)
                    tt = [self.sb(es, f"c_t{i}", [128, W], F32) for i in range(2)]
                    yo = [self.sb(es, f"c_yo{i}", [128, 4, W], BF16, dma=True) for i in range(2)]
                    def blk_ctx(b):
                        c0 = col0 + b * BW
                        cw = min(BW, T - b * BW)
                        n = cw * nseq
                        return (c0, cw, n) + tuple(t[b % 2] for t in (cv, cg, ue, uf, cc, sq, yo)) + (ue[(b + 1) % 2], uf[(b + 1) % 2])

                    def front(b):
                        (c0, cw, n, CV, CG, UE, UF, CC, SQ, Y, UEp, UFp) = blk_ctx(b)
                        self.sp.dma(out=CV.ap[:, :, :n], in_=zcv[:, :, c0:c0 + n], S=CV.sem, r=[self.zT], w=[CV])
                        self.sp.dma(out=CG.ap[:, :, :n], in_=zcg[:, :, c0:c0 + n], S=CG.sem, r=[self.zT], w=[CG])
                        self.act.op(lambda e: e.activation(out=CG.ap[:, :, :n], in_=CG.ap[:, :, :n], func=AF.Sigmoid),
                                    r=[CG], w=[CG])
                        v4 = lambda t: t.ap[:, :, :n].rearrange("p i (s c) -> p i s c", s=nseq)
                        if is_s:
                            self.dve.op(lambda e: e.tensor_copy(out=UF.ap[:, :, :, 0:30],
                                                                in_=hist.ap[:, :, :].rearrange("p i (s r) -> p i s r", r=30)),
                                        r=[hist], w=[UF])
                        elif b == 0:
                            self.dve.op(lambda e: e.memset(UF.ap[:, :, :, 0:30], 0.0), w=[UF])
                        else:
                            self.dve.op(lambda e: e.tensor_copy(out=UF.ap[:, :, :, 0:30], in_=UFp.ap[:, :, :, BW:BW + 30]),
                                        r=[UFp], w=[UF])
                        for i in range(4):
                            self.dve.op(lambda e: e.tensor_tensor(out=UF.ap[:, i, :, 30:30 + cw], in0=v4(CV)[:, i],
                                                                  in1=v4(CG)[:, i], op=ALU.mult), r=[CV, CG], wm=[UF])
                        self.act.op(lambda e: e.activation(out=UE.ap[:, :, :, :cw + 30], in_=UF.ap[:, :, :, :cw + 30],
                                                           func=AF.Copy), r=[UF], w=[UE])

                    def back(b):
                        (c0, cw, n, CV, CG, UE, UF, CC, SQ, Y, UEp, UFp) = blk_ctx(b)
                        v4 = lambda t: t.ap[:, :, :n].rearrange("p i (s c) -> p i s c", s=nseq)
                        pss = []
                        for i in range(4):
                            ps = self.psb()
                            pss.append(ps)
                            out = ps.ap[:, :n] if nseq == 1 else ps.ap[:, :n].rearrange("p (s c) -> p s c", s=nseq)
                            for j in range(31):
                                rhs = UE.ap[:, i, 0, j:j + cw] if nseq == 1 else UE.ap[:, i, :, j:j + cw]
                                self.pe.op(lambda e: e.matmul(out, lhsT=dg.ap[:, i, j, :], rhs=rhs,
                                                              start=(j == 0), stop=(j == 30)), r=[dg, UE], w=[ps])
                            bcol = pv.ap[:, PV_CCB + i:PV_CCB + i + 1]
                            self.act.op(lambda e: e.activation(out=CC.ap[:, i, :n], in_=ps.ap[:, :n], func=AF.Identity,
                                                               bias=bcol), r=[ps, pv], wm=[CC])
                            self.act.op(lambda e: e.activation(out=SQ.ap[:, i, :n], in_=ps.ap[:, :n], func=AF.Square,
                                                               bias=bcol), r=[ps, pv], wm=[SQ])
                        ps1, ps2 = self.psb(), self.psb()
                        for i in range(4):
                            self.pe.op(lambda e: e.matmul(ps1.ap[:, :n], lhsT=self.ones32.ap[:, :], rhs=CC.ap[:, i, :n],
                                                          start=(i == 0), stop=(i == 3)), r=[self.ones32, CC], w=[ps1])
                        for i in range(4):
                            self.pe.op(lambda e: e.matmul(ps2.ap[:, :n], lhsT=self.ones32.ap[:, :], rhs=SQ.ap[:, i, :n],
                                                          start=(i == 0), stop=(i == 3)), r=[self.ones32, SQ], w=[ps2])
                        self.dve.op(lambda e: e.tensor_scalar(out=mean.ap[:, :n], in0=ps1.ap[:, :n], scalar1=1.0 / 512,
                                                              scalar2=None, op0=ALU.mult), r=[ps1], w=[mean])
                        self.dve.op(lambda e: e.tensor_tensor(out=msq.ap[:, :n], in0=mean.ap[:, :n], in1=mean.ap[:, :n],
                                                              op=ALU.mult), r=[mean], w=[msq])
                        self.dve.op(lambda e: e.scalar_tensor_tensor(out=msq.ap[:, :n], in0=ps2.ap[:, :n], scalar=1.0 / 512,
                                                                     in1=msq.ap[:, :n], op0=ALU.mult, op1=ALU.subtract),
                                    r=[ps2, msq], w=[msq])
                        self.rstd(rs.ap[:, :n], msq.ap[:, :n], 1.0, [msq], rs)
                        for i in range(4):
                            T_ = tt[i % 2]
                            self.dve.op(lambda e: e.tensor_tensor(out=T_.ap[:, :n], in0=CC.ap[:, i, :n], in1=mean.ap[:, :n],
                                                                  op=ALU.subtract), r=[CC, mean], w=[T_])
                            self.dve.op(lambda e: e.tensor_tensor(out=T_.ap[:, :n], in0=T_.ap[:, :n], in1=rs.ap[:, :n],
                                                                  op=ALU.mult), r=[T_, rs], w=[T_])
                            self.act.op(lambda e: e.activation(out=Y.ap[:, i, :n], in_=T_.ap[:, :n], func=AF.Silu,
                                                               scale=pv.ap[:, PV_CLG + i:PV_CLG + i + 1],
                                                               bias=pv.ap[:, PV_CLB + i:PV_CLB + i + 1]),
                                        r=[T_, pv], wm=[Y])
                        self.sp.dma(out=yv[:, :, c0:c0 + n], in_=Y.ap[:, :, :n], S=Y.sem, r=[Y], wm=[self.yT])
                        if b == nblk - 1:
                            for i in range(4):
                                self.dve.op(lambda e: e.tensor_copy(out=stout.ap[:, i, :].rearrange("p (s r) -> p s r", r=30),
                                                                    in_=UF.ap[:, i, :, cw:cw + 30]), r=[UF], wm=[stout])

                    front(0)
                    for b in range(nblk):
                        if b + 1 < nblk:
                            front(b + 1)
                        back(b)
                    dst = (self.o_s["cconv"][l].rearrange("s r c -> (s r) c") if is_s else self.o_p["cconv"][l])
                    self.feat2tok(es, "c_fo", stout, 30 * nseq, GW, dst)
                    self.barrier()

    def mix_attn(self, l):
        TP, NS, TT = self.TP, self.NS, self.TT
        TBIDX = {128: 0, 0: 1, 192: 2, 64: 3}
        with ExitStack() as es0:
            tb = self.sb(es0, "d_tb", [128, 8, 5, 64], F32, dma=True)
            rbr = self.sb(es0, "d_rbr", [128, 8, 257], F32, dma=True)
            bmax = self.sb(es0, "d_bmax", [128, 8], F32)
            self.sp.dma(out=tb.ap[:, :, :, :], in_=self.biasT[l].rearrange("p (h t q) -> p h t q", h=8, t=5), S=tb.sem, w=[tb])
            self.sp.dma(out=rbr.ap[:, :, :], in_=self.rb_rep[l].rearrange("p (h r) -> p h r", h=8), S=rbr.sem, w=[rbr])
            self.dve.op(lambda e: e.scalar_tensor_tensor(out=rbr.ap[:, :, :], in0=rbr.ap[:, :, :], scalar=-1.0, in1=rbr.ap[:, :, :],
                                                         op0=ALU.mult, op1=ALU.max), r=[rbr], w=[rbr])
            self.dve.op(lambda e: e.tensor_reduce(out=bmax.ap[:, :], in_=rbr.ap[:, :, :], axis=AX.X, op=ALU.max),
                        r=[rbr], w=[bmax])
            if TP >= 512:
                self.sp.dma(out=self.o_p["k"][l], in_=self.ktok.ap[TP - 512:TP, :], S=self.scr_sem, r=[self.ktok])
                self.sp.dma(out=self.o_p["v"][l], in_=self.vtok_a.ap[TP - 512:TP, :], S=self.scr_sem, r=[self.vtok_a])
            self.sp.dma(out=self.o_s["k"][l].rearrange("s r c -> (s r) c"), in_=self.ktok.ap[TP:TT, :], S=self.scr_sem,
                        r=[self.ktok])
            self.sp.dma(out=self.o_s["v"][l].rearrange("s r c -> (s r) c"), in_=self.vtok_a.ap[TP:TT, :], S=self.scr_sem,
                        r=[self.vtok_a])
            seqs = [(False, 0, TP, 0, 64, 0)] + [(True, s, 16, 512, 16, TP + 16 * s) for s in range(NS)]
            import os
            self.alv = int(os.environ.get('DBG_AT', '9'))
            if self.alv < 1:
                self.barrier()
                return
            self._attn_seq(l, tb, bmax, *seqs[0], TBIDX)
            with ExitStack() as es_s:
                self._reuse, self._reuse_es = {}, es_s
                for sq_ in seqs[1:]:
                    self._attn_seq(l, tb, bmax, *sq_, TBIDX)
                self._reuse = None
                self.barrier()

    def _attn_seq(self, l, tb, bmax, is_s, sidx, T, Lc, CL, col0, TBIDX):
        import os
        NK = Lc + T
        NKT = (NK + 127) // 128
        zq = self.zT.ap[O_DQ:O_DQ + 512, :].rearrange("(i p) t -> p i t", p=128)
        zk = self.zT.ap[O_DK:O_DK + 512, :].rearrange("(i p) t -> p i t", p=128)
        yv = self.yT.ap[1536:2048, :].rearrange("(i p) t -> p i t", p=128)
        with ExitStack() as es:
            QT = self.sb(es, "d_qt", [128, 4, T], BF16)
            KT = self.sb(es, "d_kt", [128, 4, NK], BF16)
            VA = self.sb(es, "d_va", [128, NKT, 8, 65], BF16)
            ld = [self.sb(es, f"d_ld{i}", [128, 4, 512], F32, dma=True) for i in range(2)]
            sqb = [self.sb(es, f"d_sq{i}", [128, 4, 512], BF16) for i in range(2)]
            nb_k = (NK + 511) // 512 + 1
            nb_q = (T + 511) // 512
            kmx = self.sb(es, "d_kmx", [128, 8, nb_k], F32)
            qmx = self.sb(es, "d_qmx", [128, 8, nb_q], F32)
            k2 = self.sb(es, "d_k2", [128, 8], F32)
            q2 = self.sb(es, "d_q2", [128, 8], F32)
            negM = self.sb(es, "d_negm", [128, 8], F32)
            cM = self.sb(es, "d_cm", [128, 8], F32)
            nld = [0]

            def stat_block(L, ncols, mx, blk):
                SQ = sqb[nld[0] % 2]
                self.act.op(lambda e: e.activation(out=SQ.ap[:, :, :ncols], in_=L.ap[:, :, :ncols], func=AF.Square),
                            r=[L], w=[SQ])
                for pr in range(4):
                    for hb, sel in ((0, self.selA), (1, self.selB)):
                        ps = self.psb()
                        self.pe.op(lambda e: e.matmul(ps.ap[:, :ncols], lhsT=sel.ap[:, :], rhs=SQ.ap[:, pr, :ncols],
                                                      start=True, stop=True), r=[sel, SQ], w=[ps])
                        h = pr * 2 + hb
                        self.dve.op(lambda e: e.tensor_reduce(out=mx.ap[:, h, blk:blk + 1], in_=ps.ap[:, :ncols],
                                                              axis=AX.X, op=ALU.max), r=[ps], wm=[mx])

            kb = 0
            if is_s:
                with ExitStack() as es2:
                    kc = self.sb(es2, "d_kc", [128, 4, 512], F32, dma=True)
                    kf = self.sb(es2, "d_kf", [128, 4, 512], F32)
                    self.sp.dma(out=kc.ap[:, :, :], in_=self.st_k[l, sidx].rearrange("(t p) c -> p t c", p=128), S=kc.sem, w=[kc])
                    for pr in range(4):
                        ps = self.psb()
                        for t in range(4):
                            self.pe.op(lambda e: e.transpose(out=ps.ap[:, t * 128:(t + 1) * 128],
                                                             in_=kc.ap[:, t, pr * 128:(pr + 1) * 128],
                                                             identity=self.ident.ap[:, :]), r=[kc, self.ident], w=[ps])
                        self.copy(self.evac_eng(), kf.ap[:, pr, :], ps.ap[:, :], r=[ps], wm=[kf])
                    self.copy(self.dve, KT.ap[:, :, 0:512], kf.ap[:, :, :], r=[kf], wm=[KT])
                    nld[0] += 1
                    stat_block(kf, 512, kmx, kb)
                    kb += 1
                    self.barrier()
            for (c0, cw) in split_even(T, 512):
                L = ld[nld[0] % 2]
                nld[0] += 1
                self.sp.dma(out=L.ap[:, :, :cw], in_=zk[:, :, col0 + c0:col0 + c0 + cw], S=L.sem, r=[self.zT], w=[L])
                self.copy(self.dve, KT.ap[:, :, Lc + c0:Lc + c0 + cw], L.ap[:, :, :cw], r=[L], wm=[KT])
                stat_block(L, cw, kmx, kb)
                kb += 1
            qb_ = 0
            for (c0, cw) in split_even(T, 512):
                L = ld[nld[0] % 2]
                nld[0] += 1
                self.sp.dma(out=L.ap[:, :, :cw], in_=zq[:, :, col0 + c0:col0 + c0 + cw], S=L.sem, r=[self.zT], w=[L])
                self.act.op(lambda e: e.activation(out=L.ap[:, :, :cw], in_=L.ap[:, :, :cw], func=AF.Copy, scale=0.125),
                            r=[L], w=[L])
                self.copy(self.dve, QT.ap[:, :, c0:c0 + cw], L.ap[:, :, :cw], r=[L], wm=[QT])
                stat_block(L, cw, qmx, qb_)
                qb_ += 1
            self.dve.op(lambda e: e.tensor_reduce(out=k2.ap[:, :], in_=kmx.ap[:, :, :kb], axis=AX.X, op=ALU.max), r=[kmx], w=[k2])
            self.dve.op(lambda e: e.tensor_reduce(out=q2.ap[:, :], in_=qmx.ap[:, :, :qb_], axis=AX.X, op=ALU.max), r=[qmx], w=[q2])
            self.dve.op(lambda e: e.tensor_tensor(out=k2.ap[:, :], in0=k2.ap[:, :], in1=q2.ap[:, :], op=ALU.mult), r=[k2, q2], w=[k2])
            self.act.op(lambda e: e.activation(out=k2.ap[:, :], in_=k2.ap[:, :], func=AF.Sqrt), r=[k2], w=[k2])
            self.dve.op(lambda e: e.scalar_tensor_tensor(out=negM.ap[:, :], in0=k2.ap[:, :], scalar=-1.0, in1=bmax.ap[:, :],
                                                         op0=ALU.mult, op1=ALU.subtract), r=[k2, bmax], w=[negM])
            self.dve.op(lambda e: e.tensor_tensor(out=cM.ap[:, :], in0=tb.ap[:, :, 4, 0], in1=negM.ap[:, :], op=ALU.add),
                        r=[tb, negM], w=[cM])
            self.pool.op(lambda e: e.memset(VA.ap[:, :, :, 64:65], 1.0), wm=[VA])
            vsrcs = []
            if is_s:
                vsrcs.append((self.st_v[l, sidx], 512, 0, None))
            vsrcs.append((self.vtok_a.ap[col0:col0 + T, :], T, Lc // 128, self.vtok_a))
            vl = [self.sb(es, f"d_vl{i}", [128, 4, 512], F32, dma=True) for i in range(2)]
            nv = 0
            for (vap, rows, t0, dep) in vsrcs:
                for r0 in range(0, rows, 512):
                    rn = min(512, rows - r0)
                    V = vl[nv % 2]
                    nv += 1
                    rd = [dep] if dep is not None else []
                    if rn >= 128:
                        nt = rn // 128
                        self.sp.dma(out=V.ap[:, :nt, :], in_=vap[r0:r0 + rn, :].rearrange("(t p) c -> p t c", p=128),
                                    S=V.sem, r=rd, w=[V])
                        self.copy(self.act, VA.ap[:, t0 + r0 // 128:t0 + r0 // 128 + nt, :, 0:64],
                                  V.ap[:, :nt, :].rearrange("p t (h d) -> p t h d", h=8), r=[V], wm=[VA])
                    else:
                        self.sp.dma(out=V.ap[:rn, 0, :], in_=vap[r0:r0 + rn, :], S=V.sem, r=rd, w=[V])
                        self.copy(self.act, VA.ap[:rn, t0 + r0 // 128, :, 0:64],
                                  V.ap[:rn, 0, :].rearrange("p (h d) -> p h d", h=8), r=[V], wm=[VA])
            NB = 4
            ET = [self.sb(es, f"d_et{i}", [128, 5 * CL], BF16) for i in range(NB)]
            TM = [self.sb(es, f"d_tm{i}", [128, 2 * CL], F32) for i in range(NB)]
            rden = [self.sb(es, f"d_rd{i}", [128, 8], F32) for i in range(2)]
            otok = [self.sb(es, f"d_ot{i}", [128, 8, 64], F32) for i in range(2)]
            GB = 512 // CL if not is_s else 1
            yst = [self.sb(es, f"d_ys{i}", [128, 4, GB * CL], BF16, dma=True) for i in range(2)]
            pso = [self.ps[6], self.ps[7]]
            nchunk = T // CL

            def chunk_tiles(c):
                qs = Lc + c * CL
                kmin = max(0, qs - 512)
                kend = qs + CL
                tiles = []
                for kt in range(kmin // 128, (kend - 1) // 128 + 1):
                    p0 = max(kmin, kt * 128) - kt * 128
                    p1 = min(kend, kt * 128 + 128) - kt * 128
                    tiles.append((kt, p0, p1, qs - kt * 128))
                return tiles, sum(1 for t in tiles if t[3] >= 256)

            def qk(c, h):
                tiles, nconst = chunk_tiles(c)
                pr, hb = h // 2, (h % 2) * 64
                ps = self.psb()
                for si, (kt, p0, p1, d0) in enumerate(tiles):
                    mend = p1 if p0 == 0 else 128
                    self.pe.op(lambda e: e.matmul(ps.ap[:mend, si * CL:(si + 1) * CL],
                                                  lhsT=KT.ap[hb:hb + 64, pr, kt * 128:kt * 128 + mend],
                                                  rhs=QT.ap[hb:hb + 64, pr, c * CL:(c + 1) * CL],
                                                  start=True, stop=True), r=[KT, QT], w=[ps])
                return ps

            def softmax_pv(c, h, ps, E, Tm):
                tiles, nconst = chunk_tiles(c)
                toe = []
                for si, (kt, p0, p1, d0) in enumerate(tiles):
                    if d0 >= 256:
                        continue
                    j = si - nconst
                    mend = p1 if p0 == 0 else 128
                    self.dve.op(lambda e: e.tensor_tensor(out=Tm.ap[:mend, j * CL:(j + 1) * CL], in0=ps.ap[:mend, si * CL:(si + 1) * CL],
                                                          in1=tb.ap[:mend, h, TBIDX[d0], 0:CL], op=ALU.add),
                                r=[ps, tb], wm=[Tm])
                    toe.append((si, j, mend))
                if nconst:
                    self.act.op(lambda e: e.activation(out=E.ap[:, :nconst * CL], in_=ps.ap[:, :nconst * CL], func=AF.Exp,
                                                       bias=cM.ap[:, h:h + 1]), r=[ps, cM], w=[E])
                if tiles[0][1] == 64:
                    self.pool.op(lambda e: e.memset(E.ap[0:64, 0:CL], 0.0), w=[E])
                for (si, j, mend) in toe:
                    self.act.op(lambda e: e.activation(out=E.ap[:mend, si * CL:(si + 1) * CL], in_=Tm.ap[:mend, j * CL:(j + 1) * CL],
                                                       func=AF.Exp, bias=negM.ap[:mend, h:h + 1]), r=[Tm, negM], wm=[E])
                po = pso[h // 4]
                hh = h % 4
                for si, (kt, p0, p1, d0) in enumerate(tiles):
                    if p0 == 64:
                        p0 = 0
                    self.pe.op(lambda e: e.matmul(po.ap[:CL, hh * 65:(hh + 1) * 65], lhsT=E.ap[p0:p1, si * CL:(si + 1) * CL],
                                                  rhs=VA.ap[p0:p1, kt, h, :], start=(si == 0), stop=(si == len(tiles) - 1)),
                               r=[E, VA], w=[po])

            def finalize(c):
                RD, OT = rden[c % 2], otok[c % 2]
                for g in range(2):
                    pov = pso[g].ap[:CL, :260].rearrange("p (h d) -> p h d", d=65)
                    self.dve.op(lambda e: e.reciprocal(out=RD.ap[:CL, g * 4:(g + 1) * 4], in_=pov[:, :, 64]), r=[pso[g]], wm=[RD])
                    self.dve.op(lambda e: e.tensor_tensor(out=OT.ap[:CL, g * 4:(g + 1) * 4, :], in0=pov[:, :, 0:64],
                                                          in1=RD.ap[:CL, g * 4:(g + 1) * 4].unsqueeze(2).to_broadcast([CL, 4, 64]),
                                                          op=ALU.mult), r=[pso[g], RD], wm=[OT])
                Y = yst[(c // GB) % 2]
                pst = self.psb()
                otv = OT.ap[:CL, :, :].rearrange("p h d -> p (h d)")
                for i in range(4):
                    self.pe.op(lambda e: e.transpose(out=pst.ap[:, i * CL:(i + 1) * CL], in_=otv[:, i * 128:(i + 1) * 128],
                                                     identity=self.ident.ap[:CL, :CL]), r=[OT, self.ident], w=[pst])
                cc_ = (c % GB) * CL
                self.copy(self.act, Y.ap[:, :, cc_:cc_ + CL], pst.ap[:, :4 * CL].rearrange("p (i c) -> p i c", i=4),
                          r=[pst], wm=[Y])
                if (c % GB) == GB - 1 or c == nchunk - 1:
                    g0 = (c // GB) * GB * CL
                    gw = (c % GB + 1) * CL
                    self.sp.dma(out=yv[:, :, col0 + g0:col0 + g0 + gw], in_=Y.ap[:, :, :gw], S=Y.sem, r=[Y], wm=[self.yT])

            SKEW = 2
            items = [(c, h) for c in range(nchunk) for h in range(8)]
            pend = []
            for i, (c, h) in enumerate(items):
                pend.append((c, h, qk(c, h), ET[i % NB], TM[i % NB]))
                if len(pend) > SKEW:
                    it = pend.pop(0)
                    softmax_pv(*it)
                    if it[1] == 7:
                        finalize(it[0])
            while pend:
                it = pend.pop(0)
                softmax_pv(*it)
                if it[1] == 7:
                    finalize(it[0])
            self.barrier()

    def mix_gla(self, l):
        TP, NS = self.TP, self.NS
        with ExitStack() as es0:
            w2f = self.sb(es0, "b_w2f", [16, 256], F32, dma=True)
            w2 = self.sb(es0, "b_w2", [16, 256], BF16)
            negb = self.sb(es0, "b_negb", [64, 4], F32)
            self.sp.dma(out=w2f.ap[:, :], in_=self.gla_w2[l], S=w2f.sem, w=[w2f])
            self.copy(self.dve, w2.ap[:, :], w2f.ap[:, :], r=[w2f], w=[w2])
            self.dve.op(lambda e: e.tensor_scalar(out=negb.ap[:, :], in0=self.pv.ap[:64, PV_GLAB4:PV_GLAB4 + 4], scalar1=-1.0,
                                                  scalar2=None, op0=ALU.mult), r=[self.pv], w=[negb])
            seqs = [(False, 0, TP, 64, 0)] + [(True, s, 16, 16, TP + 16 * s) for s in range(NS)]
            self._gla_seq(l, w2, negb, *seqs[0])
            with ExitStack() as es_s:
                self._reuse, self._reuse_es = {}, es_s
                for sq_ in seqs[1:]:
                    self._gla_seq(l, w2, negb, *sq_)
                self._reuse = None
                self.barrier()

    def _gla_seq(self, l, w2, negb, is_s, sidx, T, L, col0):
        pv = self.pv
        zT = self.zT.ap
        zq = zT[O_Q:O_Q + 256, :].rearrange("(h p) t -> p h t", p=64)
        zk = zT[O_K:O_K + 256, :].rearrange("(h p) t -> p h t", p=64)
        zg = zT[O_G:O_G + 512, :].rearrange("(i p) t -> p i t", p=128)
        yv = self.yT.ap[512:1024, :].rearrange("(i p) t -> p i t", p=128)
        BW = min(T, 512)
        CPB = BW // L
        bc = lambda ap, shape: ap.unsqueeze(2).to_broadcast(shape)
        with ExitStack() as es:
            S = self.sb(es, "b_S", [64, 4, 128], F32, dma=True)
            Sb = [self.sb(es, f"b_Sb{i}", [64, 4, 128], BF16) for i in range(2)]
            if is_s:
                self.sp.dma(out=S.ap[:, :, :], in_=self.st_gla[l, sidx].rearrange("(h p) v -> p h v", p=64), S=S.sem, w=[S])
            else:
                self.dve.op(lambda e: e.memset(S.ap[:, :, :], 0.0), w=[S])
            self.copy(self.act, Sb[0].ap[:, :, :], S.ap[:, :, :], r=[S], w=[Sb[0]])
            nS = 0
            rmask = self.sb(es, "b_rm", [64, BW], F32)
            self.dve.op(lambda e: e.memset(rmask.ap[:, :], 1.0), w=[rmask])
            self.dve.op(lambda e: e.memset(rmask.ap[:, :].rearrange("p (c t) -> p c t", t=L)[:, :, 0:1], 0.0), w=[rmask])
            rT = self.sb(es, "b_rT", [16, BW], F32, dma=True)
            rTb = self.sb(es, "b_rTb", [16, BW], BF16)
            qf = self.sb(es, "b_qf", [64, 4, BW], F32, dma=True)
            kf = self.sb(es, "b_kf", [64, 4, BW], F32, dma=True)
            gf = self.sb(es, "b_gf", [128, 4, BW], F32, dma=True)
            vf = self.sb(es, "b_vf", [64, CPB, 512], F32, dma=True)
            vb = self.sb(es, "b_vb", [64, CPB, 512], BF16)
            yy = self.sb(es, "b_y", [64, BW], F32)
            ay = self.sb(es, "b_ay", [64, BW], F32)
            la = self.sb(es, "b_la", [64, 4, BW], F32)
            bb = self.sb(es, "b_b", [64, 4, BW], F32)
            e3 = self.sb(es, "b_e3", [64, 4, BW], F32)
            ei = self.sb(es, "b_ei", [64, 4, BW], F32)
            qb = self.sb(es, "b_qb", [64, 4, BW], BF16)
            kinv = self.sb(es, "b_kinv", [64, 4, BW], F32)
            qd = [self.sb(es, f"b_qd{i}", [64, 4, L], BF16) for i in range(2)]
            kd = [self.sb(es, f"b_kd{i}", [64, 4, L], BF16) for i in range(2)]
            kl = [self.sb(es, f"b_kl{i}", [64, 4, L], F32) for i in range(2)]
            klt = [self.sb(es, f"b_klt{i}", [64, 256], BF16) for i in range(2)]
            am = [self.sb(es, f"b_am{i}", [64, 4, L], BF16) for i in range(2)]
            oT = self.sb(es, "b_oT", [128, 4, BW], F32)
            osq = self.sb(es, "b_osq", [128, 4, BW], F32)
            rs4 = [self.sb(es, f"b_rs{i}", [128, BW], F32) for i in range(2)]
            ot = self.sb(es, "b_ot", [128, BW], F32)
            yo = [self.sb(es, f"b_yo{i}", [128, 4, BW], BF16, dma=True) for i in range(2)]
            psu = self.ps[6]
            for bi in range((T + BW - 1) // BW):
                c0 = col0 + bi * BW
                self.sp.dma(out=rT.ap[:, :], in_=zT[O_R:O_R + 16, c0:c0 + BW], S=rT.sem, r=[self.zT], w=[rT])
                self.sp.dma(out=qf.ap[:, :, :], in_=zq[:, :, c0:c0 + BW], S=qf.sem, r=[self.zT], w=[qf])
                self.sp.dma(out=kf.ap[:, :, :], in_=zk[:, :, c0:c0 + BW], S=kf.sem, r=[self.zT], w=[kf])
                self.sp.dma(out=gf.ap[:, :, :], in_=zg[:, :, c0:c0 + BW], S=gf.sem, r=[self.zT], w=[gf])
                self.sp.dma(out=vf.ap[:L, :, :], in_=self.vtok_g.ap[c0:c0 + BW, :].rearrange("(c p) v -> p c v", p=L),
                            S=vf.sem, r=[self.vtok_g], w=[vf])
                self.copy(self.act, vb.ap[:L, :, :], vf.ap[:L, :, :], r=[vf], w=[vb])
                self.copy(self.dve, rTb.ap[:, :], rT.ap[:, :], r=[rT], w=[rTb])
                for h in range(4):
                    ps = self.psb()
                    self.pe.op(lambda e: e.matmul(ps.ap[:64, :BW], lhsT=w2.ap[:, h * 64:(h + 1) * 64], rhs=rTb.ap[:, :],
                                                  start=True, stop=True), r=[w2, rTb], w=[ps])
                    self.dve.op(lambda e: e.tensor_scalar(out=yy.ap[:, :], in0=ps.ap[:64, :BW], scalar1=-1.0,
                                                          scalar2=negb.ap[:, h:h + 1], op0=ALU.mult, op1=ALU.add),
                                r=[ps, negb], w=[yy])
                    self.dve.op(lambda e: e.scalar_tensor_tensor(out=ay.ap[:, :], in0=yy.ap[:, :], scalar=-1.0, in1=yy.ap[:, :],
                                                                 op0=ALU.mult, op1=ALU.max), r=[yy], w=[ay])
                    self.act.op(lambda e: e.activation(out=ay.ap[:, :], in_=ay.ap[:, :], func=AF.Exp, scale=-1.0), r=[ay], w=[ay])
                    self.act.op(lambda e: e.activation(out=ay.ap[:, :], in_=ay.ap[:, :], func=AF.Ln, bias=self.onec.ap[:64, :]),
                                r=[ay, self.onec], w=[ay])
                    self.dve.op(lambda e: e.scalar_tensor_tensor(out=yy.ap[:, :], in0=yy.ap[:, :], scalar=0.0, in1=ay.ap[:, :],
                                                                 op0=ALU.max, op1=ALU.add), r=[yy, ay], w=[yy])
                    self.dve.op(lambda e: e.tensor_scalar(out=la.ap[:, h, :], in0=yy.ap[:, :], scalar1=-1.0 / 16.0, scalar2=None,
                                                          op0=ALU.mult), r=[yy], wm=[la])
                    self.dve.op(lambda e: e.tensor_tensor_scan(out=bb.ap[:, h, :], data0=rmask.ap[:, :], data1=la.ap[:, h, :],
                                                               initial=0.0, op0=ALU.mult, op1=ALU.add), r=[rmask, la], wm=[bb])
                self.act.op(lambda e: e.activation(out=e3.ap[:, :, :], in_=bb.ap[:, :, :], func=AF.Exp), r=[bb], w=[e3])
                self.act.op(lambda e: e.activation(out=ei.ap[:, :, :], in_=bb.ap[:, :, :], func=AF.Exp, scale=-1.0), r=[bb], w=[ei])
                self.dve.op(lambda e: e.scalar_tensor_tensor(out=qf.ap[:, :, :], in0=qf.ap[:, :, :], scalar=0.125, in1=e3.ap[:, :, :],
                                                             op0=ALU.mult, op1=ALU.mult), r=[qf, e3], w=[qf])
                self.copy(self.act, qb.ap[:, :, :], qf.ap[:, :, :], r=[qf], w=[qb])
                self.dve.op(lambda e: e.tensor_tensor(out=kinv.ap[:, :, :], in0=kf.ap[:, :, :], in1=ei.ap[:, :, :], op=ALU.mult),
                            r=[kf, ei], w=[kinv])
                def stage_a(c):
                    t0, t1, mid = c * L, (c + 1) * L, c * L + L // 2
                    QD, KD, KL, KLT, AM = (x[c % 2] for x in (qd, kd, kl, klt, am))
                    self.dve.op(lambda e: e.tensor_tensor(out=QD.ap[:, :, :], in0=qf.ap[:, :, t0:t1], in1=bc(ei.ap[:, :, mid], [64, 4, L]),
                                                          op=ALU.mult), r=[qf, ei], w=[QD])
                    self.dve.op(lambda e: e.tensor_tensor(out=KD.ap[:, :, :], in0=kinv.ap[:, :, t0:t1], in1=bc(e3.ap[:, :, mid], [64, 4, L]),
                                                          op=ALU.mult), r=[kinv, e3], w=[KD])
                    self.dve.op(lambda e: e.tensor_tensor(out=KL.ap[:, :, :], in0=kinv.ap[:, :, t0:t1], in1=bc(e3.ap[:, :, t1 - 1], [64, 4, L]),
                                                          op=ALU.mult), r=[kinv, e3], w=[KL])
                    pst = self.psb()
                    for h in range(4):
                        self.pe.op(lambda e: e.transpose(out=pst.ap[:L, h * 64:(h + 1) * 64], in_=KL.ap[:, h, :],
                                                         identity=self.ident.ap[:64, :64]), r=[KL, self.ident], w=[pst])
                    self.copy(self.act, KLT.ap[:L, :], pst.ap[:L, :256], r=[pst], w=[KLT])
                    psa = self.psb()
                    for h in range(4):
                        self.pe.op(lambda e: e.matmul(psa.ap[:L, h * L:(h + 1) * L], lhsT=KD.ap[:, h, :], rhs=QD.ap[:, h, :],
                                                      start=True, stop=True), r=[KD, QD], w=[psa])
                    self.dve.op(lambda e: e.tensor_tensor(out=AM.ap[:L, :, :], in0=psa.ap[:L, :4 * L].rearrange("p (h t) -> p h t", h=4),
                                                          in1=self.trimask.ap[:L, :L].unsqueeze(1).to_broadcast([L, 4, L]), op=ALU.mult),
                                r=[psa, self.trimask], w=[AM])

                def stage_b(c, nS):
                    t0, t1 = c * L, (c + 1) * L
                    KLT, AM = klt[c % 2], am[c % 2]
                    pso = self.psb()
                    SB = Sb[nS % 2]
                    for h in range(4):
                        self.pe.op(lambda e: e.matmul(pso.ap[:, h * L:(h + 1) * L], lhsT=vb.ap[:L, c, h * 128:(h + 1) * 128], rhs=AM.ap[:L, h, :],
                                                      start=True, stop=False), r=[vb, AM], w=[pso])
                        self.pe.op(lambda e: e.matmul(pso.ap[:, h * L:(h + 1) * L], lhsT=SB.ap[:, h, :], rhs=qb.ap[:, h, t0:t1],
                                                      start=False, stop=True), r=[SB, qb], w=[pso])
                    self.copy(self.act, oT.ap[:, :, t0:t1], pso.ap[:, :4 * L].rearrange("p (h t) -> p h t", h=4), r=[pso], wm=[oT])
                    for h in range(4):
                        self.pe.op(lambda e: e.matmul(psu.ap[:64, h * 128:(h + 1) * 128], lhsT=KLT.ap[:L, h * 64:(h + 1) * 64],
                                                      rhs=vb.ap[:L, c, h * 128:(h + 1) * 128], start=True, stop=True), r=[KLT, vb], w=[psu])
                    self.dve.op(lambda e: e.tensor_tensor(out=S.ap[:, :, :], in0=S.ap[:, :, :], in1=bc(e3.ap[:, :, t1 - 1], [64, 4, 128]),
                                                          op=ALU.mult), r=[S, e3], w=[S])
                    self.dve.op(lambda e: e.tensor_tensor(out=S.ap[:, :, :], in0=S.ap[:, :, :],
                                                          in1=psu.ap[:64, :].rearrange("p (h v) -> p h v", h=4), op=ALU.add),
                                r=[S, psu], w=[S])
                    self.copy(self.act, Sb[(nS + 1) % 2].ap[:, :, :], S.ap[:, :, :], r=[S], w=[Sb[(nS + 1) % 2]])

                stage_a(0)
                for c in range(CPB):
                    if c + 1 < CPB:
                        stage_a(c + 1)
                    stage_b(c, nS)
                    nS += 1
                self.act.op(lambda e: e.activation(out=osq.ap[:, :, :], in_=oT.ap[:, :, :], func=AF.Square), r=[oT], w=[osq])
                Y = yo[bi % 2]
                for h in range(4):
                    self.act.op(lambda e: e.activation(out=gf.ap[:, h, :], in_=gf.ap[:, h, :], func=AF.Silu), r=[gf], w=[gf])
                for h in range(4):
                    ps = self.psb()
                    self.pe.op(lambda e: e.matmul(ps.ap[:, :BW], lhsT=self.ones32.ap[:, :], rhs=osq.ap[:, h, :], start=True, stop=True),
                               r=[self.ones32, osq], w=[ps])
                    RS = rs4[h % 2]
                    self.rstd(RS.ap[:, :], ps.ap[:, :BW], 1.0 / 128, [ps], RS)
                    self.dve.op(lambda e: e.tensor_tensor(out=ot.ap[:, :], in0=oT.ap[:, h, :], in1=RS.ap[:, :], op=ALU.mult),
                                r=[oT, RS], w=[ot])
                    self.dve.op(lambda e: e.scalar_tensor_tensor(out=Y.ap[:, h, :], in0=ot.ap[:, :], scalar=pv.ap[:, PV_GLAG:PV_GLAG + 1],
                                                                 in1=gf.ap[:, h, :], op0=ALU.mult, op1=ALU.mult), r=[ot, pv, gf], wm=[Y])
                self.sp.dma(out=yv[:, :, c0:c0 + BW], in_=Y.ap[:, :, :], S=Y.sem, r=[Y], wm=[self.yT])
            dst = self.o_s["gla"][l, sidx] if is_s else self.o_p["gla"][l]
            self.sp.dma(out=dst.rearrange("(h p) v -> p h v", p=64), in_=S.ap[:, :, :], S=S.sem, r=[S])
            self.barrier()

    def phase_resproj(self, src, KT, W, CW, BWID, dbl=False, src_r0=0):
        blocks = [([(W, c0, BWID)], "f") for c0 in range(0, D, BWID)]
        with ExitStack() as es:
            xr = [self.sb(es, f"r_x{i}", [128, CW], F32, dma=True) for i in range(2)]
            cnt = [0]

            def handler(tag, parts, wt, xin, s0, sw):
                (_, c0, wd) = parts[0]
                for m0 in range(0, wd, 128):
                    kt = (c0 + m0) // 128
                    X = xr[cnt[0] % 2]
                    cnt[0] += 1
                    xt = self.xTt[kt]
                    self.sp.dma(out=X.ap[:, :sw], in_=self.xT.ap[kt * 128:(kt + 1) * 128, s0:s0 + sw], S=X.sem, r=[xt], w=[X])
                    for (n0, nw) in split_even(sw, 512):
                        ps = self.mm_f(wt, m0, 128, xin, n0, nw, KT)
                        self.dve.op(lambda e: e.tensor_tensor(out=X.ap[:, n0:n0 + nw], in0=ps.ap[:, :nw], in1=X.ap[:, n0:n0 + nw],
                                                              op=ALU.add), r=[ps, X], w=[X])
                    self.sp.dma(out=self.xT.ap[kt * 128:(kt + 1) * 128, s0:s0 + sw], in_=X.ap[:, :sw], S=X.sem, r=[X], wm=[xt])

            self.linear(es, src, KT, blocks, CW, BWID, handler, dbl=dbl, src_r0=src_r0)
            self.barrier()

    def phase_ffn_hidden(self, l):
        TP, NS, TT = self.TP, self.NS, self.TT
        pv = self.pv
        CW = 2080
        Wg, Wu = self.w_gate[l], self.w_up[l]
        blocks = [([(Wg, c0, 256), (Wu, c0, 256)], "g") for c0 in range(0, DFF, 256)]
        seqs = self.seq_ranges()
        with ExitStack() as es:
            ghp = self.sb(es, "f_ghp", [128, 44, 2], F32)
            self.dve.op(lambda e: e.memset(ghp.ap[:, :, :], 0.0), w=[ghp])
            ghs = self.sb(es, "f_ghs", [128, 44, 2 * NS], F32)
            with ExitStack() as es2:
                t = self.tok2feat(es2, "f_hi", self.st_ffn[l].rearrange("s r c -> (s r) c"), 2 * NS, DFF)
                self.copy(self.dve, ghs.ap[:, :, :], t.ap[:, :, :], r=[t], w=[ghs])
                self.barrier()
            with ExitStack() as es3:
                WX = CW + 2 * (1 + NS)
                ge = [self.sb(es3, f"f_ge{i}", [128, WX], F32) for i in range(2)]
                ub = [self.sb(es3, f"f_ub{i}", [128, WX], F32) for i in range(2)]
                cb = [self.sb(es3, f"f_cb{i}", [128, WX], F32) for i in range(2)]
                ab = [self.sb(es3, f"f_ab{i}", [128, WX], BF16, dma=True) for i in range(2)]
                for u_ in ub:
                    self.pool.op(lambda e: e.memset(u_.ap[:, :], 0.0), w=[u_])
                cnt = [0]

                def hist_ap(j, q):
                    return ghp.ap[:, j, :] if q == 0 else ghs.ap[:, j, 2 * (q - 1):2 * q]

                import os
                flv = int(os.environ.get('DBG_FF', '9'))

                def handler(tag, parts, wt, xin, s0, sw):
                    c0 = parts[0][1]
                    if flv < 2:
                        return
                    segs = []
                    o = 0
                    for q, (a0, ln) in enumerate(seqs):
                        lo, hi = max(a0, s0), min(a0 + ln, s0 + sw)
                        if lo < hi:
                            segs.append((q, lo - s0, hi - lo, o))
                            o += hi - lo + 2
                    Wc = o - 2
                    for m in range(2):
                        j = (c0 + m * 128) // 128
                        G, U, C_, A = (x[cnt[0] % 2] for x in (ge, ub, cb, ab))
                        cnt[0] += 1
                        hq = hist_ap
                        for (q, a, ln, og) in segs:
                            self.dve.op(lambda e: e.tensor_copy(out=G.ap[:, og:og + 2], in_=hq(j, q)),
                                        r=[ghp if q == 0 else ghs], wm=[G])
                        f2 = int(os.environ.get('DBG_FF2', '9'))
                        for which, dstt, off in ((0, G, 2), (1, U, 0)):
                            if f2 < 2:
                                break
                            for (n0, nw) in split_even(sw, 512):
                                ps = self.mm_f(wt, which * 256 + m * 128, 128, xin, n0, nw, 16)
                                if f2 < 3:
                                    continue
                                for (q, a, ln, og) in segs:
                                    lo, hi = max(n0, a), min(n0 + nw, a + ln)
                                    if lo < hi:
                                        self.copy(self.evac_eng(), dstt.ap[:, og + off + lo - a:og + off + hi - a], ps.ap[:, lo - n0:hi - n0],
                                                  r=[ps], wm=[dstt])
                        if flv < 3:
                            continue
                        wc = PV_FCW + j * 3
                        self.dve.op(lambda e: e.tensor_scalar(out=C_.ap[:, :Wc], in0=G.ap[:, 0:Wc], scalar1=pv.ap[:, wc:wc + 1], scalar2=None,
                                                              op0=ALU.mult), r=[G, pv], w=[C_])
                        for t_ in (1, 2):
                            self.dve.op(lambda e: e.scalar_tensor_tensor(out=C_.ap[:, :Wc], in0=G.ap[:, t_:t_ + Wc], scalar=pv.ap[:, wc + t_:wc + t_ + 1],
                                                                         in1=C_.ap[:, :Wc], op0=ALU.mult, op1=ALU.add), r=[G, pv, C_], w=[C_])
                        self.act.op(lambda e: e.activation(out=C_.ap[:, :Wc], in_=C_.ap[:, :Wc], func=AF.Silu), r=[C_], w=[C_])
                        self.dve.op(lambda e: e.tensor_tensor(out=A.ap[:, :Wc], in0=C_.ap[:, :Wc], in1=U.ap[:, :Wc], op=ALU.mult),
                                    r=[C_, U], w=[A])
                        if flv < 4:
                            continue
                        for (q, a, ln, og) in segs:
                            self.sp.dma(out=self.aT.ap[j * 128:(j + 1) * 128, s0 + a:s0 + a + ln], in_=A.ap[:, og:og + ln], S=A.sem,
                                        r=[A], wm=[self.aT])
                            self.dve.op(lambda e: e.tensor_copy(out=hq(j, q), in_=G.ap[:, og + ln:og + ln + 2]), r=[G],
                                        wm=[ghp if q == 0 else ghs])

                self.linear(es3, self.hT, 16, blocks, CW, 512, handler)
                self.barrier()
            if flv < 5:
                self.barrier()
                return
            self.feat2tok(es, "f_op", ghp, 2, DFF, self.o_p["ffn"][l])
            self.feat2tok(es, "f_os", ghs, 2 * NS, DFF, self.o_s["ffn"][l].rearrange("s r c -> (s r) c"))
            self.barrier()

    def build(self):
        self.setup()
        self.phase_transpose_in()
        for l in range(self.depth):
            self.load_pvec(l)
            self.phase_norm(PV_GMIX)
            self.phase_inproj(l)
            self.mix_short(l)
            self.mix_gla(l)
            self.mix_cconv(l)
            self.mix_attn(l)
            self.phase_resproj(self.yT, 16, self.w_out[l], 2080, 512, dbl=True)
            self.phase_norm(PV_GFFN)
            self.phase_ffn_hidden(l)
            self.phase_resproj(self.aT, 22, self.w_down[l][0:2816, :], 2080, 512, src_r0=0)
            self.phase_resproj(self.aT, 22, self.w_down[l][2816:5632, :], 2080, 512, src_r0=2816)
        self.phase_norm(PV_GFIN, final=True)
        return self.finish()

    def finish(self):
        self.barrier()
        self.es.close()
        return self.nc


def _fm(v, ntile):
    return np.ascontiguousarray(np.asarray(v, np.float32).reshape(ntile, 128).T)


def prep_pvec(inp, depth):
    out = np.zeros((depth, 128, NV), np.float32)
    for l in range(depth):
        o = out[l]
        o[:, PV_GMIX:PV_GMIX + 16] = _fm(inp["g_mix"][l], 16)
        o[:, PV_GFFN:PV_GFFN + 16] = _fm(inp["g_ffn"][l], 16)
        for j in range(3):
            o[:, PV_CAW + j:PV_CAW + 12:3] = _fm(inp["conv_a_w"][l, j], 4)
        for j in range(31):
            o[:, PV_CCW + j:PV_CCW + 124:31] = _fm(inp["cconv_w"][l, j], 4)
        o[:, PV_CCB:PV_CCB + 4] = _fm(inp["cconv_b"][l], 4)
        o[:, PV_CLG:PV_CLG + 4] = _fm(inp["cln_g"][l], 4)
        o[:, PV_CLB:PV_CLB + 4] = _fm(inp["cln_b"][l], 4)
        o[:, PV_GLAB:PV_GLAB + 2] = _fm(inp["gla_b_gate"][l], 2)
        o[:, PV_GLAG:PV_GLAG + 1] = _fm(inp["gla_g_norm"][l], 1)
        o[:64, PV_GLAB4:PV_GLAB4 + 4] = np.asarray(inp["gla_b_gate"][l], np.float32).reshape(4, 64).T
        for j in range(3):
            o[:, PV_FCW + j:PV_FCW + 132:3] = _fm(inp["ffn_conv_w"][l, j], 44)
        o[:, PV_GFIN:PV_GFIN + 16] = _fm(inp["g_final"], 16)
    return out


def prep_bias(inp, depth):
    rb = np.asarray(inp["rel_bias"], np.float32)[:depth]
    p = np.arange(128)[:, None]
    q = np.arange(64)[None, :]
    tabs = []
    for d0 in (128, 0, 192, 64, 1024):
        idx = np.clip(d0 + q - p, -128, 128) + 128
        tabs.append(rb[:, :, idx])
    t = np.stack(tabs, axis=2)
    t = np.transpose(t, (0, 3, 1, 2, 4))
    biasT = np.ascontiguousarray(t.reshape(depth, 128, 8 * 5 * 64))
    rb_rep = np.ascontiguousarray(np.broadcast_to(rb.reshape(depth, 1, 8 * 257), (depth, 128, 8 * 257)))
    return biasT, rb_rep


_NC_CACHE = {}


def make_in_maps(inp, n_cores, TP, NS, depth):
    f32 = lambda a: np.ascontiguousarray(np.asarray(a, np.float32))
    B = inp["x_prompt"].shape[0]
    pvec = prep_pvec(inp, depth)
    biasT, rb_rep = prep_bias(inp, depth)
    shared = {
        "pvec": pvec, "biasT": biasT, "rb_rep": rb_rep,
        "w_in": f32(inp["w_in"][:depth]), "w_out": f32(inp["w_out"][:depth]),
        "w_gate": f32(inp["w_ffn_gate"][:depth]), "w_up": f32(inp["w_ffn_up"][:depth]),
        "w_down": f32(inp["w_ffn_down"][:depth]), "gla_w2": f32(inp["gla_w_gate2"][:depth]),
    }
    maps = []
    for c in range(n_cores):
        b = c % B
        s0, s1 = c * NS, (c + 1) * NS
        m = dict(shared)
        m["x_all"] = f32(np.concatenate([inp["x_prompt"][b, :TP], np.asarray(inp["x_sample"][s0:s1]).reshape(NS * 16, D)], 0))
        m["st_short"] = f32(inp["state_short_conv"][:depth, s0:s1])
        m["st_gla"] = f32(np.asarray(inp["state_gla"][:depth, s0:s1]).reshape(depth, NS, 256, 128))
        m["st_cconv"] = f32(inp["state_conformer_conv"][:depth, s0:s1])
        m["st_k"] = f32(np.asarray(inp["cache_attn_k"][:depth, s0:s1]).reshape(depth, NS, 512, GW))
        m["st_v"] = f32(np.asarray(inp["cache_attn_v"][:depth, s0:s1]).reshape(depth, NS, 512, GW))
        m["st_ffn"] = f32(inp["state_ffn_conv"][:depth, s0:s1])
        maps.append(m)
    return maps


def gather_outputs(results, B, TP, NS, depth):
    n = len(results)
    r = results
    yp = np.stack([r[b]["y_all"][:TP] for b in range(B)], 0)
    ys = np.concatenate([r[c]["y_all"][TP:].reshape(NS, 16, D) for c in range(n)], 0)
    P = lambda k: np.stack([r[b][k] for b in range(B)], 1)
    Sx = lambda k: np.concatenate([r[c][k] for c in range(n)], 1)
    kr = min(512, TP)
    outs = (
        yp, ys,
        P("p_short"), P("p_gla").reshape(depth, B, 4, 64, 128), P("p_cconv"),
        P("p_k")[:, :, :kr].reshape(depth, B, kr, 8, 64), P("p_v")[:, :, :kr].reshape(depth, B, kr, 8, 64), P("p_ffn"),
        Sx("s_short"), Sx("s_gla").reshape(depth, n * NS, 4, 64, 128), Sx("s_cconv"),
        Sx("s_k").reshape(depth, n * NS, 16, 8, 64), Sx("s_v").reshape(depth, n * NS, 16, 8, 64), Sx("s_ffn"),
    )
    return tuple(np.ascontiguousarray(o, dtype=np.float32) for o in outs)


def kernel(**inputs):
    n_cores = 8
    TP, NS, depth = 4096, 4, DEPTH
    key = (TP, NS, depth)
    if key not in _NC_CACHE:
        _NC_CACHE[key] = K(TP, NS, depth).build()
    nc = _NC_CACHE[key]
    maps = make_in_maps(inputs, n_cores, TP, NS, depth)
    res = run_bass_kernel_spmd(nc, maps, core_ids=list(range(n_cores)))
    return gather_outputs(res.results, 4, TP, NS, depth)
```
